# Optimizing a Trainium2 kernel written in Bass

```python
import jax, jax.numpy as jnp
from jax import lax
import numpy as np

D_MODEL = 2048
BATCH = 2
SEQ = 4096
DEPTH = 1

GDN_HEADS = 8
GDN_HEAD_DIM = 128
GDN_WIDTH = GDN_HEADS * GDN_HEAD_DIM
GDN_CONV = 4
GDN_CHUNK = 64
NSA_HEADS = 8
NSA_KV_HEADS = 2
NSA_HEAD_DIM = 128
NSA_WIDTH = NSA_HEADS * NSA_HEAD_DIM
NSA_KV_WIDTH = NSA_KV_HEADS * NSA_HEAD_DIM
CMP_BLOCK = 32
CMP_STRIDE = 16
SLC_BLOCK = 64
SLC_TOPK = 16
WINDOW = 512
Q_BLOCK = 128
ROPE_THETA = 500000.0
ROPE_DIM = NSA_HEAD_DIM // 4
D_FF = 5632
N_MOD = 9
N_IN = 4 * GDN_WIDTH + 2 * GDN_HEADS + NSA_WIDTH + 6 * NSA_KV_WIDTH + 3 * NSA_HEADS + 2 * D_MODEL
EPS = 1e-6
NEG_INF = -1e30
FORCE_BONUS = 1e3

kernel_name = "hybrid_gdn_nsa_macaron_adaln"


def _rms_norm(x, w):
    xf = x.astype(jnp.float32)
    y = xf * lax.rsqrt(jnp.mean(xf * xf, axis=-1, keepdims=True) + EPS)
    return (y * w.astype(jnp.float32)).astype(x.dtype)


def _modulate(x, gain, shift, scale):
    return _rms_norm(x, gain) * (1.0 + scale[:, None, :]) + shift[:, None, :]


def _swiglu(h, w_gate, w_up, w_down):
    return (jax.nn.silu(h @ w_gate) * (h @ w_up)) @ w_down


def _l2norm(x):
    return x * lax.rsqrt(jnp.sum(x * x, axis=-1, keepdims=True) + EPS)


def _masked_softmax(s, mask):
    s = jnp.where(mask, s.astype(jnp.float32), NEG_INF)
    m = jnp.max(s, axis=-1, keepdims=True)
    e = jnp.where(mask, jnp.exp(s - m), 0.0)
    den = jnp.sum(e, axis=-1, keepdims=True)
    return e / jnp.where(den > 0, den, 1.0)


def _partial_rope(x, cos, sin):
    half = ROPE_DIM // 2
    c = cos[None, :, None, :].astype(x.dtype)
    s = sin[None, :, None, :].astype(x.dtype)
    x1, x2, xp = x[..., :half], x[..., half:ROPE_DIM], x[..., ROPE_DIM:]
    return jnp.concatenate([x1 * c - x2 * s, x2 * c + x1 * s, xp], axis=-1)


def _causal_conv(x, w):
    k = w.shape[1]
    return lax.conv_general_dilated(
        x, w.T[:, None, :].astype(x.dtype), window_strides=(1,), padding=[(k - 1, 0)],
        dimension_numbers=('NWC', 'WIO', 'NWC'), feature_group_count=x.shape[-1])


def _gated_delta_rule(q, k, v, g, beta):
    b, s, h, dk = q.shape
    c = GDN_CHUNK
    n = s // c
    q = _l2norm(q) * (dk ** -0.5)
    k = _l2norm(k)

    def chunks(t):
        return t.reshape(b, n, c, h, -1).transpose(0, 3, 1, 2, 4)

    q, k, v = chunks(q), chunks(k), chunks(v)
    g = g.reshape(b, n, c, h).transpose(0, 3, 1, 2)
    beta = beta.reshape(b, n, c, h).transpose(0, 3, 1, 2)
    gc = jnp.cumsum(g, axis=-1)
    causal = jnp.tril(jnp.ones((c, c), bool))
    strict = jnp.tril(jnp.ones((c, c), bool), -1)
    decay = jnp.exp(jnp.where(causal, gc[..., :, None] - gc[..., None, :], -jnp.inf))
    k_beta = k * beta[..., None]
    lmat = jnp.where(strict, jnp.einsum('bhnid,bhnjd->bhnij', k_beta, k) * decay, 0.0)
    eye = jnp.eye(c, dtype=jnp.float32)
    tmat = lax.linalg.triangular_solve(eye + lmat, jnp.broadcast_to(eye, lmat.shape),
                                       left_side=True, lower=True, unit_diagonal=True)
    u = tmat @ (v * beta[..., None])
    w = tmat @ (k_beta * jnp.exp(gc)[..., None])
    a_intra = jnp.where(causal, jnp.einsum('bhnid,bhnjd->bhnij', q, k) * decay, 0.0)
    q_g = q * jnp.exp(gc)[..., None]
    k_d = k * jnp.exp(gc[..., -1:] - gc)[..., None]
    d_end = jnp.exp(gc[..., -1])

    def step(state, inp):
        qg_i, kd_i, u_i, w_i, a_i, de_i = inp
        v_new = u_i - w_i @ state
        o_i = qg_i @ state + a_i @ v_new
        state = state * de_i[..., None, None] + jnp.swapaxes(kd_i, -1, -2) @ v_new
        return state, o_i

    xs = tuple(jnp.moveaxis(t, 2, 0) for t in (q_g, k_d, u, w, a_intra, d_end))
    state0 = jnp.zeros((b, h, dk, v.shape[-1]), jnp.float32)
    _, o = lax.scan(step, state0, xs)
    return o.transpose(1, 0, 3, 2, 4).reshape(b, s, h, -1)


def _compress(x, pos, w1, b1, w2):
    b, s, g, d = x.shape
    r = CMP_BLOCK // CMP_STRIDE
    n_cmp = s // CMP_STRIDE - r + 1
    xs = x.reshape(b, s // CMP_STRIDE, CMP_STRIDE, g, d)
    blocks = jnp.concatenate([xs[:, j:j + n_cmp] for j in range(r)], axis=2)
    blocks = blocks + pos[None, None, :, None, :]
    flat = blocks.transpose(0, 1, 3, 2, 4).reshape(b, n_cmp, g, CMP_BLOCK * d)
    return jax.nn.silu(flat @ w1 + b1) @ w2


def _nsa(q, qr, kcmp, vcmp, ks, vs, kw, vw, gates):
    b, s, hq, d = q.shape
    g = ks.shape[2]
    r = hq // g
    scale = d ** -0.5
    q5 = q.reshape(b, s, g, r, d)
    qr5 = qr.reshape(b, s, g, r, d)
    n_cmp = kcmp.shape[1]
    cmp_end = jnp.arange(n_cmp) * CMP_STRIDE + (CMP_BLOCK - 1)
    n_slc = s // SLC_BLOCK
    topk = min(SLC_TOPK, n_slc)
    kb = ks.reshape(b, n_slc, SLC_BLOCK, g, d).transpose(0, 3, 1, 2, 4)
    vb = vs.reshape(b, n_slc, SLC_BLOCK, g, d).transpose(0, 3, 1, 2, 4)
    kw_pad = jnp.pad(kw, ((0, 0), (WINDOW, 0), (0, 0), (0, 0)))
    vw_pad = jnp.pad(vw, ((0, 0), (WINDOW, 0), (0, 0), (0, 0)))
    b_idx = jnp.arange(b)[:, None, None, None]
    g_idx = jnp.arange(g)[None, :, None, None]
    blk = jnp.arange(n_slc)

    def one_block(qb):
        q0 = qb * Q_BLOCK
        t = q0 + jnp.arange(Q_BLOCK)
        qc = lax.dynamic_slice_in_dim(q5, q0, Q_BLOCK, 1)
        qs = lax.dynamic_slice_in_dim(qr5, q0, Q_BLOCK, 1)
        gt = lax.dynamic_slice_in_dim(gates, q0, Q_BLOCK, 1)
        s_c = jnp.einsum('bqgrd,bngd->bgrqn', qc, kcmp) * scale
        p_c = _masked_softmax(s_c, cmp_end[None, :] <= t[:, None])
        o_c = jnp.einsum('bgrqn,bngd->bqgrd', p_c.astype(vcmp.dtype), vcmp)
        imp = jnp.sum(p_c, axis=2)
        imp = jnp.pad(imp, ((0, 0), (0, 0), (0, 0), (0, s // CMP_STRIDE - n_cmp)))
        imp = imp.reshape(b, g, Q_BLOCK, n_slc, SLC_BLOCK // CMP_STRIDE).sum(-1)
        tb = t // SLC_BLOCK
        visible = blk[None, :] * SLC_BLOCK <= t[:, None]
        forced = (blk[None, :] == 0) | (blk[None, :] == tb[:, None]) | (blk[None, :] == tb[:, None] - 1)
        score = jnp.where(visible, imp + jnp.where(forced, FORCE_BONUS, 0.0), NEG_INF)
        top_val, top_idx = lax.top_k(score, topk)
        k_sel = kb[b_idx, g_idx, top_idx]
        v_sel = vb[b_idx, g_idx, top_idx]
        tok = top_idx[..., None] * SLC_BLOCK + jnp.arange(SLC_BLOCK)
        sel_mask = (top_val > 0.5 * NEG_INF)[..., None] & (tok <= t[None, None, :, None, None])
        s_s = jnp.einsum('bqgrd,bgqjkd->bgrqjk', qs, k_sel) * scale
        s_s = s_s.reshape(b, g, r, Q_BLOCK, topk * SLC_BLOCK)
        p_s = _masked_softmax(s_s, sel_mask.reshape(b, g, 1, Q_BLOCK, topk * SLC_BLOCK))
        p_s = p_s.reshape(b, g, r, Q_BLOCK, topk, SLC_BLOCK).astype(v_sel.dtype)
        o_s = jnp.einsum('bgrqjk,bgqjkd->bqgrd', p_s, v_sel)
        k_win = lax.dynamic_slice_in_dim(kw_pad, q0, WINDOW + Q_BLOCK, 1)
        v_win = lax.dynamic_slice_in_dim(vw_pad, q0, WINDOW + Q_BLOCK, 1)
        kpos = q0 - WINDOW + jnp.arange(WINDOW + Q_BLOCK)
        dist = t[:, None] - kpos[None, :]
        w_mask = (kpos[None, :] >= 0) & (dist >= 0) & (dist < WINDOW)
        s_w = jnp.einsum('bqgrd,bkgd->bgrqk', qs, k_win) * scale
        p_w = _masked_softmax(s_w, w_mask)
        o_w = jnp.einsum('bgrqk,bkgd->bqgrd', p_w.astype(v_win.dtype), v_win)
        return gt[..., 0:1] * o_c + gt[..., 1:2] * o_s + gt[..., 2:3] * o_w

    out = lax.map(one_block, jnp.arange(s // Q_BLOCK))
    return out.transpose(1, 0, 2, 3, 4, 5).reshape(b, s, hq * d)


def _split_points():
    sizes = [GDN_WIDTH, GDN_WIDTH, GDN_WIDTH, GDN_HEADS, GDN_HEADS, GDN_WIDTH,
             NSA_WIDTH, 6 * NSA_KV_WIDTH, 3 * NSA_HEADS, 2 * D_MODEL]
    pts, acc = [], 0
    for sz in sizes[:-1]:
        acc += sz
        pts.append(acc)
    return pts


def _hybrid_mixer(h, cos, sin, w_in, conv_w, a_log, dt_bias, gdn_norm_w, gdn_w_up,
                  pos_k, k_w1, k_b1, k_w2, pos_v, v_w1, v_b1, v_w2, nsa_w_up, w_out):
    b, s, _ = h.shape
    proj = h @ w_in
    gq, gk, gv, gb, ga, gz, nq, nkv, ng, mg = jnp.split(proj, _split_points(), axis=-1)
    qkv = jax.nn.silu(_causal_conv(jnp.concatenate([gq, gk, gv], axis=-1), conv_w))
    q_a, k_a, v_a = jnp.split(qkv.astype(jnp.float32), 3, axis=-1)
    shp = (b, s, GDN_HEADS, GDN_HEAD_DIM)
    beta = jax.nn.sigmoid(gb.astype(jnp.float32))
    g_log = -jnp.exp(a_log.astype(jnp.float32)) * jax.nn.softplus(ga.astype(jnp.float32) + dt_bias.astype(jnp.float32))
    o_a = _gated_delta_rule(q_a.reshape(shp), k_a.reshape(shp), v_a.reshape(shp), g_log, beta)
    o_a = _rms_norm(o_a, gdn_norm_w) * jax.nn.silu(gz.astype(jnp.float32).reshape(shp))
    y_a = o_a.reshape(b, s, GDN_WIDTH).astype(h.dtype) @ gdn_w_up
    q_b = nq.reshape(b, s, NSA_HEADS, NSA_HEAD_DIM)
    kc, vc, ks, vs, kw, vw = [t.reshape(b, s, NSA_KV_HEADS, NSA_HEAD_DIM) for t in jnp.split(nkv, 6, axis=-1)]
    qr = _partial_rope(q_b, cos, sin)
    ks = _partial_rope(ks, cos, sin)
    kw = _partial_rope(kw, cos, sin)
    kcmp = _compress(kc, pos_k, k_w1, k_b1, k_w2)
    vcmp = _compress(vc, pos_v, v_w1, v_b1, v_w2)
    gates = jax.nn.sigmoid(ng).reshape(b, s, NSA_KV_HEADS, NSA_HEADS // NSA_KV_HEADS, 3)
    y_b = _nsa(q_b, qr, kcmp, vcmp, ks, vs, kw, vw, gates) @ nsa_w_up
    m_a, m_b = jnp.split(jax.nn.sigmoid(mg), 2, axis=-1)
    return (m_a * y_a + m_b * y_b) @ w_out


def setup_inputs(seed: int = 0) -> dict:
    key = jax.random.key(seed)
    keys = iter(jax.random.split(key, 48))

    def nrm(shape, scale):
        return jax.random.normal(next(keys), shape, jnp.float32) * scale

    def gain(shape):
        return 1.0 + nrm(shape, 0.02)

    L, D, hd = DEPTH, D_MODEL, NSA_HEAD_DIM
    dt = jnp.exp(jax.random.uniform(next(keys), (L, GDN_HEADS), jnp.float32, np.log(1e-3), np.log(1e-1)))
    return {
        "x": nrm((BATCH, SEQ, D), 1.0),
        "c": nrm((BATCH, D), 1.0),
        "ada_w": nrm((L, D, N_MOD * D), D ** -0.5),
        "ada_b": nrm((L, N_MOD * D), 0.02),
        "ffn1_norm": gain((L, D)),
        "ffn1_w_gate": nrm((L, D, D_FF), D ** -0.5),
        "ffn1_w_up": nrm((L, D, D_FF), D ** -0.5),
        "ffn1_w_down": nrm((L, D_FF, D), D_FF ** -0.5),
        "mix_norm": gain((L, D)),
        "w_in": nrm((L, D, N_IN), D ** -0.5),
        "gdn_conv_w": nrm((L, 3 * GDN_WIDTH, GDN_CONV), GDN_CONV ** -0.5),
        "gdn_a_log": jnp.log(jax.random.uniform(next(keys), (L, GDN_HEADS), jnp.float32, 1.0, 16.0)),
        "gdn_dt_bias": dt + jnp.log(-jnp.expm1(-dt)),
        "gdn_norm_w": gain((L, GDN_HEAD_DIM)),
        "gdn_w_up": nrm((L, GDN_WIDTH, D), GDN_WIDTH ** -0.5),
        "cmp_pos_k": nrm((L, CMP_BLOCK, hd), 0.02),
        "cmp_k_w1": nrm((L, CMP_BLOCK * hd, hd), (CMP_BLOCK * hd) ** -0.5),
        "cmp_k_b1": nrm((L, hd), 0.02),
        "cmp_k_w2": nrm((L, hd, hd), hd ** -0.5),
        "cmp_pos_v": nrm((L, CMP_BLOCK, hd), 0.02),
        "cmp_v_w1": nrm((L, CMP_BLOCK * hd, hd), (CMP_BLOCK * hd) ** -0.5),
        "cmp_v_b1": nrm((L, hd), 0.02),
        "cmp_v_w2": nrm((L, hd, hd), hd ** -0.5),
        "nsa_w_up": nrm((L, NSA_WIDTH, D), NSA_WIDTH ** -0.5),
        "w_out": nrm((L, D, D), D ** -0.5),
        "ffn2_norm": gain((L, D)),
        "ffn2_w_gate": nrm((L, D, D_FF), D ** -0.5),
        "ffn2_w_up": nrm((L, D, D_FF), D ** -0.5),
        "ffn2_w_down": nrm((L, D_FF, D), D_FF ** -0.5),
        "final_norm": gain((D,)),
    }


def reference(x, c, ada_w, ada_b, ffn1_norm, ffn1_w_gate, ffn1_w_up, ffn1_w_down, mix_norm, w_in,
              gdn_conv_w, gdn_a_log, gdn_dt_bias, gdn_norm_w, gdn_w_up, cmp_pos_k, cmp_k_w1, cmp_k_b1,
              cmp_k_w2, cmp_pos_v, cmp_v_w1, cmp_v_b1, cmp_v_w2, nsa_w_up, w_out, ffn2_norm,
              ffn2_w_gate, ffn2_w_up, ffn2_w_down, final_norm):
    b, s, d = x.shape
    pos = jnp.arange(s, dtype=jnp.float32)
    inv_freq = ROPE_THETA ** (-jnp.arange(0, ROPE_DIM, 2, dtype=jnp.float32) / ROPE_DIM)
    ang = pos[:, None] * inv_freq[None, :]
    cos, sin = jnp.cos(ang), jnp.sin(ang)
    c_act = jax.nn.silu(c)
    for l in range(DEPTH):
        mods = (c_act @ ada_w[l] + ada_b[l]).reshape(b, N_MOD, d)
        h = _modulate(x, ffn1_norm[l], mods[:, 0], mods[:, 1])
        x = x + 0.5 * mods[:, 2][:, None, :] * _swiglu(h, ffn1_w_gate[l], ffn1_w_up[l], ffn1_w_down[l])
        h = _modulate(x, mix_norm[l], mods[:, 3], mods[:, 4])
        y = _hybrid_mixer(h, cos, sin, w_in[l], gdn_conv_w[l], gdn_a_log[l], gdn_dt_bias[l], gdn_norm_w[l],
                          gdn_w_up[l], cmp_pos_k[l], cmp_k_w1[l], cmp_k_b1[l], cmp_k_w2[l], cmp_pos_v[l],
                          cmp_v_w1[l], cmp_v_b1[l], cmp_v_w2[l], nsa_w_up[l], w_out[l])
        x = x + mods[:, 5][:, None, :] * y
        h = _modulate(x, ffn2_norm[l], mods[:, 6], mods[:, 7])
        x = x + 0.5 * mods[:, 8][:, None, :] * _swiglu(h, ffn2_w_gate[l], ffn2_w_up[l], ffn2_w_down[l])
    return _rms_norm(x, final_norm)
```

```python
import numpy as np
from contextlib import ExitStack
import concourse.bass as bass
import concourse.mybir as mybir
from concourse.bass_utils import run_bass_kernel_spmd

F32 = mybir.dt.float32
BF16 = mybir.dt.bfloat16
ALU = mybir.AluOpType
AF = mybir.ActivationFunctionType
AX = mybir.AxisListType

D = 2048
DFF = 5632
S = 4096
NT = 1024
EPS = 1e-6
NCORE = 8


class T:
    __slots__ = ("ap", "w", "r", "name")

    def __init__(self, ap, name=""):
        self.ap = ap
        self.w = None
        self.r = {}
        self.name = name


class K:
    def __init__(self, nc, stack, n_dma_sems=40):
        self.nc = nc
        self.stack = stack
        self.engs = {}
        for name, h in (("pe", nc.tensor), ("act", nc.scalar), ("dve", nc.vector),
                        ("pool", nc.gpsimd), ("sp", nc.sync)):
            sem = stack.enter_context(nc.semaphore("s_" + name))
            self.engs[name] = dict(h=h, sem=sem, cnt=0, known={}, name=name)
        self.sems = {e["name"]: e["sem"] for e in self.engs.values()}
        self.dma_sems = []
        self.dma_pool = {"sw": [], "hw": []}
        for i in range(n_dma_sems):
            s = stack.enter_context(nc.semaphore("s_dma%d" % i))
            key = "dma%d" % i
            self.sems[key] = s
            d = dict(key=key, cnt=0)
            self.dma_sems.append(d)
            self.dma_pool["sw" if i < n_dma_sems // 2 else "hw"].append(d)
        self.dma_rr = {"sw": 0, "hw": 0}
        self.n_wait = 0
        self.n_inst = 0
        self._uid = 0

    def sb(self, shape, dtype, name=None):
        self._uid += 1
        return self.stack.enter_context(self.nc.sbuf_tensor(name or "sb%d" % self._uid, shape, dtype))

    def ps(self, shape, dtype, name=None):
        self._uid += 1
        return self.stack.enter_context(self.nc.psum_tensor(name or "ps%d" % self._uid, shape, dtype))

    def _wait(self, e, toks, skip_self=False):
        need = {}
        for tok in toks:
            if tok is None:
                continue
            kk, v = tok
            if need.get(kk, 0) < v:
                need[kk] = v
        for kk, v in need.items():
            if skip_self and kk == e["name"]:
                continue
            if e["known"].get(kk, 0) >= v:
                continue
            e["h"].wait_ge(self.sems[kk], v)
            e["known"][kk] = v
            self.n_wait += 1

    def op(self, eng, fn, reads=(), writes=(), pe_accum=False):
        e = self.engs[eng]
        toks = []
        for t in reads:
            toks.append(t.w)
        for t in writes:
            toks.append(t.w)
            for kk, v in t.r.items():
                toks.append((kk, v))
        self._wait(e, toks, skip_self=pe_accum)
        ins = fn(e["h"])
        e["cnt"] += 1
        ins.then_inc(e["sem"], 1)
        tok = (eng, e["cnt"])
        for t in reads:
            if t.r.get(eng, 0) < e["cnt"]:
                t.r[eng] = e["cnt"]
        for t in writes:
            t.w = tok
            t.r = {}
        self.n_inst += 1
        return ins

    def dma(self, eng, out_ap, in_ap, reads=(), writes=(), **kw):
        e = self.engs[eng]
        kind = "sw" if eng == "pool" else "hw"
        pool_ = self.dma_pool[kind]
        d = pool_[self.dma_rr[kind]]
        self.dma_rr[kind] = (self.dma_rr[kind] + 1) % len(pool_)
        toks = []
        for t in reads:
            toks.append(t.w)
        for t in writes:
            toks.append(t.w)
            toks.extend(t.r.items())
        if d["cnt"] > 0:
            toks.append((d["key"], d["cnt"] * 16))
        self._wait(e, toks)
        ins = e["h"].dma_start(out=out_ap, in_=in_ap, **kw)
        d["cnt"] += 1
        ins.then_inc(self.sems[d["key"]], 16)
        tok = (d["key"], d["cnt"] * 16)
        for t in reads:
            t.r[d["key"]] = d["cnt"] * 16
        for t in writes:
            t.w = tok
            t.r = {}
        self.n_inst += 1
        return tok

    def barrier(self):
        toks = [(e["name"], e["cnt"]) for e in self.engs.values() if e["cnt"] > 0]
        toks += [(d["key"], d["cnt"] * 16) for d in self.dma_sems if d["cnt"] > 0]
        for e in self.engs.values():
            self._wait(e, toks)

    def finish(self):
        toks = [(e["name"], e["cnt"]) for e in self.engs.values() if e["cnt"] > 0]
        toks += [(d["key"], d["cnt"] * 16) for d in self.dma_sems if d["cnt"] > 0]
        self._wait(self.engs["sp"], toks)


class PsumPool:
    def __init__(self, k, n=8, shape=(128, 512), dtype=F32):
        self.tiles = [T(k.ps(list(shape), dtype)[:]) for _ in range(n)]
        self.i = 0

    def get(self):
        t = self.tiles[self.i]
        self.i = (self.i + 1) % len(self.tiles)
        return t


class WStream:
    def __init__(self, k, nbuf, free_elems, depth=None, eng="pool"):
        self.k = k
        self.buf = [k.sb([128, free_elems], BF16) for _ in range(nbuf)]
        self.ts = [T(b[:]) for b in self.buf]
        self.jobs = []
        self.issued = 0
        self.depth = depth if depth is not None else nbuf - 2
        self.eng = eng

    def add(self, src_ap, inner):
        self.jobs.append((src_ap, inner))
        return len(self.jobs) - 1

    def get(self, i):
        upto = min(len(self.jobs), i + 1 + self.depth)
        while self.issued < upto:
            j = self.issued
            src, inner = self.jobs[j]
            t = self.ts[j % len(self.ts)]
            n = src.shape[1] * src.shape[2]
            dst = self.buf[j % len(self.ts)][:, 0:n].rearrange("p (a b) -> p a b", b=inner)
            self.k.dma(self.eng, dst, src, writes=[t])
            self.issued += 1
        t = self.ts[i % len(self.ts)]
        n_a = self.jobs[i][0].shape[1]
        inner = self.jobs[i][1]
        view = self.buf[i % len(self.ts)][:, 0:n_a * inner].rearrange("p (a b) -> p a b", b=inner)
        return t, view


def emit_norm_mod(k, pp, xres, ones_bf, sq_ts, rstd_t, tmp_ts, A_ap, B_ap, out_fn, nt=NT, post=None, extra_reads=()):
    nh = nt // 512
    pss = [pp.get() for _ in range(nh)]
    for kc in range(16):
        sq = sq_ts[kc % len(sq_ts)]
        k.op("act", lambda h: h.activation(sq.ap, xres[kc].ap, AF.Square), reads=[xres[kc]], writes=[sq])
        for th in range(nh):
            k.op("pe", lambda h: h.matmul(pss[th].ap, ones_bf.ap, sq.ap[:, th * 512:(th + 1) * 512],
                                          start=(kc == 0), stop=(kc == 15)),
                 reads=[ones_bf, sq], writes=[pss[th]], pe_accum=True)
    for th in range(nh):
        sl = slice(th * 512, (th + 1) * 512)
        k.op("dve", lambda h: h.tensor_scalar(rstd_t.ap[:, sl], pss[th].ap, 1.0 / D, EPS, ALU.mult, ALU.add),
             reads=[pss[th]], writes=[rstd_t])
    k.op("act", lambda h: h.activation(rstd_t.ap, rstd_t.ap, AF.Sqrt), reads=[rstd_t], writes=[rstd_t])
    k.op("dve", lambda h: h.reciprocal(rstd_t.ap, rstd_t.ap), reads=[rstd_t], writes=[rstd_t])
    for kc in range(16):
        tmp = tmp_ts[kc % len(tmp_ts)]
        k.op("dve", lambda h: h.tensor_tensor(tmp.ap, xres[kc].ap, rstd_t.ap, ALU.mult),
             reads=[xres[kc], rstd_t], writes=[tmp])
        ot, oap = out_fn(kc)
        if ot is None:
            ot, oap = tmp, tmp.ap
        k.op("act", lambda h: h.activation(oap, tmp.ap, AF.Identity, bias=B_ap[:, kc:kc + 1], scale=A_ap[:, kc:kc + 1]),
             reads=[tmp] + list(extra_reads), writes=[ot])
        if post is not None:
            post(kc, ot)


def emit_ffn(k, pp, ws, xres, hT, aT, sil_ts, wg, wu, wd, hg_ap, hg_t, nt=NT):
    nh = nt // 512
    wgv = wg.rearrange("(kc p) n -> p kc n", p=128)
    wuv = wu.rearrange("(kc p) n -> p kc n", p=128)
    wdv = wd.rearrange("(jc p) f -> p jc f", p=128)
    NJ = DFF // 128
    HJ = NJ // 2
    jobs = {}
    for hh in range(2):
        for jj in range(HJ):
            j = hh * HJ + jj
            jobs[("g", j)] = ws.add(wgv[:, :, j * 128:(j + 1) * 128], 128)
            jobs[("u", j)] = ws.add(wuv[:, :, j * 128:(j + 1) * 128], 128)
        for fc in range(16):
            jobs[("d", hh, fc)] = ws.add(wdv[:, hh * HJ:(hh + 1) * HJ, fc * 128:(fc + 1) * 128], 128)
    for hh in range(2):
        for jj in range(HJ):
            j = hh * HJ + jj
            tg, vg = ws.get(jobs[("g", j)])
            tu, vu = ws.get(jobs[("u", j)])
            for th in range(nh):
                sl = slice(th * 512, (th + 1) * 512)
                pg = pp.get()
                pu = pp.get()
                for kc in range(16):
                    k.op("pe", lambda h: h.matmul(pg.ap, vg[:, kc, :], hT[kc].ap[:, sl], start=(kc == 0), stop=(kc == 15)),
                         reads=[tg, hT[kc]], writes=[pg], pe_accum=True)
                for kc in range(16):
                    k.op("pe", lambda h: h.matmul(pu.ap, vu[:, kc, :], hT[kc].ap[:, sl], start=(kc == 0), stop=(kc == 15)),
                         reads=[tu, hT[kc]], writes=[pu], pe_accum=True)
                st = sil_ts[(jj * nh + th) % len(sil_ts)]
                k.op("act", lambda h: h.activation(st.ap, pg.ap, AF.Silu), reads=[pg], writes=[st])
                k.op("dve", lambda h: h.tensor_tensor(aT[jj].ap[:, sl], st.ap, pu.ap, ALU.mult),
                     reads=[st, pu], writes=[aT[jj]])
        for fc in range(16):
            td, vd = ws.get(jobs[("d", hh, fc)])
            for th in range(nh):
                sl = slice(th * 512, (th + 1) * 512)
                py = pp.get()
                for jj in range(HJ):
                    k.op("pe", lambda h: h.matmul(py.ap, vd[:, jj, :], aT[jj].ap[:, sl], start=(jj == 0), stop=(jj == HJ - 1)),
                         reads=[td, aT[jj]], writes=[py], pe_accum=True)
                k.op("dve", lambda h: h.scalar_tensor_tensor(xres[fc].ap[:, sl], py.ap, hg_ap[:, fc:fc + 1],
                                                             xres[fc].ap[:, sl], ALU.mult, ALU.add),
                     reads=[py, hg_t, xres[fc]], writes=[xres[fc]])


def build_l1():
    nc = bass.Bass("TRN2", target_bir_lowering=False)
    dt = lambda n, s, kind: nc.dram_tensor(n, s, F32, kind=kind).ap()
    xT = dt("xT", [D, NT], "ExternalInput")
    cb = dt("cb", [128, 16], "ExternalInput")
    ada_w = dt("ada_w", [D, 9 * D], "ExternalInput")
    ada_b = dt("ada_b", [128, 144], "ExternalInput")
    n1 = dt("n1", [128, 16], "ExternalInput")
    n2 = dt("n2", [128, 16], "ExternalInput")
    wg = dt("wg", [D, DFF], "ExternalInput")
    wu = dt("wu", [D, DFF], "ExternalInput")
    wd = dt("wd", [DFF, D], "ExternalInput")
    x1T = dt("x1T", [D, NT], "ExternalOutput")
    h2T = dt("h2T", [D, NT], "ExternalOutput")
    modT = dt("modT", [128, 144], "ExternalOutput")
    with ExitStack() as st:
        k = K(nc, st)
        pp = PsumPool(k, 7)
        mps = T(k.ps([128, 144], F32)[:])
        xres_b = k.sb([128, 16, NT], F32)
        xres = [T(xres_b[:, i, :]) for i in range(16)]
        hT_b = k.sb([128, 16, NT], BF16)
        hT = [T(hT_b[:, i, :]) for i in range(16)]
        aT_b = k.sb([128, 22, NT], BF16)
        aT = [T(aT_b[:, i, :]) for i in range(22)]
        ws = WStream(k, 5, 22 * 128)
        small = k.sb([128, 512], F32)
        cact = T(small[:, 0:16]); modS = T(small[:, 16:160]); adab = T(small[:, 160:304])
        n1t = T(small[:, 304:320]); n2t = T(small[:, 320:336])
        A1 = T(small[:, 336:352]); hg1 = T(small[:, 352:368]); A2 = T(small[:, 368:384])
        ones_b = k.sb([128, 128], BF16); ones_bf = T(ones_b[:])
        rstd = T(k.sb([128, NT], F32)[:])
        sq_ts = [T(k.sb([128, NT], BF16)[:]) for _ in range(2)]
        tmp_ts = [T(k.sb([128, NT], F32)[:]) for _ in range(2)]
        sil_ts = [T(k.sb([128, 512], BF16)[:]) for _ in range(2)]
        adw = [k.sb([128, 16, 128], F32) for _ in range(2)]
        adw_t = [T(a[:]) for a in adw]

        k.op("dve", lambda h: h.memset(ones_bf.ap, 1.0), writes=[ones_bf])
        k.dma("sp", cact.ap, cb, writes=[cact])
        k.dma("sp", adab.ap, ada_b, writes=[adab])
        k.dma("sp", n1t.ap, n1, writes=[n1t])
        k.dma("sp", n2t.ap, n2, writes=[n2t])
        for kc in range(16):
            k.dma("act", xres[kc].ap, xT[kc * 128:(kc + 1) * 128, :], writes=[xres[kc]])
        k.op("act", lambda h: h.activation(cact.ap, cact.ap, AF.Silu), reads=[cact], writes=[cact])
        adv = ada_w.rearrange("(kc p) n -> p kc n", p=128)
        for g in range(144):
            wt = adw_t[g % 2]
            k.dma("sp", wt.ap, adv[:, :, g * 128:(g + 1) * 128], writes=[wt])
            for kc in range(16):
                k.op("pe", lambda h: h.matmul(mps.ap[:, g:g + 1], adw[g % 2][:, kc, :],
                                              cact.ap[:, kc:kc + 1], start=(kc == 0), stop=(kc == 15)),
                     reads=[wt, cact], writes=[mps], pe_accum=True)
        k.op("dve", lambda h: h.tensor_tensor(modS.ap, mps.ap, adab.ap, ALU.add), reads=[mps, adab], writes=[modS])
        tmod = T(modT)
        k.dma("sp", modT, modS.ap, reads=[modS], writes=[tmod])
        m = lambda j: modS.ap[:, j * 16:(j + 1) * 16]
        k.op("dve", lambda h: h.scalar_tensor_tensor(A1.ap, m(1), 1.0, n1t.ap, ALU.add, ALU.mult), reads=[modS, n1t], writes=[A1])
        k.op("dve", lambda h: h.tensor_scalar(hg1.ap, m(2), 0.5, None, ALU.mult), reads=[modS], writes=[hg1])
        k.op("dve", lambda h: h.scalar_tensor_tensor(A2.ap, m(4), 1.0, n2t.ap, ALU.add, ALU.mult), reads=[modS, n2t], writes=[A2])
        emit_norm_mod(k, pp, xres, ones_bf, sq_ts, rstd, tmp_ts, A1.ap, m(0), lambda kc: (hT[kc], hT[kc].ap))
        emit_ffn(k, pp, ws, xres, hT, aT, sil_ts, wg, wu, wd, hg1.ap, hg1)
        tx1 = T(x1T)
        for kc in range(16):
            k.dma("sp", x1T[kc * 128:(kc + 1) * 128, :], xres[kc].ap, reads=[xres[kc]], writes=[tx1])
        th2 = T(h2T)
        nh = NT // 512
        pss = [pp.get() for _ in range(nh)]
        for kc in range(16):
            sq = sq_ts[kc % 2]
            k.op("act", lambda h: h.activation(sq.ap, xres[kc].ap, AF.Square), reads=[xres[kc]], writes=[sq])
            for t_ in range(nh):
                k.op("pe", lambda h: h.matmul(pss[t_].ap, ones_bf.ap, sq.ap[:, t_ * 512:(t_ + 1) * 512],
                                              start=(kc == 0), stop=(kc == 15)),
                     reads=[ones_bf, sq], writes=[pss[t_]], pe_accum=True)
        for t_ in range(nh):
            sl = slice(t_ * 512, (t_ + 1) * 512)
            k.op("dve", lambda h: h.tensor_scalar(rstd.ap[:, sl], pss[t_].ap, 1.0 / D, EPS, ALU.mult, ALU.add),
                 reads=[pss[t_]], writes=[rstd])
        k.op("act", lambda h: h.activation(rstd.ap, rstd.ap, AF.Sqrt), reads=[rstd], writes=[rstd])
        k.op("dve", lambda h: h.reciprocal(rstd.ap, rstd.ap), reads=[rstd], writes=[rstd])
        for kc in range(16):
            tmp = tmp_ts[kc % 2]
            k.op("dve", lambda h: h.tensor_tensor(tmp.ap, xres[kc].ap, rstd.ap, ALU.mult), reads=[xres[kc], rstd], writes=[tmp])
            o = tmp
            k.op("act", lambda h: h.activation(o.ap, tmp.ap, AF.Identity, bias=m(3)[:, kc:kc + 1], scale=A2.ap[:, kc:kc + 1]),
                 reads=[tmp, modS, A2], writes=[o])
            k.dma("sp", h2T[kc * 128:(kc + 1) * 128, :], o.ap, reads=[o], writes=[th2])
        k.finish()
        print("L1 inst", k.n_inst, "waits", k.n_wait)
    return nc


def build_l3():
    nc = bass.Bass("TRN2", target_bir_lowering=False)
    dt = lambda n, s, kind="ExternalInput": nc.dram_tensor(n, s, F32, kind=kind).ap()
    x1T = dt("x1T", [D, NT]); h2T = dt("h2T", [D, NT])
    oaT = dt("oaT", [1024, NT]); obT = dt("obT", [1024, NT])
    modT = dt("modT", [128, 144]); n3 = dt("n3", [128, 16]); nf = dt("nf", [128, 16])
    wmg = dt("wmg", [D, 2 * D]); gup = dt("gup", [1024, D]); nup = dt("nup", [1024, D]); wo = dt("wo", [D, D])
    wg = dt("wg", [D, DFF]); wu = dt("wu", [D, DFF]); wd = dt("wd", [DFF, D])
    outT = dt("outT", [D, NT], "ExternalOutput")
    with ExitStack() as st:
        k = K(nc, st)
        pp = PsumPool(k, 8)
        xres_b = k.sb([128, 16, NT], F32)
        xres = [T(xres_b[:, i, :]) for i in range(16)]
        hT_b = k.sb([128, 16, NT], BF16)
        hT = [T(hT_b[:, i, :]) for i in range(16)]
        aT_b = k.sb([128, 22, NT], BF16)
        aT = [T(aT_b[:, i, :]) for i in range(22)]
        ws = WStream(k, 5, 22 * 128)
        small = k.sb([128, 512], F32)
        modS = T(small[:, 0:144]); n3t = T(small[:, 144:160]); nft = T(small[:, 160:176])
        A3 = T(small[:, 176:192]); hg3 = T(small[:, 192:208]); zer = T(small[:, 208:224])
        ones_b = k.sb([128, 128], BF16); ones_bf = T(ones_b[:])
        rstd = T(k.sb([128, NT], F32)[:])
        sq_ts = [T(k.sb([128, NT], BF16)[:]) for _ in range(2)]
        tmp_ts = [T(k.sb([128, NT], F32)[:]) for _ in range(2)]
        sil_ts = [T(k.sb([128, 512], BF16)[:]) for _ in range(2)]
        sg_ts = [T(k.sb([128, 512], F32)[:]) for _ in range(2)]
        m = lambda j: modS.ap[:, j * 16:(j + 1) * 16]

        k.op("dve", lambda h: h.memset(ones_bf.ap, 1.0), writes=[ones_bf])
        k.op("dve", lambda h: h.memset(zer.ap, 0.0), writes=[zer])
        k.dma("sp", modS.ap, modT, writes=[modS])
        k.dma("sp", n3t.ap, n3, writes=[n3t])
        k.dma("sp", nft.ap, nf, writes=[nft])
        for kc in range(16):
            k.dma("pool", hT[kc].ap, h2T[kc * 128:(kc + 1) * 128, :], writes=[hT[kc]])
        for kc in range(16):
            k.dma("act", xres[kc].ap, x1T[kc * 128:(kc + 1) * 128, :], writes=[xres[kc]])
        k.op("dve", lambda h: h.scalar_tensor_tensor(A3.ap, m(7), 1.0, n3t.ap, ALU.add, ALU.mult), reads=[modS, n3t], writes=[A3])
        k.op("dve", lambda h: h.tensor_scalar(hg3.ap, m(8), 0.5, None, ALU.mult), reads=[modS], writes=[hg3])
        wmv = wmg.rearrange("(kc p) n -> p kc n", p=128)
        guv = gup.rearrange("(kc p) n -> p kc n", p=128)
        nuv = nup.rearrange("(kc p) n -> p kc n", p=128)
        wov = wo.rearrange("(kc p) n -> p kc n", p=128)
        oav = oaT.rearrange("(c p) t -> p c t", p=128)
        obv = obT.rearrange("(c p) t -> p c t", p=128)
        mer_t = [T(aT_b[:, i // 2, (i % 2) * 512:(i % 2 + 1) * 512]) for i in range(16)]
        oa_t = [T(aT_b[:, 8 + i // 2, (i % 2) * 512:(i % 2 + 1) * 512]) for i in range(8)]
        ob_t = [T(aT_b[:, 12 + i // 2, (i % 2) * 512:(i % 2 + 1) * 512]) for i in range(8)]
        jobs = {}
        for th in range(2):
            for fc in range(16):
                cs = slice(fc * 128, (fc + 1) * 128)
                jobs[("ma", th, fc)] = ws.add(wmv[:, :, cs], 128)
                jobs[("ga", th, fc)] = ws.add(guv[:, :, cs], 128)
                jobs[("mb", th, fc)] = ws.add(wmv[:, :, D + fc * 128:D + (fc + 1) * 128], 128)
                jobs[("gb", th, fc)] = ws.add(nuv[:, :, cs], 128)
            for fc in range(16):
                jobs[("wo", th, fc)] = ws.add(wov[:, :, fc * 128:(fc + 1) * 128], 128)
        for th in range(2):
            sl = slice(th * 512, (th + 1) * 512)
            for c in range(8):
                k.dma("pool", oa_t[c].ap, oav[:, c, sl], writes=[oa_t[c]])
                k.dma("pool", ob_t[c].ap, obv[:, c, sl], writes=[ob_t[c]])
            for fc in range(16):
                parts = []
                for nm_m, nm_g, src in (("ma", "ga", oa_t), ("mb", "gb", ob_t)):
                    tm, vm = ws.get(jobs[(nm_m, th, fc)])
                    tg, vg = ws.get(jobs[(nm_g, th, fc)])
                    pm = pp.get(); py = pp.get()
                    for kc in range(16):
                        k.op("pe", lambda h: h.matmul(pm.ap, vm[:, kc, :], hT[kc].ap[:, sl], start=(kc == 0), stop=(kc == 15)),
                             reads=[tm, hT[kc]], writes=[pm], pe_accum=True)
                    for c in range(8):
                        k.op("pe", lambda h: h.matmul(py.ap, vg[:, c, :], src[c].ap, start=(c == 0), stop=(c == 7)),
                             reads=[tg, src[c]], writes=[py], pe_accum=True)
                    sg = sg_ts[len(parts)]
                    k.op("act", lambda h: h.activation(sg.ap, pm.ap, AF.Sigmoid), reads=[pm], writes=[sg])
                    k.op("dve", lambda h: h.tensor_tensor(sg.ap, sg.ap, py.ap, ALU.mult), reads=[sg, py], writes=[sg])
                    parts.append(sg)
                k.op("pool", lambda h: h.tensor_tensor(mer_t[fc].ap, parts[0].ap, parts[1].ap, ALU.add),
                     reads=parts, writes=[mer_t[fc]])
            for fc2 in range(16):
                tw, vw = ws.get(jobs[("wo", th, fc2)])
                pz = pp.get()
                for fc in range(16):
                    k.op("pe", lambda h: h.matmul(pz.ap, vw[:, fc, :], mer_t[fc].ap, start=(fc == 0), stop=(fc == 15)),
                         reads=[tw, mer_t[fc]], writes=[pz], pe_accum=True)
                k.op("dve", lambda h: h.scalar_tensor_tensor(xres[fc2].ap[:, sl], pz.ap, m(5)[:, fc2:fc2 + 1],
                                                             xres[fc2].ap[:, sl], ALU.mult, ALU.add),
                     reads=[pz, modS, xres[fc2]], writes=[xres[fc2]])
        k.barrier()
        emit_norm_mod(k, pp, xres, ones_bf, sq_ts, rstd, tmp_ts, A3.ap, m(6), lambda kc: (hT[kc], hT[kc].ap),
                      extra_reads=[A3, modS])
        emit_ffn(k, pp, ws, xres, hT, aT, sil_ts, wg, wu, wd, hg3.ap, hg3)
        tout = T(outT)

        def post(kc, t):
            k.dma("sp", outT[kc * 128:(kc + 1) * 128, :], t.ap, reads=[t], writes=[tout])
        emit_norm_mod(k, pp, xres, ones_bf, sq_ts, rstd, tmp_ts, nft.ap, zer.ap, lambda kc: (None, None),
                      post=post, extra_reads=[nft, zer])
        k.finish()
        print("L3 inst", k.n_inst, "waits", k.n_wait)
    return nc


def run_l3(inp, x1T_l, h2T_l, modT_l, oaT_l, obT_l):
    nc = build_l3()
    wmg = np.ascontiguousarray(inp["w_in"][0][:, -2 * D:])
    maps = []
    for c in range(NCORE):
        maps.append({
            "x1T": x1T_l[c], "h2T": h2T_l[c], "oaT": oaT_l[c], "obT": obT_l[c], "modT": modT_l[c],
            "n3": _pc(inp["ffn2_norm"][0]), "nf": _pc(inp["final_norm"]),
            "wmg": wmg, "gup": inp["gdn_w_up"][0], "nup": inp["nsa_w_up"][0], "wo": inp["w_out"][0],
            "wg": inp["ffn2_w_gate"][0], "wu": inp["ffn2_w_up"][0], "wd": inp["ffn2_w_down"][0],
        })
    return run_bass_kernel_spmd(nc, maps, core_ids=list(range(NCORE)))

GC = 64
NCH = S // GC


def build_l2(do_nsa=True, do_gdn=True, hp_static=0, stage=99, heads=(0, 1)):
    nc = bass.Bass("TRN2", target_bir_lowering=False)
    dt = lambda n, s, kind="ExternalInput": nc.dram_tensor(n, s, F32, kind=kind).ap()
    h2T = dt("h2T", [D, S])
    wfm = dt("wfm", [D, 16 * 128])
    wtm = dt("wtm", [D, 272])
    convw = dt("convw", [128, 6, 4])
    alog = dt("alog", [128, 2]); dtb = dt("dtb", [128, 2])
    gnw = dt("gnw", [128, 128])
    ident_d = dt("ident", [128, 128]); tri_d = dt("tri", [64, 64])
    ms_d = dt("ms", [64, 64]); mc_d = dt("mc", [64, 64])
    oa = dt("oa", [S, 256], "ExternalOutput")
    ob = dt("ob", [S, 256], "ExternalOutput")
    nsa_in = (dt("cosT", [128, S]), dt("sinT", [128, S]), dt("rT", [128, 128]),
              dt("w1k", [4096, 128]), dt("w1v", [4096, 128]), dt("b1k", [128, 1]), dt("b1v", [128, 1]),
              dt("w2k", [128, 128]), dt("w2v", [128, 128]), dt("posk", [128, 32]), dt("posv", [128, 32]),
              dt("gsel", [256, 64]), dt("cmask", [256, S]), dt("bmask", [S, 64]), dt("ex", [64, S]),
              dt("caus", [128, 4, 512]), dt("band", [128, 8, 512]))
    with ExitStack() as st:
        k = K(nc, st)
        cs = k.sb([128, 1024], F32)
        ident = T(cs[:, 0:128]); tri = T(cs[0:64, 128:192]); ms = T(cs[0:64, 192:256]); mc = T(cs[0:64, 256:320])
        ones = T(cs[:, 320:448]); cw = T(cs[:, 448:472]); alg = T(cs[:, 472:474]); dtbt = T(cs[:, 474:476])
        gnwt = T(cs[:, 512:640])
        k.dma("sp", ident.ap, ident_d, writes=[ident]); k.dma("sp", tri.ap, tri_d, writes=[tri])
        k.dma("sp", ms.ap, ms_d, writes=[ms]); k.dma("sp", mc.ap, mc_d, writes=[mc])
        k.dma("sp", cw.ap, convw.rearrange("p a b -> p (a b)"), writes=[cw])
        k.dma("sp", alg.ap, alog, writes=[alg]); k.dma("sp", dtbt.ap, dtb, writes=[dtbt])
        k.dma("sp", gnwt.ap, gnw, writes=[gnwt])
        k.op("dve", lambda h: h.memset(ones.ap, 1.0), writes=[ones])
        k.op("act", lambda h: h.activation(alg.ap, alg.ap, AF.Exp), reads=[alg], writes=[alg])
        k.op("dve", lambda h: h.tensor_scalar(alg.ap, alg.ap, -1.0, None, ALU.mult), reads=[alg], writes=[alg])
        toa = T(oa)
        h2v = h2T.rearrange("(kc p) t -> p kc t", p=128)
        wfv = wfm.rearrange("(kc p) n -> p kc n", p=128)
        wtv = wtm.rearrange("(kc p) n -> p kc n", p=128)
        with ExitStack() as pst:
            k.stack = pst
            pp = PsumPool(k, 8)
            for hl in (heads if do_gdn else ()):
                with ExitStack() as ph:
                    k.stack = ph
                    emit_gdn_head(k, pp, hl, h2v, wfv, wtv, ident, tri, ms, mc, ones, cw, alg, dtbt, gnwt, oa, toa, stage)
                    k.barrier()
                k.stack = pst
            k.barrier()
        k.stack = st
        if do_nsa:
            with ExitStack() as ph:
                k.stack = ph
                emit_nsa(k, hp_static, h2v, wfv, wtv, ident, ones, ob, nsa_in)
                k.barrier()
            k.stack = st
        k.finish()
        print("L2 inst", k.n_inst, "waits", k.n_wait)
    return nc


def emit_gdn_head(k, pp, hl, h2v, wfv, wtv, ident, tri, ms, mc, ones, cw, alg, dtbt, gnwt, oa, toa, stage=99, fz=None):
    qT_b = k.sb([128, S], F32); kT_b = k.sb([128, S], F32)
    qT = [T(qT_b[:, b * 512:(b + 1) * 512]) for b in range(8)]
    kT = [T(kT_b[:, b * 512:(b + 1) * 512]) for b in range(8)]
    gz0 = 0 if fz is None else fz["c_first"]
    ktm_b = k.sb([64, NCH, 128], F32); vtm_b = k.sb([64, NCH, 128], F32); gz_b = k.sb([64, NCH - gz0, 128], F32)
    ktm = [T(ktm_b[:, c, :]) for c in range(NCH)]
    vtm = [T(vtm_b[:, c, :]) for c in range(NCH)]
    gz = [None] * gz0 + [T(gz_b[:, c, :]) for c in range(NCH - gz0)]
    gba_b = k.sb([64, 2, NCH], F32); gba = T(gba_b[:])
    hb_b = [k.sb([128, 16, 512], BF16) for _ in range(1)]
    hb = [T(b[:]) for b in hb_b]
    wq_b = k.sb([128, 3, 16, 128], BF16); wq = T(wq_b[:])
    wt_b = k.sb([128, 16, 130], BF16); wt = T(wt_b[:])
    cst_b = [k.sb([128, 3 + 512], F32) for _ in range(3)]
    cst = [T(b[:]) for b in cst_b]
    acc_ts = [T(k.sb([128, 512], F32)[:]) for _ in range(2)]
    sil_ts = [T(k.sb([128, 512], F32)[:]) for _ in range(2)]
    sq_t = T(k.sb([128, 512], F32)[:])
    rn_t = T(k.sb([128, 512], F32)[:])
    if fz is None:
        chs = (hl, 2 + hl, 4 + hl)
        for i, ch in enumerate(chs):
            k.dma("pool", wq_b[:, i, :, :], wfv[:, :, ch * 128:(ch + 1) * 128], writes=[wq])
        k.dma("pool", wt_b[:, :, :], wtv[:, :, hl * 130:(hl + 1) * 130], writes=[wt])
        sc_col = hl
    else:
        hd = fz["hd"]
        chs = (hd, 8 + hd, 16 + hd)
        for i in range(3):
            k.dma("pool", wq_b[:, i, :, :], fz["w_in_v"][:, :, i * 1024 + hd * 128:i * 1024 + (hd + 1) * 128], writes=[wq])
        k.dma("pool", wt_b[:, :, 0:128], fz["w_in_v"][:, :, 3088 + hd * 128:3088 + (hd + 1) * 128], writes=[wt])
        k.dma("pool", wt_b[:, :, 128:130], fz["wba_v"][:, :, 2 * hd:2 * hd + 2], writes=[wt])
        sc_col = hd
        vb_t = [T(k.sb([128, 512], F32)[:]) for _ in range(2)]
        vtm_t = T(k.sb([64, NCH], F32)[:])
        k.dma("sp", vtm_t.ap, fz["valid_tm"], writes=[vtm_t])
        ofT_t = [T(k.sb([128, 64], F32)[:]) for _ in range(2)]
    for i in range(3):
        k.op("dve", lambda h: h.memset(cst_b[i][:, 0:3], 0.0), writes=[cst[i]])
    for tb in range(8 if stage > 0.15 else 0):
        hbt = hb[0]; hbb = hb_b[0]
        k.dma("pool", hbt.ap, h2v[:, :, tb * 512:(tb + 1) * 512], writes=[hbt])
        if fz is not None:
            k.dma("sp", vb_t[tb % 2].ap, fz["validT"][:, tb * 512:(tb + 1) * 512], writes=[vb_t[tb % 2]])
        for i in range(3):
            ch = chs[i]
            ps = pp.get()
            for kc in range(16):
                k.op("pe", lambda h: h.matmul(ps.ap, wq_b[:, i, kc, :], hbb[:, kc, :], start=(kc == 0), stop=(kc == 15)),
                     reads=[wq, hbt], writes=[ps], pe_accum=True)
            c_ = cst[i]; cb_ = cst_b[i]
            if tb > 0:
                k.op("act", lambda h: h.activation(cb_[:, 0:3], cb_[:, 512:515], AF.Copy), reads=[c_], writes=[c_])
            if fz is None:
                k.op("act", lambda h: h.activation(cb_[:, 3:515], ps.ap, AF.Copy), reads=[ps, c_], writes=[c_])
            else:
                k.op("dve", lambda h: h.tensor_tensor(cb_[:, 3:515], ps.ap, vb_t[tb % 2].ap, ALU.mult), reads=[ps, c_, vb_t[tb % 2]], writes=[c_])
            if stage < 0.25:
                continue
            acc = acc_ts[i % 2]
            k.op("dve", lambda h: h.tensor_scalar(acc.ap, cb_[:, 0:512], cw.ap[:, ch * 4:ch * 4 + 1], None, ALU.mult),
                 reads=[c_, cw], writes=[acc])
            for j in range(1, 4):
                k.op("dve", lambda h: h.scalar_tensor_tensor(acc.ap, cb_[:, j:j + 512], cw.ap[:, ch * 4 + j:ch * 4 + j + 1],
                                                             acc.ap, ALU.mult, ALU.add),
                     reads=[c_, cw, acc], writes=[acc])
            sl_ = sil_ts[i % 2]
            k.op("act", lambda h: h.activation(sl_.ap, acc.ap, AF.Silu), reads=[acc], writes=[sl_])
            if i < 2:
                k.op("dve", lambda h: h.tensor_tensor(sq_t.ap, sl_.ap, sl_.ap, ALU.mult), reads=[sl_], writes=[sq_t])
                pn = pp.get()
                k.op("pe", lambda h: h.matmul(pn.ap, ones.ap, sq_t.ap, start=True, stop=True), reads=[ones, sq_t], writes=[pn])
                k.op("dve", lambda h: h.tensor_scalar(rn_t.ap, pn.ap, EPS, None, ALU.add), reads=[pn], writes=[rn_t])
                k.op("act", lambda h: h.activation(rn_t.ap, rn_t.ap, AF.Sqrt), reads=[rn_t], writes=[rn_t])
                k.op("dve", lambda h: h.reciprocal(rn_t.ap, rn_t.ap), reads=[rn_t], writes=[rn_t])
                dstT = (qT, kT)[i][tb]
                if i == 0:
                    k.op("dve", lambda h: h.scalar_tensor_tensor(dstT.ap, sl_.ap, 128.0 ** -0.5, rn_t.ap, ALU.mult, ALU.mult),
                         reads=[sl_, rn_t], writes=[dstT])
                else:
                    k.op("dve", lambda h: h.tensor_tensor(dstT.ap, sl_.ap, rn_t.ap, ALU.mult), reads=[sl_, rn_t], writes=[dstT])
            if i >= 1 and stage > 0.35:
                src_t = kT[tb] if i == 1 else sl_
                dst_l = ktm if i == 1 else vtm
                for cc in range(8):
                    c = tb * 8 + cc
                    pt = pp.get()
                    k.op("pe", lambda h: h.transpose(pt.ap[0:64, 0:128], src_t.ap[:, cc * 64:(cc + 1) * 64], ident.ap),
                         reads=[src_t, ident], writes=[pt])
                    k.op("dve", lambda h: h.tensor_copy(dst_l[c].ap, pt.ap[0:64, 0:128]), reads=[pt], writes=[dst_l[c]])
        for cc in range(8 if stage > 0.45 else 0):
            c = tb * 8 + cc
            pz = pp.get()
            for kc in range(16):
                k.op("pe", lambda h: h.matmul(pz.ap[0:64, 0:130], hbb[:, kc, cc * 64:(cc + 1) * 64], wt_b[:, kc, :],
                                              start=(kc == 0), stop=(kc == 15)),
                     reads=[hbt, wt], writes=[pz], pe_accum=True)
            if gz[c] is not None:
                k.op("act", lambda h: h.activation(gz[c].ap, pz.ap[0:64, 0:128], AF.Silu), reads=[pz], writes=[gz[c]])
                k.op("dve", lambda h: h.tensor_tensor(gz[c].ap, gz[c].ap, gnwt.ap[0:64, :], ALU.mult), reads=[gz[c], gnwt], writes=[gz[c]])
                k.op("dve", lambda h: h.tensor_copy(gba_b[:, :, c], pz.ap[0:64, 128:130]), reads=[pz, gz[c]], writes=[gba])
            else:
                k.op("dve", lambda h: h.tensor_copy(gba_b[:, :, c], pz.ap[0:64, 128:130]), reads=[pz], writes=[gba])
    if stage <= 1:
        return
    sm = k.sb([128, 8, NCH], F32)
    beta = T(sm[0:64, 0, :]); g = T(sm[0:64, 1, :]); gcs = T(sm[0:64, 2, :]); egk = T(sm[0:64, 3, :])
    ekd = T(sm[0:64, 4, :]); nbeta = T(sm[0:64, 5, :]); dend = T(sm[:, 6, :]); bk = T(sm[0:64, 7, :])
    k.op("act", lambda h: h.activation(beta.ap, gba_b[:, 0, :], AF.Sigmoid), reads=[gba], writes=[beta])
    if fz is not None:
        k.op("dve", lambda h: h.tensor_tensor(beta.ap, beta.ap, vtm_t.ap, ALU.mult), reads=[beta, vtm_t], writes=[beta])
    k.op("act", lambda h: h.activation(g.ap, gba_b[:, 1, :], AF.Exp, bias=dtbt.ap[0:64, sc_col:sc_col + 1]), reads=[gba, dtbt], writes=[g])
    k.op("dve", lambda h: h.tensor_scalar(g.ap, g.ap, 1.0, None, ALU.add), reads=[g], writes=[g])
    k.op("act", lambda h: h.activation(g.ap, g.ap, AF.Ln), reads=[g], writes=[g])
    k.op("dve", lambda h: h.tensor_scalar(g.ap, g.ap, alg.ap[0:64, sc_col:sc_col + 1], None, ALU.mult), reads=[g, alg], writes=[g])
    pg = pp.get()
    k.op("pe", lambda h: h.matmul(pg.ap[0:64, 0:NCH], tri.ap, g.ap, start=True, stop=True), reads=[tri, g], writes=[pg])
    k.op("dve", lambda h: h.tensor_copy(gcs.ap, pg.ap[0:64, 0:NCH]), reads=[pg], writes=[gcs])
    pl = pp.get()
    k.op("pe", lambda h: h.matmul(pl.ap[:, 0:NCH], ones.ap[0:64, :], g.ap, start=True, stop=True), reads=[ones, g], writes=[pl])
    k.op("act", lambda h: h.activation(dend.ap, pl.ap[:, 0:NCH], AF.Exp), reads=[pl], writes=[dend])
    k.op("dve", lambda h: h.tensor_tensor(ekd.ap, pl.ap[0:64, 0:NCH], gcs.ap, ALU.subtract), reads=[pl, gcs, dend], writes=[ekd])
    k.op("act", lambda h: h.activation(ekd.ap, ekd.ap, AF.Exp), reads=[ekd], writes=[ekd])
    k.op("act", lambda h: h.activation(egk.ap, gcs.ap, AF.Exp), reads=[gcs], writes=[egk])
    k.op("dve", lambda h: h.tensor_tensor(bk.ap, beta.ap, egk.ap, ALU.mult), reads=[beta, egk], writes=[bk])
    k.op("dve", lambda h: h.tensor_scalar(nbeta.ap, beta.ap, -1.0, None, ALU.mult), reads=[beta], writes=[nbeta])
    if stage <= 2:
        return
    S_t = T(k.sb([128, 128], F32)[:])
    k.op("dve", lambda h: h.memset(S_t.ap, 0.0), writes=[S_t])
    NB = 2
    def mk(shape):
        return [T(k.sb(shape, F32)[:]) for _ in range(NB)]
    NPP = 6 if stage == 3.5 else 2
    diag = mk([64, 64]); ds = mk([64, 64]); dT = mk([64, 64]); Nm = [mk([64, 64]) for _ in range(NPP)]; Mm = [mk([64, 64]) for _ in range(NPP)]
    Pm = [mk([64, 64]) for _ in range(NPP)]; AT = mk([64, 64]); Vb = mk([64, 128]); Kb = mk([64, 128]); u = mk([64, 128]); wT = mk([128, 64])
    eg = mk([128, 64]); qg = mk([128, 64]); kd = mk([64, 128]); vn = mk([64, 128]); osb = mk([64, 128]); ss = mk([64, 1]); of = mk([64, 128])
    pre = {}

    def precompute(c):
        b = c % NB
        tb, off = c // 8, (c % 8) * 64
        kTc = kT[tb].ap[:, off:off + 64]; qTc = qT[tb].ap[:, off:off + 64]
        col = slice(c, c + 1)
        k.op("dve", lambda h: h.tensor_scalar(diag[b].ap, ident.ap[0:64, 0:64], gcs.ap[:, col], None, ALU.mult),
             reads=[ident, gcs], writes=[diag[b]])
        pb = pp.get()
        k.op("pe", lambda h: h.matmul(pb.ap[:, 0:64], ones.ap[0:64, :], diag[b].ap, start=True, stop=True),
             reads=[ones, diag[b]], writes=[pb])
        k.op("dve", lambda h: h.scalar_tensor_tensor(ds[b].ap, pb.ap[0:64, 0:64], gcs.ap[:, col], ms.ap, ALU.subtract, ALU.add),
             reads=[pb, gcs, ms], writes=[ds[b]])
        k.op("act", lambda h: h.activation(ds[b].ap, ds[b].ap, AF.Exp, scale=-1.0), reads=[ds[b]], writes=[ds[b]])
        k.op("dve", lambda h: h.scalar_tensor_tensor(dT[b].ap, pb.ap[0:64, 0:64], gcs.ap[:, col], mc.ap, ALU.subtract, ALU.add),
             reads=[pb, gcs, mc], writes=[dT[b]])
        k.op("act", lambda h: h.activation(dT[b].ap, dT[b].ap, AF.Exp), reads=[dT[b]], writes=[dT[b]])
        k.op("act", lambda h: h.activation(eg[b].ap, pb.ap[:, 0:64], AF.Exp), reads=[pb], writes=[eg[b]])
        k.op("dve", lambda h: h.tensor_tensor(qg[b].ap, qTc, eg[b].ap, ALU.mult), reads=[qT[tb], eg[b]], writes=[qg[b]])
        pk = pp.get()
        k.op("pe", lambda h: h.matmul(pk.ap[0:64, 0:64], kTc, kTc, start=True, stop=True), reads=[kT[tb]], writes=[pk])
        N0 = Nm[0][b]
        k.op("dve", lambda h: h.scalar_tensor_tensor(N0.ap, pk.ap[0:64, 0:64], nbeta.ap[:, col], ds[b].ap, ALU.mult, ALU.mult),
             reads=[pk, nbeta, ds[b]], writes=[N0])
        pm = pp.get()
        k.op("pe", lambda h: h.transpose(pm.ap[0:64, 0:64], N0.ap, ident.ap[0:64, 0:64]), reads=[N0, ident], writes=[pm])
        M0 = Mm[0][b]
        k.op("dve", lambda h: h.tensor_copy(M0.ap, pm.ap[0:64, 0:64]), reads=[pm], writes=[M0])
        P0 = Pm[0][b]
        k.op("dve", lambda h: h.tensor_tensor(P0.ap, pm.ap[0:64, 0:64], ident.ap[0:64, 0:64], ALU.add), reads=[pm, ident], writes=[P0])
        Ncur, Mcur, Pcur = N0, M0, P0
        for s_ in range(1, 6):
            Nn = Nm[s_ % NPP][b]; Mn = Mm[s_ % NPP][b]; Pn = Pm[s_ % NPP][b]
            pn_ = pp.get()
            k.op("pe", lambda h: h.matmul(pn_.ap[0:64, 0:64], Mcur.ap, Ncur.ap, start=True, stop=True), reads=[Mcur, Ncur], writes=[pn_])
            if s_ < 5:
                pm_ = pp.get()
                k.op("pe", lambda h: h.matmul(pm_.ap[0:64, 0:64], Ncur.ap, Mcur.ap, start=True, stop=True), reads=[Mcur, Ncur], writes=[pm_])
            k.op("act", lambda h: h.activation(Nn.ap, pn_.ap[0:64, 0:64], AF.Copy), reads=[pn_], writes=[Nn])
            if s_ < 5:
                k.op("dve", lambda h: h.tensor_copy(Mn.ap, pm_.ap[0:64, 0:64]), reads=[pm_], writes=[Mn])
            pq = pp.get()
            k.op("pe", lambda h: h.matmul(pq.ap[0:64, 0:64], Nn.ap, Pcur.ap, start=True, stop=True), reads=[Nn, Pcur], writes=[pq])
            k.op("dve", lambda h: h.tensor_tensor(Pn.ap, pq.ap[0:64, 0:64], Pcur.ap, ALU.add), reads=[pq, Pcur], writes=[Pn])
            Ncur, Mcur, Pcur = Nn, Mn, Pn
        TT = Pcur
        pa = pp.get()
        k.op("pe", lambda h: h.matmul(pa.ap[0:64, 0:64], kTc, qTc, start=True, stop=True), reads=[kT[tb], qT[tb]], writes=[pa])
        k.op("dve", lambda h: h.tensor_tensor(AT[b].ap, pa.ap[0:64, 0:64], dT[b].ap, ALU.mult), reads=[pa, dT[b]], writes=[AT[b]])
        k.op("dve", lambda h: h.tensor_scalar(Vb[b].ap, vtm[c].ap, beta.ap[:, col], None, ALU.mult), reads=[vtm[c], beta], writes=[Vb[b]])
        k.op("dve", lambda h: h.tensor_scalar(Kb[b].ap, ktm[c].ap, bk.ap[:, col], None, ALU.mult), reads=[ktm[c], bk], writes=[Kb[b]])
        k.op("dve", lambda h: h.tensor_scalar(kd[b].ap, ktm[c].ap, ekd.ap[:, col], None, ALU.mult), reads=[ktm[c], ekd], writes=[kd[b]])
        pu = pp.get()
        k.op("pe", lambda h: h.matmul(pu.ap[0:64, 0:128], TT.ap, Vb[b].ap, start=True, stop=True), reads=[TT, Vb[b]], writes=[pu])
        k.op("act", lambda h: h.activation(u[b].ap, pu.ap[0:64, 0:128], AF.Copy), reads=[pu], writes=[u[b]])
        pw = pp.get()
        k.op("pe", lambda h: h.matmul(pw.ap[:, 0:64], Kb[b].ap, TT.ap, start=True, stop=True), reads=[TT, Kb[b]], writes=[pw])
        k.op("act", lambda h: h.activation(wT[b].ap, pw.ap[:, 0:64], AF.Copy), reads=[pw], writes=[wT[b]])

    def scan(c):
        b = c % NB
        col = slice(c, c + 1)
        p1 = pp.get()
        k.op("pe", lambda h: h.matmul(p1.ap[0:64, 0:128], wT[b].ap, S_t.ap, start=True, stop=True), reads=[wT[b], S_t], writes=[p1])
        k.op("dve", lambda h: h.tensor_tensor(vn[b].ap, u[b].ap, p1.ap[0:64, 0:128], ALU.subtract), reads=[u[b], p1], writes=[vn[b]])
        want_out = (fz is None) or (c >= fz["c_first"])
        if want_out:
            p2 = pp.get()
            k.op("pe", lambda h: h.matmul(p2.ap[0:64, 0:128], qg[b].ap, S_t.ap, start=True, stop=False), reads=[qg[b], S_t], writes=[p2])
            k.op("pe", lambda h: h.matmul(p2.ap[0:64, 0:128], AT[b].ap, vn[b].ap, start=False, stop=True), reads=[AT[b], vn[b]], writes=[p2],
                 pe_accum=True)
        p3 = pp.get()
        k.op("pe", lambda h: h.matmul(p3.ap[:, 0:128], kd[b].ap, vn[b].ap, start=True, stop=True), reads=[kd[b], vn[b]], writes=[p3])
        k.op("dve", lambda h: h.scalar_tensor_tensor(S_t.ap, S_t.ap, dend.ap[:, col], p3.ap[:, 0:128], ALU.mult, ALU.add),
             reads=[S_t, dend, p3], writes=[S_t])
        if not want_out:
            return
        k.op("act", lambda h: h.activation(osb[b].ap, p2.ap[0:64, 0:128], AF.Square, accum_out=ss[b].ap), reads=[p2], writes=[osb[b], ss[b]])
        k.op("dve", lambda h: h.tensor_scalar(ss[b].ap, ss[b].ap, 1.0 / 128, EPS, ALU.mult, ALU.add), reads=[ss[b]], writes=[ss[b]])
        k.op("act", lambda h: h.activation(ss[b].ap, ss[b].ap, AF.Sqrt), reads=[ss[b]], writes=[ss[b]])
        k.op("dve", lambda h: h.reciprocal(ss[b].ap, ss[b].ap), reads=[ss[b]], writes=[ss[b]])
        k.op("dve", lambda h: h.scalar_tensor_tensor(of[b].ap, p2.ap[0:64, 0:128], ss[b].ap, gz[c].ap, ALU.mult, ALU.mult),
             reads=[p2, ss[b], gz[c]], writes=[of[b]])
        if fz is None:
            k.dma("sp", oa[c * 64:(c + 1) * 64, hl * 128:(hl + 1) * 128], of[b].ap, reads=[of[b]], writes=[toa])
        else:
            pT_ = pp.get()
            k.op("pe", lambda h: h.transpose(pT_.ap[:, 0:64], of[b].ap, ident.ap[0:64, 0:64]), reads=[of[b], ident], writes=[pT_])
            oT_ = ofT_t[b]
            k.op("dve", lambda h: h.tensor_copy(oT_.ap, pT_.ap[:, 0:64]), reads=[pT_], writes=[oT_])
            lc = c - fz["c_first"]
            k.dma("sp", fz["oaT"][fz["hd"] * 128:(fz["hd"] + 1) * 128, lc * 64:(lc + 1) * 64], oT_.ap, reads=[oT_], writes=[fz["toaT"]])

    precompute(0)
    if stage == 3.5:
        scan(0)
        dumps = [(0, 0, qT[0].ap[:, 0:256], [qT[0]]), (128, 0, kT[0].ap[:, 0:256], [kT[0]]),
                 (256, 0, ktm[0].ap, [ktm[0]]), (256, 128, vtm[0].ap, [vtm[0]]),
                 (320, 0, gz[0].ap, [gz[0]]), (320, 128, gba_b[:, 0, :], [gba]), (320, 192, gba_b[:, 1, :], [gba]),
                 (384, 0, beta.ap, [beta]), (384, 64, g.ap, [g]), (384, 128, gcs.ap, [gcs]), (384, 192, egk.ap, [egk]),
                 (448, 0, ekd.ap, [ekd]), (448, 64, nbeta.ap, [nbeta]), (448, 128, bk.ap, [bk]), (448, 192, dend.ap[0:64, :], [dend]),
                 (512, 0, ds[0].ap, [ds[0]]), (512, 64, dT[0].ap, [dT[0]]), (512, 128, Nm[0][0].ap, [Nm[0][0]]), (512, 192, Mm[0][0].ap, [Mm[0][0]]),
                 (576, 0, Pm[5][0].ap, [Pm[5][0]]), (576, 64, AT[0].ap, [AT[0]]), (576, 128, u[0].ap, [u[0]]),
                 (640, 0, wT[0].ap, [wT[0]]), (640, 64, qg[0].ap, [qg[0]]), (640, 128, eg[0].ap, [eg[0]]),
                 (768, 0, kd[0].ap, [kd[0]]), (768, 128, Vb[0].ap, [Vb[0]]),
                 (832, 0, vn[0].ap, [vn[0]]), (832, 128, of[0].ap, [of[0]]), (896, 0, S_t.ap, [S_t]), (896, 128, Kb[0].ap, [Kb[0]])]
        for i_ in range(6):
            dumps.append((1024 + 64 * i_, 0, Nm[i_][0].ap, [Nm[i_][0]]))
            dumps.append((1024 + 64 * i_, 128, Pm[i_][0].ap, [Pm[i_][0]]))
            if i_ < 5:
                dumps.append((1024 + 64 * i_, 64, Mm[i_][0].ap, [Mm[i_][0]]))
        for (r0, c0, ap_, ts_) in dumps:
            k.dma("sp", oa[r0 + 1024:r0 + 1024 + ap_.shape[0], c0:c0 + ap_.shape[1]], ap_, reads=ts_, writes=[toa])
        return
    if stage <= 3:
        return
    for c in range(NCH if stage > 4 else 2):
        if c + 1 < NCH:
            precompute(c + 1)
        scan(c)

def emit_nsa(k, hp, h2v, wfv, wtv, ident, ones, ob, nsa_in):
    (cosT, sinT, rT_d, w1k_d, w1v_d, b1k_d, b1v_d, w2k_d, w2v_d, posk_d, posv_d, gsel_d, cmask_d, bmask_d, ex_d, caus_d, band_d) = nsa_in
    SC = 128.0 ** -0.5
    r_own = (2 * (hp % 2), 2 * (hp % 2) + 1)
    pp = PsumPool(k, 4)
    po = [T(k.ps([128, 512], F32)[:]) for _ in range(4)]
    tob = T(ob)
    qT_b = k.sb([128, 4, S], BF16); qT = T(qT_b[:])
    qr_b = k.sb([128, 2, S], BF16); qr = T(qr_b[:])
    kc_b = k.sb([128, S], BF16); kcT = T(kc_b[:]); vc_b = k.sb([128, S], BF16); vcT = T(vc_b[:])
    ks_b = k.sb([128, S], BF16); ksT = T(ks_b[:]); kw_b = k.sb([128, S], BF16); kwT = T(kw_b[:])
    vs_b = k.sb([128, 32, 130], BF16); vs_tm = T(vs_b[:]); vw_b = k.sb([128, 32, 130], BF16); vw_tm = T(vw_b[:])
    gts_b = k.sb([128, 32, 12], F32); gts = T(gts_b[:])
    imp_b = k.sb([128, 32, 64], F32); imp = T(imp_b[:])
    oacc_b = k.sb([128, 32, 256], F32); oacc = T(oacc_b[:])
    selT_b = k.sb([64, S], BF16); selT = T(selT_b[:])
    k.op("dve", lambda h: h.memset(vs_b[:, :, 128:129], 1.0), writes=[vs_tm])
    k.op("dve", lambda h: h.memset(vw_b[:, :, 128:129], 1.0), writes=[vw_tm])
    k.op("dve", lambda h: h.memset(imp.ap, 0.0), writes=[imp])
    k.op("dve", lambda h: h.memset(oacc.ap, 0.0), writes=[oacc])
    with ExitStack() as ph:
        old = k.stack; k.stack = ph
        hb_b = k.sb([128, 16, 512], BF16); hb = T(hb_b[:])
        ws = WStream(k, 4, 16 * 128)
        wng_b = k.sb([128, 16, 12], BF16); wng = T(wng_b[:])
        k.dma("pool", wng_b[:], wtv[:, :, 260:272], writes=[wng])
        rT_b = k.sb([128, 128], BF16); rT = T(rT_b[:])
        k.dma("pool", rT.ap, rT_d, writes=[rT])
        cs_t = [T(k.sb([128, 512], F32)[:]) for _ in range(2)]
        sn_t = [T(k.sb([128, 512], F32)[:]) for _ in range(2)]
        xb_t = [T(k.sb([128, 512], BF16)[:]) for _ in range(2)]
        t1_t = [T(k.sb([128, 512], F32)[:]) for _ in range(2)]
        t2_t = [T(k.sb([128, 512], F32)[:]) for _ in range(2)]
        jobs = {}
        for tb in range(8):
            for ch in range(10):
                jobs[(tb, ch)] = ws.add(wfv[:, :, (6 + ch) * 128:(7 + ch) * 128], 128)
        nr = 0
        for tb in range(8):
            bs = slice(tb * 512, (tb + 1) * 512)
            k.dma("pool", hb.ap, h2v[:, :, bs], writes=[hb])
            k.dma("sp", cs_t[tb % 2].ap, cosT[:, bs], writes=[cs_t[tb % 2]])
            k.dma("sp", sn_t[tb % 2].ap, sinT[:, bs], writes=[sn_t[tb % 2]])
            for ch in range(10):
                tw, vw_ = ws.get(jobs[(tb, ch)])
                ps = pp.get()
                for kc in range(16):
                    k.op("pe", lambda h: h.matmul(ps.ap, vw_[:, kc, :], hb_b[:, kc, :], start=(kc == 0), stop=(kc == 15)),
                         reads=[tw, hb], writes=[ps], pe_accum=True)
                rope_dst = None
                if ch < 4:
                    k.op("act", lambda h: h.activation(qT_b[:, ch, bs], ps.ap, AF.Copy), reads=[ps], writes=[qT])
                    if ch in r_own:
                        rope_dst = (qr, qr_b[:, r_own.index(ch), bs])
                elif ch == 4:
                    k.op("act", lambda h: h.activation(kc_b[:, bs], ps.ap, AF.Copy), reads=[ps], writes=[kcT])
                elif ch == 5:
                    k.op("act", lambda h: h.activation(vc_b[:, bs], ps.ap, AF.Copy), reads=[ps], writes=[vcT])
                elif ch == 6:
                    rope_dst = (ksT, ks_b[:, bs])
                elif ch == 8:
                    rope_dst = (kwT, kw_b[:, bs])
                else:
                    dst_t, dst_b = (vs_tm, vs_b) if ch == 7 else (vw_tm, vw_b)
                    xf = t1_t[nr % 2]; nr += 1
                    k.op("act", lambda h: h.activation(xf.ap, ps.ap, AF.Copy), reads=[ps], writes=[xf])
                    for tt in range(4):
                        pt = pp.get()
                        k.op("pe", lambda h: h.transpose(pt.ap[:, 0:128], xf.ap[:, tt * 128:(tt + 1) * 128], ident.ap),
                             reads=[xf, ident], writes=[pt])
                        k.op("dve", lambda h: h.tensor_copy(dst_b[:, tb * 4 + tt, 0:128], pt.ap[:, 0:128]), reads=[pt], writes=[dst_t])
                if rope_dst is not None:
                    dt_, dap = rope_dst
                    xb = xb_t[nr % 2]; nr += 1
                    k.op("act", lambda h: h.activation(xb.ap, ps.ap, AF.Copy), reads=[ps], writes=[xb])
                    pr = pp.get()
                    k.op("pe", lambda h: h.matmul(pr.ap, rT.ap, xb.ap, start=True, stop=True), reads=[rT, xb], writes=[pr])
                    t1 = t1_t[nr % 2]; t2 = t2_t[nr % 2]
                    k.op("dve", lambda h: h.tensor_tensor(t1.ap, ps.ap, cs_t[tb % 2].ap, ALU.mult), reads=[ps, cs_t[tb % 2], xb], writes=[t1])
                    k.op("dve", lambda h: h.tensor_tensor(t2.ap, pr.ap, sn_t[tb % 2].ap, ALU.mult), reads=[pr, sn_t[tb % 2]], writes=[t2])
                    k.op("dve", lambda h: h.tensor_tensor(dap, t1.ap, t2.ap, ALU.add), reads=[t1, t2], writes=[dt_])
            for tt in range(4):
                pg = pp.get()
                for kc in range(16):
                    k.op("pe", lambda h: h.matmul(pg.ap[:, 0:12], hb_b[:, kc, tt * 128:(tt + 1) * 128], wng_b[:, kc, :],
                                                  start=(kc == 0), stop=(kc == 15)),
                         reads=[hb, wng], writes=[pg], pe_accum=True)
                k.op("act", lambda h: h.activation(gts_b[:, tb * 4 + tt, :], pg.ap[:, 0:12], AF.Sigmoid), reads=[pg], writes=[gts])
        k.barrier()
        k.stack = old
    kcmp_b = k.sb([128, 256], BF16); kcmp = T(kcmp_b[:])
    vca_b = k.sb([128, 2, 193], BF16); vca = T(vca_b[:])
    k.op("dve", lambda h: h.memset(kcmp.ap, 0.0), writes=[kcmp])
    k.op("dve", lambda h: h.memset(vca.ap, 0.0), writes=[vca])
    k.op("dve", lambda h: h.memset(vca_b[:, :, 128:129], 1.0), writes=[vca])
    k.dma("pool", vca_b[:, :, 129:193], gsel_d.rearrange("(a p) j -> p a j", p=128), writes=[vca])
    with ExitStack() as ph:
        old = k.stack; k.stack = ph
        w1_b = k.sb([128, 32, 128], BF16); w1 = T(w1_b[:])
        w2_b = k.sb([128, 128], BF16); w2 = T(w2_b[:])
        pos_b = k.sb([128, 32], BF16); pos = T(pos_b[:])
        b1_b = k.sb([128, 1], F32); b1 = T(b1_b[:])
        bias_b = k.sb([128, 1], F32); bias = T(bias_b[:])
        hid_b = k.sb([128, 256], BF16); hid = T(hid_b[:])
        for which in range(2):
            w1d, b1d, w2d, posd, src_b, src_t = ((w1k_d, b1k_d, w2k_d, posk_d, kc_b, kcT), (w1v_d, b1v_d, w2v_d, posv_d, vc_b, vcT))[which]
            k.dma("pool", w1.ap, w1d.rearrange("(j d) m -> d j m", d=128), writes=[w1])
            k.dma("pool", w2.ap, w2d, writes=[w2])
            k.dma("pool", pos.ap, posd, writes=[pos])
            k.dma("sp", b1.ap, b1d, writes=[b1])
            pb = pp.get()
            for j in range(32):
                k.op("pe", lambda h: h.matmul(pb.ap[:, 0:1], w1_b[:, j, :], pos_b[:, j:j + 1], start=(j == 0), stop=(j == 31)),
                     reads=[w1, pos], writes=[pb], pe_accum=True)
            k.op("dve", lambda h: h.tensor_tensor(bias.ap, pb.ap[:, 0:1], b1.ap, ALU.add), reads=[pb, b1], writes=[bias])
            ph_ = pp.get()
            for j in range(32):
                k.op("pe", lambda h: h.matmul(ph_.ap[:, 0:255], w1_b[:, j, :], src_b[:, j:j + 16 * 254 + 1:16], start=(j == 0), stop=(j == 31)),
                     reads=[w1, src_t], writes=[ph_], pe_accum=True)
            k.op("dve", lambda h: h.memset(hid.ap, 0.0), writes=[hid])
            k.op("act", lambda h: h.activation(hid_b[:, 0:255], ph_.ap[:, 0:255], AF.Silu, bias=bias.ap), reads=[ph_, bias], writes=[hid])
            if which == 0:
                pk = pp.get()
                k.op("pe", lambda h: h.matmul(pk.ap[:, 0:255], w2.ap, hid_b[:, 0:255], start=True, stop=True), reads=[w2, hid], writes=[pk])
                k.op("act", lambda h: h.activation(kcmp_b[:, 0:255], pk.ap[:, 0:255], AF.Copy), reads=[pk], writes=[kcmp])
            else:
                for nt_ in range(2):
                    pv = pp.get()
                    k.op("pe", lambda h: h.matmul(pv.ap[:, 0:128], hid_b[:, nt_ * 128:(nt_ + 1) * 128], w2.ap, start=True, stop=True),
                         reads=[w2, hid], writes=[pv])
                    k.op("act", lambda h: h.activation(vca_b[:, nt_, 0:128], pv.ap[:, 0:128], AF.Copy), reads=[pv], writes=[vca])
        k.barrier()
        k.stack = old
    e_t = [T(k.sb([128, 512], BF16)[:]) for _ in range(3)]
    p_t = [T(k.sb([128, 512], BF16)[:]) for _ in range(3)]
    mk_t = [T(k.sb([128, 512], BF16)[:]) for _ in range(2)]
    rd_t = [T(k.sb([128, 1], F32)[:]) for _ in range(2)]
    rg_t = [T(k.sb([128, 1], F32)[:]) for _ in range(2)]
    tmpo = [T(k.sb([128, 128], F32)[:]) for _ in range(2)]
    cnt = [0]

    def finish_sub(pacc, qt, r, gate_j, with_imp):
        i = cnt[0] % 2; cnt[0] += 1
        rd = rd_t[i]; rg = rg_t[i]
        k.op("dve", lambda h: h.tensor_scalar(rd.ap, pacc.ap[:, 128:129], 1e-30, None, ALU.max), reads=[pacc], writes=[rd])
        k.op("dve", lambda h: h.reciprocal(rd.ap, rd.ap), reads=[rd], writes=[rd])
        if with_imp:
            k.op("dve", lambda h: h.scalar_tensor_tensor(imp_b[:, qt, :], pacc.ap[:, 129:193], rd.ap, imp_b[:, qt, :], ALU.mult, ALU.add),
                 reads=[pacc, rd, imp], writes=[imp])
        if r in r_own:
            lo = r_own.index(r)
            k.op("dve", lambda h: h.tensor_tensor(rg.ap, rd.ap, gts_b[:, qt, r * 3 + gate_j:r * 3 + gate_j + 1], ALU.mult),
                 reads=[rd, gts], writes=[rg])
            k.op("dve", lambda h: h.scalar_tensor_tensor(oacc_b[:, qt, lo * 128:(lo + 1) * 128], pacc.ap[:, 0:128], rg.ap,
                                                         oacc_b[:, qt, lo * 128:(lo + 1) * 128], ALU.mult, ALU.add),
                 reads=[pacc, rg, oacc], writes=[oacc])

    for r in range(4):
        for qg in range(8):
            qs = slice(qg * 512, (qg + 1) * 512)
            pts = []
            for nt_ in range(2):
                ps = pp.get()
                k.op("pe", lambda h: h.matmul(ps.ap, kcmp_b[:, nt_ * 128:(nt_ + 1) * 128], qT_b[:, r, qs], start=True, stop=True),
                     reads=[kcmp, qT], writes=[ps])
                e = e_t[nt_]; mkk = mk_t[nt_]; pt = p_t[nt_]
                k.op("act", lambda h: h.activation(e.ap, ps.ap, AF.Exp, scale=SC), reads=[ps], writes=[e])
                k.dma("pool", mkk.ap, cmask_d[nt_ * 128:(nt_ + 1) * 128, qs], writes=[mkk])
                k.op("dve", lambda h: h.tensor_tensor(pt.ap, e.ap, mkk.ap, ALU.mult), reads=[e, mkk], writes=[pt])
                pts.append(pt)
            for sub in range(4):
                pa = po[sub]
                for nt_ in range(2):
                    k.op("pe", lambda h: h.matmul(pa.ap[:, 0:193], pts[nt_].ap[:, sub * 128:(sub + 1) * 128], vca_b[:, nt_, :],
                                                  start=(nt_ == 0), stop=(nt_ == 1)),
                         reads=[pts[nt_], vca], writes=[pa], pe_accum=True)
                finish_sub(pa, qg * 4 + sub, r, 0, True)
    sc_t = [T(k.sb([128, 64], F32)[:]) for _ in range(2)]
    wk_t = [T(k.sb([128, 64], F32)[:]) for _ in range(2)]
    bm_t = [T(k.sb([128, 64], F32)[:]) for _ in range(2)]
    m8_t = [T(k.sb([128, 8], F32)[:]) for _ in range(2)]
    thr_t = [T(k.sb([128, 1], F32)[:]) for _ in range(2)]
    selb_t = [T(k.sb([128, 64], F32)[:]) for _ in range(2)]
    for qt in range(32):
        i = qt % 2
        sc = sc_t[i]; wk = wk_t[i]; bm = bm_t[i]; m8 = m8_t[i]; thr = thr_t[i]; selb = selb_t[i]
        k.dma("sp", bm.ap, bmask_d[qt * 128:(qt + 1) * 128, :], writes=[bm])
        k.op("dve", lambda h: h.tensor_tensor(sc.ap, imp_b[:, qt, :], bm.ap, ALU.add), reads=[imp, bm], writes=[sc])
        k.op("dve", lambda h: h.max(out=m8.ap, in_=sc.ap), reads=[sc], writes=[m8])
        k.op("dve", lambda h: h.match_replace(out=wk.ap, in_to_replace=m8.ap, in_values=sc.ap, imm_value=-3e38), reads=[sc, m8], writes=[wk])
        k.op("dve", lambda h: h.max(out=m8.ap, in_=wk.ap), reads=[wk], writes=[m8])
        k.op("dve", lambda h: h.tensor_reduce(out=thr.ap, in_=m8.ap, axis=AX.X, op=ALU.min), reads=[m8], writes=[thr])
        k.op("dve", lambda h: h.tensor_scalar(wk.ap, sc.ap, thr.ap, None, ALU.is_ge), reads=[sc, thr], writes=[wk])
        k.op("dve", lambda h: h.tensor_scalar(sc.ap, bm.ap, -1.0, None, ALU.is_ge), reads=[bm], writes=[sc])
        k.op("dve", lambda h: h.tensor_tensor(selb.ap, wk.ap, sc.ap, ALU.mult), reads=[wk, sc], writes=[selb])
        pt = pp.get()
        k.op("pe", lambda h: h.transpose(pt.ap[0:64, 0:128], selb.ap, ident.ap), reads=[selb, ident], writes=[pt])
        k.op("dve", lambda h: h.tensor_copy(selT_b[:, qt * 128:(qt + 1) * 128], pt.ap[0:64, 0:128]), reads=[pt], writes=[selT])
    ex_b = k.sb([64, S], BF16); ex = T(ex_b[:])
    k.dma("pool", ex.ap, ex_d, writes=[ex])
    caus_b = k.sb([128, 4, 512], BF16); caus = T(caus_b[:])
    k.dma("pool", caus.ap, caus_d, writes=[caus])
    band_b = k.sb([128, 8, 512], BF16); band = T(band_b[:])
    k.dma("pool", band.ap, band_d, writes=[band])
    for (br, kT_b, kT_t, v_b, v_t, gate_j) in (("slc", ks_b, ksT, vs_b, vs_tm, 1), ("win", kw_b, kwT, vw_b, vw_tm, 2)):
        for lo, r in enumerate(r_own):
            for qg in range(8):
                qs = slice(qg * 512, (qg + 1) * 512)
                kt0 = 0 if br == "slc" else max(0, 4 * qg - 4)
                kts = list(range(kt0, 4 * qg + 4))
                for kt in kts:
                    dd = kt - 4 * qg
                    ps = pp.get()
                    k.op("pe", lambda h: h.matmul(ps.ap, kT_b[:, kt * 128:(kt + 1) * 128], qr_b[:, lo, qs], start=True, stop=True),
                         reads=[kT_t, qr], writes=[ps])
                    i = cnt[0] % 3; cnt[0] += 1
                    e = e_t[i]; pt = p_t[i]
                    k.op("act", lambda h: h.activation(e.ap, ps.ap, AF.Exp, scale=SC), reads=[ps], writes=[e])
                    if br == "slc":
                        pm = pp.get()
                        k.op("pe", lambda h: h.matmul(pm.ap, ex_b[:, kt * 128:(kt + 1) * 128], selT_b[:, qs], start=True, stop=True),
                             reads=[ex, selT], writes=[pm])
                        k.op("dve", lambda h: h.tensor_tensor(pt.ap, e.ap, pm.ap, ALU.mult), reads=[e, pm], writes=[pt])
                        if dd >= 0:
                            k.op("dve", lambda h: h.tensor_tensor(pt.ap, pt.ap, caus_b[:, dd, :], ALU.mult), reads=[pt, caus], writes=[pt])
                    else:
                        k.op("dve", lambda h: h.tensor_tensor(pt.ap, e.ap, band_b[:, dd + 4, :], ALU.mult), reads=[e, band], writes=[pt])
                    for sub in range(4):
                        k.op("pe", lambda h: h.matmul(po[sub].ap[:, 0:129], pt.ap[:, sub * 128:(sub + 1) * 128], v_b[:, kt, 0:129],
                                                      start=(kt == kts[0]), stop=(kt == kts[-1])),
                             reads=[pt, v_t], writes=[po[sub]], pe_accum=True)
                for sub in range(4):
                    finish_sub(po[sub], qg * 4 + sub, r, gate_j, False)
    for qt in range(32):
        k.dma("sp", ob[qt * 128:(qt + 1) * 128, :], oacc_b[:, qt, :], reads=[oacc], writes=[tob])


def emit_nsa_f(k, g, h2v, w_in_v, ident, obT, tobT, nsa_in, validk_d):
    (cosT, sinT, rT_d, w1k_d, w1v_d, b1k_d, b1v_d, w2k_d, w2v_d, posk_d, posv_d, gsel_d, cmask_d, bmask_d, ex_d, caus_d, band_d) = nsa_in
    SC = 128.0 ** -0.5
    r_own = (0, 1, 2, 3)
    QG0 = 6
    pp = PsumPool(k, 4)
    po = [T(k.ps([128, 512], F32)[:]) for _ in range(4)]
    qT_b = k.sb([128, 4, NT], BF16); qT = T(qT_b[:])
    qr_b = k.sb([128, 4, NT], BF16); qr = T(qr_b[:])
    kc_b = k.sb([128, S], BF16); kcT = T(kc_b[:]); vc_b = k.sb([128, S], BF16); vcT = T(vc_b[:])
    ks_b = k.sb([128, S], BF16); ksT = T(ks_b[:]); kw_b = k.sb([128, S], BF16); kwT = T(kw_b[:])
    vs_b = k.sb([128, 32, 130], BF16); vs_tm = T(vs_b[:]); vw_b = k.sb([128, 32, 130], BF16); vw_tm = T(vw_b[:])
    gts_b = k.sb([128, 8, 12], F32); gts = T(gts_b[:])
    imp_b = k.sb([128, 8, 64], F32); imp = T(imp_b[:])
    oacc_b = k.sb([128, 8, 512], F32); oacc = T(oacc_b[:])
    selT_b = k.sb([64, NT], BF16); selT = T(selT_b[:])
    k.op("dve", lambda h: h.memset(vs_b[:, :, 128:129], 1.0), writes=[vs_tm])
    k.op("dve", lambda h: h.memset(vw_b[:, :, 128:129], 1.0), writes=[vw_tm])
    k.op("dve", lambda h: h.memset(imp.ap, 0.0), writes=[imp])
    k.op("dve", lambda h: h.memset(oacc.ap, 0.0), writes=[oacc])
    with ExitStack() as ph:
        old = k.stack; k.stack = ph
        hb_b = k.sb([128, 16, 512], BF16); hb = T(hb_b[:])
        ws = WStream(k, 4, 16 * 128)
        wng_b = k.sb([128, 16, 12], BF16); wng = T(wng_b[:])
        k.dma("pool", wng_b[:], w_in_v[:, :, 6672 + 12 * g:6672 + 12 * g + 12], writes=[wng])
        vk_t = None
        rT_b = k.sb([128, 128], BF16); rT = T(rT_b[:])
        k.dma("pool", rT.ap, rT_d, writes=[rT])
        cs_t = [T(k.sb([128, 512], F32)[:]) for _ in range(2)]
        sn_t = [T(k.sb([128, 512], F32)[:]) for _ in range(2)]
        xb_t = [T(k.sb([128, 512], BF16)[:]) for _ in range(2)]
        t1_t = [T(k.sb([128, 512], F32)[:]) for _ in range(2)]
        t2_t = [T(k.sb([128, 512], F32)[:]) for _ in range(2)]
        jobs = {}
        def wcol(ch):
            if ch < 4:
                c0 = 4112 + (4 * g + ch) * 128
            else:
                c0 = 5136 + (ch - 4) * 256 + g * 128
            return w_in_v[:, :, c0:c0 + 128]
        for tb in range(8):
            for ch in range(10):
                if ch < 4 and tb < QG0:
                    continue
                jobs[(tb, ch)] = ws.add(wcol(ch), 128)
        nr = 0
        for tb in range(8):
            bs = slice(tb * 512, (tb + 1) * 512)
            k.dma("pool", hb.ap, h2v[:, :, bs], writes=[hb])
            k.dma("sp", cs_t[tb % 2].ap, cosT[:, bs], writes=[cs_t[tb % 2]])
            k.dma("sp", sn_t[tb % 2].ap, sinT[:, bs], writes=[sn_t[tb % 2]])
            for ch in range(10):
                if ch < 4 and tb < QG0:
                    continue
                lbs = slice((tb - QG0) * 512, (tb - QG0 + 1) * 512)
                tw, vw_ = ws.get(jobs[(tb, ch)])
                ps = pp.get()
                for kc in range(16):
                    k.op("pe", lambda h: h.matmul(ps.ap, vw_[:, kc, :], hb_b[:, kc, :], start=(kc == 0), stop=(kc == 15)),
                         reads=[tw, hb], writes=[ps], pe_accum=True)
                rope_dst = None
                if ch < 4:
                    k.op("act", lambda h: h.activation(qT_b[:, ch, lbs], ps.ap, AF.Copy), reads=[ps], writes=[qT])
                    rope_dst = (qr, qr_b[:, ch, lbs])
                elif ch == 4:
                    k.op("act", lambda h: h.activation(kc_b[:, bs], ps.ap, AF.Copy), reads=[ps], writes=[kcT])
                elif ch == 5:
                    k.op("act", lambda h: h.activation(vc_b[:, bs], ps.ap, AF.Copy), reads=[ps], writes=[vcT])
                elif ch == 6:
                    rope_dst = (ksT, ks_b[:, bs])
                elif ch == 8:
                    rope_dst = (kwT, kw_b[:, bs])
                else:
                    dst_t, dst_b = (vs_tm, vs_b) if ch == 7 else (vw_tm, vw_b)
                    xf = t1_t[nr % 2]; nr += 1
                    k.op("act", lambda h: h.activation(xf.ap, ps.ap, AF.Copy), reads=[ps], writes=[xf])
                    for tt in range(4):
                        pt = pp.get()
                        k.op("pe", lambda h: h.transpose(pt.ap[:, 0:128], xf.ap[:, tt * 128:(tt + 1) * 128], ident.ap),
                             reads=[xf, ident], writes=[pt])
                        k.op("dve", lambda h: h.tensor_copy(dst_b[:, tb * 4 + tt, 0:128], pt.ap[:, 0:128]), reads=[pt], writes=[dst_t])
                if rope_dst is not None:
                    dt_, dap = rope_dst
                    xb = xb_t[nr % 2]; nr += 1
                    k.op("act", lambda h: h.activation(xb.ap, ps.ap, AF.Copy), reads=[ps], writes=[xb])
                    pr = pp.get()
                    k.op("pe", lambda h: h.matmul(pr.ap, rT.ap, xb.ap, start=True, stop=True), reads=[rT, xb], writes=[pr])
                    t1 = t1_t[nr % 2]; t2 = t2_t[nr % 2]
                    k.op("dve", lambda h: h.tensor_tensor(t1.ap, ps.ap, cs_t[tb % 2].ap, ALU.mult), reads=[ps, cs_t[tb % 2], xb], writes=[t1])
                    k.op("dve", lambda h: h.tensor_tensor(t2.ap, pr.ap, sn_t[tb % 2].ap, ALU.mult), reads=[pr, sn_t[tb % 2]], writes=[t2])
                    k.op("dve", lambda h: h.tensor_tensor(dap, t1.ap, t2.ap, ALU.add), reads=[t1, t2], writes=[dt_])
            for tt in range(4 if tb >= QG0 else 0):
                pg = pp.get()
                for kc in range(16):
                    k.op("pe", lambda h: h.matmul(pg.ap[:, 0:12], hb_b[:, kc, tt * 128:(tt + 1) * 128], wng_b[:, kc, :],
                                                  start=(kc == 0), stop=(kc == 15)),
                         reads=[hb, wng], writes=[pg], pe_accum=True)
                k.op("act", lambda h: h.activation(gts_b[:, (tb - QG0) * 4 + tt, :], pg.ap[:, 0:12], AF.Sigmoid), reads=[pg], writes=[gts])
        k.barrier()
        k.stack = old
    kcmp_b = k.sb([128, 256], BF16); kcmp = T(kcmp_b[:])
    vca_b = k.sb([128, 2, 193], BF16); vca = T(vca_b[:])
    k.op("dve", lambda h: h.memset(kcmp.ap, 0.0), writes=[kcmp])
    k.op("dve", lambda h: h.memset(vca.ap, 0.0), writes=[vca])
    k.op("dve", lambda h: h.memset(vca_b[:, :, 128:129], 1.0), writes=[vca])
    k.dma("pool", vca_b[:, :, 129:193], gsel_d.rearrange("(a p) j -> p a j", p=128), writes=[vca])
    with ExitStack() as ph:
        old = k.stack; k.stack = ph
        w1_b = k.sb([128, 32, 128], BF16); w1 = T(w1_b[:])
        w2_b = k.sb([128, 128], BF16); w2 = T(w2_b[:])
        pos_b = k.sb([128, 32], BF16); pos = T(pos_b[:])
        b1_b = k.sb([128, 1], F32); b1 = T(b1_b[:])
        bias_b = k.sb([128, 1], F32); bias = T(bias_b[:])
        hid_b = k.sb([128, 256], BF16); hid = T(hid_b[:])
        for which in range(2):
            w1d, b1d, w2d, posd, src_b, src_t = ((w1k_d, b1k_d, w2k_d, posk_d, kc_b, kcT), (w1v_d, b1v_d, w2v_d, posv_d, vc_b, vcT))[which]
            k.dma("pool", w1.ap, w1d.rearrange("(j d) m -> d j m", d=128), writes=[w1])
            k.dma("pool", w2.ap, w2d, writes=[w2])
            k.dma("pool", pos.ap, posd, writes=[pos])
            k.dma("sp", b1.ap, b1d, writes=[b1])
            pb = pp.get()
            for j in range(32):
                k.op("pe", lambda h: h.matmul(pb.ap[:, 0:1], w1_b[:, j, :], pos_b[:, j:j + 1], start=(j == 0), stop=(j == 31)),
                     reads=[w1, pos], writes=[pb], pe_accum=True)
            k.op("dve", lambda h: h.tensor_tensor(bias.ap, pb.ap[:, 0:1], b1.ap, ALU.add), reads=[pb, b1], writes=[bias])
            ph_ = pp.get()
            for j in range(32):
                k.op("pe", lambda h: h.matmul(ph_.ap[:, 0:255], w1_b[:, j, :], src_b[:, j:j + 16 * 254 + 1:16], start=(j == 0), stop=(j == 31)),
                     reads=[w1, src_t], writes=[ph_], pe_accum=True)
            k.op("dve", lambda h: h.memset(hid.ap, 0.0), writes=[hid])
            k.op("act", lambda h: h.activation(hid_b[:, 0:255], ph_.ap[:, 0:255], AF.Silu, bias=bias.ap), reads=[ph_, bias], writes=[hid])
            if which == 0:
                pk = pp.get()
                k.op("pe", lambda h: h.matmul(pk.ap[:, 0:255], w2.ap, hid_b[:, 0:255], start=True, stop=True), reads=[w2, hid], writes=[pk])
                k.op("act", lambda h: h.activation(kcmp_b[:, 0:255], pk.ap[:, 0:255], AF.Copy), reads=[pk], writes=[kcmp])
            else:
                for nt_ in range(2):
                    pv = pp.get()
                    k.op("pe", lambda h: h.matmul(pv.ap[:, 0:128], hid_b[:, nt_ * 128:(nt_ + 1) * 128], w2.ap, start=True, stop=True),
                         reads=[w2, hid], writes=[pv])
                    k.op("act", lambda h: h.activation(vca_b[:, nt_, 0:128], pv.ap[:, 0:128], AF.Copy), reads=[pv], writes=[vca])
        k.barrier()
        k.stack = old
    e_t = [T(k.sb([128, 512], BF16)[:]) for _ in range(3)]
    p_t = [T(k.sb([128, 512], BF16)[:]) for _ in range(3)]
    mk_t = [T(k.sb([128, 512], BF16)[:]) for _ in range(2)]
    rd_t = [T(k.sb([128, 1], F32)[:]) for _ in range(2)]
    rg_t = [T(k.sb([128, 1], F32)[:]) for _ in range(2)]
    tmpo = [T(k.sb([128, 128], F32)[:]) for _ in range(2)]
    cnt = [0]

    def finish_sub(pacc, qt, r, gate_j, with_imp):
        i = cnt[0] % 2; cnt[0] += 1
        rd = rd_t[i]; rg = rg_t[i]
        k.op("dve", lambda h: h.tensor_scalar(rd.ap, pacc.ap[:, 128:129], 1e-30, None, ALU.max), reads=[pacc], writes=[rd])
        k.op("dve", lambda h: h.reciprocal(rd.ap, rd.ap), reads=[rd], writes=[rd])
        if with_imp:
            k.op("dve", lambda h: h.scalar_tensor_tensor(imp_b[:, qt, :], pacc.ap[:, 129:193], rd.ap, imp_b[:, qt, :], ALU.mult, ALU.add),
                 reads=[pacc, rd, imp], writes=[imp])
        if r in r_own:
            lo = r
            k.op("dve", lambda h: h.tensor_tensor(rg.ap, rd.ap, gts_b[:, qt, r * 3 + gate_j:r * 3 + gate_j + 1], ALU.mult),
                 reads=[rd, gts], writes=[rg])
            k.op("dve", lambda h: h.scalar_tensor_tensor(oacc_b[:, qt, lo * 128:(lo + 1) * 128], pacc.ap[:, 0:128], rg.ap,
                                                         oacc_b[:, qt, lo * 128:(lo + 1) * 128], ALU.mult, ALU.add),
                 reads=[pacc, rg, oacc], writes=[oacc])

    for r in range(4):
        for qg in range(2):
            qs = slice(qg * 512, (qg + 1) * 512)
            pts = []
            for nt_ in range(2):
                ps = pp.get()
                k.op("pe", lambda h: h.matmul(ps.ap, kcmp_b[:, nt_ * 128:(nt_ + 1) * 128], qT_b[:, r, qs], start=True, stop=True),
                     reads=[kcmp, qT], writes=[ps])
                e = e_t[nt_]; mkk = mk_t[nt_]; pt = p_t[nt_]
                k.op("act", lambda h: h.activation(e.ap, ps.ap, AF.Exp, scale=SC), reads=[ps], writes=[e])
                k.dma("pool", mkk.ap, cmask_d[nt_ * 128:(nt_ + 1) * 128, qs], writes=[mkk])
                k.op("dve", lambda h: h.tensor_tensor(pt.ap, e.ap, mkk.ap, ALU.mult), reads=[e, mkk], writes=[pt])
                pts.append(pt)
            for sub in range(4):
                pa = po[sub]
                for nt_ in range(2):
                    k.op("pe", lambda h: h.matmul(pa.ap[:, 0:193], pts[nt_].ap[:, sub * 128:(sub + 1) * 128], vca_b[:, nt_, :],
                                                  start=(nt_ == 0), stop=(nt_ == 1)),
                         reads=[pts[nt_], vca], writes=[pa], pe_accum=True)
                finish_sub(pa, qg * 4 + sub, r, 0, True)
    sc_t = [T(k.sb([128, 64], F32)[:]) for _ in range(2)]
    wk_t = [T(k.sb([128, 64], F32)[:]) for _ in range(2)]
    bm_t = [T(k.sb([128, 64], F32)[:]) for _ in range(2)]
    m8_t = [T(k.sb([128, 8], F32)[:]) for _ in range(2)]
    thr_t = [T(k.sb([128, 1], F32)[:]) for _ in range(2)]
    selb_t = [T(k.sb([128, 64], F32)[:]) for _ in range(2)]
    for qt in range(8):
        i = qt % 2
        sc = sc_t[i]; wk = wk_t[i]; bm = bm_t[i]; m8 = m8_t[i]; thr = thr_t[i]; selb = selb_t[i]
        k.dma("sp", bm.ap, bmask_d[qt * 128:(qt + 1) * 128, :], writes=[bm])
        k.op("dve", lambda h: h.tensor_tensor(sc.ap, imp_b[:, qt, :], bm.ap, ALU.add), reads=[imp, bm], writes=[sc])
        k.op("dve", lambda h: h.max(out=m8.ap, in_=sc.ap), reads=[sc], writes=[m8])
        k.op("dve", lambda h: h.match_replace(out=wk.ap, in_to_replace=m8.ap, in_values=sc.ap, imm_value=-3e38), reads=[sc, m8], writes=[wk])
        k.op("dve", lambda h: h.max(out=m8.ap, in_=wk.ap), reads=[wk], writes=[m8])
        k.op("dve", lambda h: h.tensor_reduce(out=thr.ap, in_=m8.ap, axis=AX.X, op=ALU.min), reads=[m8], writes=[thr])
        k.op("dve", lambda h: h.tensor_scalar(wk.ap, sc.ap, thr.ap, None, ALU.is_ge), reads=[sc, thr], writes=[wk])
        k.op("dve", lambda h: h.tensor_scalar(sc.ap, bm.ap, -1.0, None, ALU.is_ge), reads=[bm], writes=[sc])
        k.op("dve", lambda h: h.tensor_tensor(selb.ap, wk.ap, sc.ap, ALU.mult), reads=[wk, sc], writes=[selb])
        pt = pp.get()
        k.op("pe", lambda h: h.transpose(pt.ap[0:64, 0:128], selb.ap, ident.ap), reads=[selb, ident], writes=[pt])
        k.op("dve", lambda h: h.tensor_copy(selT_b[:, qt * 128:(qt + 1) * 128], pt.ap[0:64, 0:128]), reads=[pt], writes=[selT])
    ex_b = k.sb([64, S], BF16); ex = T(ex_b[:])
    k.dma("pool", ex.ap, ex_d, writes=[ex])
    caus_b = k.sb([128, 4, 512], BF16); caus = T(caus_b[:])
    k.dma("pool", caus.ap, caus_d, writes=[caus])
    band_b = k.sb([128, 8, 512], BF16); band = T(band_b[:])
    k.dma("pool", band.ap, band_d, writes=[band])
    vk = T(k.sb([128, 32], F32)[:])
    k.dma("sp", vk.ap, validk_d, writes=[vk])
    for (br, kT_b, kT_t, v_b, v_t, gate_j) in (("slc", ks_b, ksT, vs_b, vs_tm, 1), ("win", kw_b, kwT, vw_b, vw_tm, 2)):
        for lo, r in enumerate(r_own):
            for lqg in range(2):
                qg = QG0 + lqg
                qs = slice(lqg * 512, (lqg + 1) * 512)
                kt0 = 0 if br == "slc" else max(0, 4 * qg - 4)
                kts = list(range(kt0, 4 * qg + 4))
                for kt in kts:
                    dd = kt - 4 * qg
                    ps = pp.get()
                    k.op("pe", lambda h: h.matmul(ps.ap, kT_b[:, kt * 128:(kt + 1) * 128], qr_b[:, lo, qs], start=True, stop=True),
                         reads=[kT_t, qr], writes=[ps])
                    i = cnt[0] % 3; cnt[0] += 1
                    e = e_t[i]; pt = p_t[i]
                    k.op("act", lambda h: h.activation(e.ap, ps.ap, AF.Exp, scale=SC), reads=[ps], writes=[e])
                    if br == "slc":
                        pm = pp.get()
                        k.op("pe", lambda h: h.matmul(pm.ap, ex_b[:, kt * 128:(kt + 1) * 128], selT_b[:, qs], start=True, stop=True),
                             reads=[ex, selT], writes=[pm])
                        k.op("dve", lambda h: h.tensor_tensor(pt.ap, e.ap, pm.ap, ALU.mult), reads=[e, pm], writes=[pt])
                        if dd >= 0:
                            k.op("dve", lambda h: h.tensor_tensor(pt.ap, pt.ap, caus_b[:, dd, :], ALU.mult), reads=[pt, caus], writes=[pt])
                    else:
                        k.op("dve", lambda h: h.scalar_tensor_tensor(pt.ap, e.ap, vk.ap[:, kt:kt + 1], band_b[:, dd + 4, :], ALU.mult, ALU.mult),
                             reads=[e, band, vk], writes=[pt])
                    for sub in range(4):
                        k.op("pe", lambda h: h.matmul(po[sub].ap[:, 0:129], pt.ap[:, sub * 128:(sub + 1) * 128], v_b[:, kt, 0:129],
                                                      start=(kt == kts[0]), stop=(kt == kts[-1])),
                             reads=[pt, v_t], writes=[po[sub]], pe_accum=True)
                for sub in range(4):
                    finish_sub(po[sub], lqg * 4 + sub, r, gate_j, False)
    oT_t = [T(k.sb([128, 128], F32)[:]) for _ in range(2)]
    for qt in range(8):
        for r in range(4):
            pt = pp.get()
            k.op("pe", lambda h: h.transpose(pt.ap[:, 0:128], oacc_b[:, qt, r * 128:(r + 1) * 128], ident.ap), reads=[oacc, ident], writes=[pt])
            oT_ = oT_t[(qt * 4 + r) % 2]
            k.op("dve", lambda h: h.tensor_copy(oT_.ap, pt.ap[:, 0:128]), reads=[pt], writes=[oT_])
            k.dma("sp", obT[(4 * g + r) * 128:(4 * g + r + 1) * 128, qt * 128:(qt + 1) * 128], oT_.ap, reads=[oT_], writes=[tobT])


def nsa_consts():
    pos = np.arange(S, dtype=np.float32)
    inv_freq = (np.float32(500000.0) ** (-np.arange(0, 32, 2, dtype=np.float32) / np.float32(32))).astype(np.float32)
    ang = (pos[:, None] * inv_freq[None, :]).astype(np.float32)
    cos, sin = np.cos(ang).astype(np.float32), np.sin(ang).astype(np.float32)
    cosT = np.ones((128, S), np.float32); sinT = np.zeros((128, S), np.float32)
    cosT[0:16] = cos.T; cosT[16:32] = cos.T; sinT[0:16] = sin.T; sinT[16:32] = sin.T
    R = np.zeros((128, 128), np.float32)
    for d_ in range(16):
        R[d_, d_ + 16] = -1.0
        R[d_ + 16, d_] = 1.0
    n = np.arange(256); jb = np.arange(64); t = np.arange(S); kk = np.arange(128); q = np.arange(512)
    gsel = ((n[:, None] // 4 == jb[None, :]) & (n[:, None] < 255)).astype(np.float32)
    cmask = ((16 * n[:, None] + 31 <= t[None, :]) & (n[:, None] < 255)).astype(np.float32)
    vis = 64 * jb[None, :] <= t[:, None]
    tb_ = t[:, None] // 64
    forced = (jb[None, :] == 0) | (jb[None, :] == tb_) | (jb[None, :] == tb_ - 1)
    bmask = np.where(vis, np.where(forced, 1e3, 0.0), -1e30).astype(np.float32)
    ex = (t[None, :] // 64 == jb[:, None]).astype(np.float32)
    caus = np.stack([((128 * dd + kk[:, None]) <= q[None, :]) for dd in range(4)], axis=1).astype(np.float32)
    band = np.stack([((q[None, :] - (128 * dd + kk[:, None]) >= 0) & (q[None, :] - (128 * dd + kk[:, None]) < 512))
                     for dd in range(-4, 4)], axis=1).astype(np.float32)
    return dict(cosT=cosT, sinT=sinT, rT=np.ascontiguousarray(R.T), gsel=gsel, cmask=cmask, bmask=bmask, ex=ex,
                caus=np.ascontiguousarray(caus), band=np.ascontiguousarray(band))


def l2_inputs(inp, h2T_full, c):
    b, hp = c // 4, c % 4
    g = hp // 2
    w = inp["w_in"][0]
    heads = (2 * hp, 2 * hp + 1)
    cols = []
    for sec in range(3):
        for hd in heads:
            cols.append(np.arange(sec * 1024 + hd * 128, sec * 1024 + (hd + 1) * 128))
    hq_order = [2 * hp, 2 * hp + 1] + [hq for hq in range(g * 4, g * 4 + 4) if hq not in (2 * hp, 2 * hp + 1)]
    for hq in hq_order:
        cols.append(np.arange(4112 + hq * 128, 4112 + (hq + 1) * 128))
    for j in range(6):
        cols.append(np.arange(5136 + j * 256 + g * 128, 5136 + j * 256 + (g + 1) * 128))
    wfm = np.ascontiguousarray(w[:, np.concatenate(cols)])
    tcols = []
    for hd in heads:
        tcols += [np.arange(3088 + hd * 128, 3088 + (hd + 1) * 128), np.array([3072 + hd, 3080 + hd])]
    tcols += [np.arange(6672 + hq * 3, 6672 + hq * 3 + 3) for hq in hq_order]
    wtm = np.ascontiguousarray(w[:, np.concatenate(tcols)])
    cwf = inp["gdn_conv_w"][0]
    convw = np.stack([cwf[sec * 1024 + hd * 128: sec * 1024 + (hd + 1) * 128] for sec in range(3) for hd in heads], axis=1)
    bc = lambda v: np.ascontiguousarray(np.broadcast_to(np.asarray(v, np.float32)[None, :], (128, len(v))))
    ii = np.arange(64)
    return {
        "h2T": h2T_full[b], "wfm": wfm, "wtm": wtm, "convw": np.ascontiguousarray(convw.astype(np.float32)),
        "alog": bc(inp["gdn_a_log"][0][list(heads)]), "dtb": bc(inp["gdn_dt_bias"][0][list(heads)]),
        "gnw": bc(inp["gdn_norm_w"][0]),
        "ident": np.eye(128, dtype=np.float32),
        "tri": (ii[:, None] <= ii[None, :]).astype(np.float32),
        "ms": np.where(ii[:, None] > ii[None, :], 0.0, 1e4).astype(np.float32),
        "mc": np.where(ii[None, :] >= ii[:, None], 0.0, -1e4).astype(np.float32),
        "w1k": inp["cmp_k_w1"][0], "w1v": inp["cmp_v_w1"][0],
        "b1k": np.ascontiguousarray(inp["cmp_k_b1"][0][:, None]), "b1v": np.ascontiguousarray(inp["cmp_v_b1"][0][:, None]),
        "w2k": inp["cmp_k_w2"][0], "w2v": inp["cmp_v_w2"][0],
        "posk": np.ascontiguousarray(inp["cmp_pos_k"][0].T), "posv": np.ascontiguousarray(inp["cmp_pos_v"][0].T),
    }


def run_l2(inp, h2T_full, do_nsa=True, do_gdn=True, stage=99, heads=(0, 1)):
    nc = build_l2(do_nsa, do_gdn, stage=stage, heads=heads)
    consts = nsa_consts()
    maps = []
    for c in range(NCORE):
        m_ = l2_inputs(inp, h2T_full, c)
        m_.update(consts)
        maps.append(m_)
    return run_bass_kernel_spmd(nc, maps, core_ids=list(range(NCORE)))


def build_fused():
    nc = bass.Bass("TRN2", target_bir_lowering=False)
    dt = lambda n, s_, kind="ExternalInput": nc.dram_tensor(n, s_, F32, kind=kind).ap()
    xTp = dt("xTp", [D, S]); cb = dt("cb", [128, 16]); ada_w = dt("ada_w", [D, 9 * D]); ada_b = dt("ada_b", [128, 144])
    n1 = dt("n1", [128, 16]); n2 = dt("n2", [128, 16]); n3 = dt("n3", [128, 16]); nf = dt("nf", [128, 16])
    wg1 = dt("wg1", [D, DFF]); wu1 = dt("wu1", [D, DFF]); wd1 = dt("wd1", [DFF, D])
    wg2 = dt("wg2", [D, DFF]); wu2 = dt("wu2", [D, DFF]); wd2 = dt("wd2", [DFF, D])
    w_in = dt("w_in", [D, 10792]); wba = dt("wba", [D, 16])
    convw = dt("convw", [128, 24, 4]); alog = dt("alog", [128, 8]); dtb = dt("dtb", [128, 8]); gnw = dt("gnw", [128, 128])
    ident_d = dt("ident", [128, 128]); tri_d = dt("tri", [64, 64]); ms_d = dt("ms", [64, 64]); mc_d = dt("mc", [64, 64])
    validT = dt("validT", [128, S]); valid_tm = dt("valid_tm", [64, NCH]); validk = dt("validk", [128, 32])
    nsa_in = (dt("cosT", [128, S]), dt("sinT", [128, S]), dt("rT", [128, 128]),
              dt("w1k", [4096, 128]), dt("w1v", [4096, 128]), dt("b1k", [128, 1]), dt("b1v", [128, 1]),
              dt("w2k", [128, 128]), dt("w2v", [128, 128]), dt("posk", [128, 32]), dt("posv", [128, 32]),
              dt("gsel", [256, 64]), dt("cmask", [256, NT]), dt("bmask", [NT, 64]), dt("ex", [64, S]),
              dt("caus", [128, 4, 512]), dt("band", [128, 8, 512]))
    gup = dt("gup", [1024, D]); nup = dt("nup", [1024, D]); wo = dt("wo", [D, D])
    outT = dt("outT", [D, NT], "ExternalOutput")
    h2s = nc.dram_tensor("h2s", [D, S], F32).ap(); x1s = nc.dram_tensor("x1s", [D, NT], F32).ap()
    oaT = nc.dram_tensor("oaT", [1024, NT], F32).ap(); obT = nc.dram_tensor("obT", [1024, NT], F32).ap()
    th2s = [T(h2s[:, i * NT:(i + 1) * NT]) for i in range(4)]; tx1s = T(x1s); toaT = T(oaT); tobT = T(obT)
    with ExitStack() as st:
        k = K(nc, st)
        small = k.sb([128, 512], F32)
        cact = T(small[:, 0:16]); modS = T(small[:, 16:160]); adab = T(small[:, 160:304])
        n1t = T(small[:, 304:320]); n2t = T(small[:, 320:336])
        A1 = T(small[:, 336:352]); hg1 = T(small[:, 352:368]); A2 = T(small[:, 368:384])
        n3t = T(small[:, 384:400]); nft = T(small[:, 400:416]); A3 = T(small[:, 416:432]); hg3 = T(small[:, 432:448]); zer = T(small[:, 448:464])
        ones_b = k.sb([128, 128], BF16); ones_bf = T(ones_b[:])
        m = lambda j: modS.ap[:, j * 16:(j + 1) * 16]
        k.op("dve", lambda h: h.memset(ones_bf.ap, 1.0), writes=[ones_bf])
        k.op("dve", lambda h: h.memset(zer.ap, 0.0), writes=[zer])
        for t_, d_ in ((cact, cb), (adab, ada_b), (n1t, n1), (n2t, n2), (n3t, n3), (nft, nf)):
            k.dma("sp", t_.ap, d_, writes=[t_])
        with ExitStack() as ph:
            k.stack = ph
            pp = PsumPool(k, 7)
            mps = T(k.ps([128, 144], F32)[:])
            xres_b = k.sb([128, 16, NT], F32); xres = [T(xres_b[:, i, :]) for i in range(16)]
            hT_b = k.sb([128, 16, NT], BF16); hT = [T(hT_b[:, i, :]) for i in range(16)]
            aT_b = k.sb([128, 22, NT], BF16); aT = [T(aT_b[:, i, :]) for i in range(22)]
            ws = WStream(k, 5, 22 * 128)
            rstd = T(k.sb([128, NT], F32)[:])
            sq_ts = [T(k.sb([128, NT], BF16)[:]) for _ in range(2)]
            tmp_ts = [T(k.sb([128, NT], F32)[:]) for _ in range(2)]
            sil_ts = [T(k.sb([128, 512], BF16)[:]) for _ in range(2)]
            adw = [k.sb([128, 16, 128], F32) for _ in range(2)]
            adw_t = [T(a_[:]) for a_ in adw]
            k.op("act", lambda h: h.activation(cact.ap, cact.ap, AF.Silu), reads=[cact], writes=[cact])
            adv = ada_w.rearrange("(kc p) n -> p kc n", p=128)
            for g_ in range(144):
                wt_ = adw_t[g_ % 2]
                k.dma("sp", wt_.ap, adv[:, :, g_ * 128:(g_ + 1) * 128], writes=[wt_])
                for kc in range(16):
                    k.op("pe", lambda h: h.matmul(mps.ap[:, g_:g_ + 1], adw[g_ % 2][:, kc, :],
                                                  cact.ap[:, kc:kc + 1], start=(kc == 0), stop=(kc == 15)),
                         reads=[wt_, cact], writes=[mps], pe_accum=True)
            k.op("dve", lambda h: h.tensor_tensor(modS.ap, mps.ap, adab.ap, ALU.add), reads=[mps, adab], writes=[modS])
            k.op("dve", lambda h: h.scalar_tensor_tensor(A1.ap, m(1), 1.0, n1t.ap, ALU.add, ALU.mult), reads=[modS, n1t], writes=[A1])
            k.op("dve", lambda h: h.tensor_scalar(hg1.ap, m(2), 0.5, None, ALU.mult), reads=[modS], writes=[hg1])
            k.op("dve", lambda h: h.scalar_tensor_tensor(A2.ap, m(4), 1.0, n2t.ap, ALU.add, ALU.mult), reads=[modS, n2t], writes=[A2])
            k.op("dve", lambda h: h.scalar_tensor_tensor(A3.ap, m(7), 1.0, n3t.ap, ALU.add, ALU.mult), reads=[modS, n3t], writes=[A3])
            k.op("dve", lambda h: h.tensor_scalar(hg3.ap, m(8), 0.5, None, ALU.mult), reads=[modS], writes=[hg3])
            for blk in range(4):
                bsl = slice(blk * NT, (blk + 1) * NT)
                for kc in range(16):
                    k.dma("act", xres[kc].ap, xTp[kc * 128:(kc + 1) * 128, bsl], writes=[xres[kc]])
                emit_norm_mod(k, pp, xres, ones_bf, sq_ts, rstd, tmp_ts, A1.ap, m(0), lambda kc: (hT[kc], hT[kc].ap),
                              extra_reads=[A1, modS])
                emit_ffn(k, pp, ws, xres, hT, aT, sil_ts, wg1, wu1, wd1, hg1.ap, hg1)
                if blk == 3:
                    for kc in range(16):
                        k.dma("sp", x1s[kc * 128:(kc + 1) * 128, :], xres[kc].ap, reads=[xres[kc]], writes=[tx1s])

                def post_h2(kc, t_, blk=blk, bsl=bsl):
                    k.dma("sp", h2s[kc * 128:(kc + 1) * 128, bsl], t_.ap, reads=[t_], writes=[th2s[blk]])
                emit_norm_mod(k, pp, xres, ones_bf, sq_ts, rstd, tmp_ts, A2.ap, m(3), lambda kc: (None, None),
                              post=post_h2, extra_reads=[A2, modS])
            k.barrier()
        k.stack = st
        h2v = h2s.rearrange("(kc p) t -> p kc t", p=128)
        w_in_v = w_in.rearrange("(kc p) n -> p kc n", p=128)
        wba_v = wba.rearrange("(kc p) n -> p kc n", p=128)
        with ExitStack() as pst:
            k.stack = pst
            cs = k.sb([128, 1024], F32)
            ident = T(cs[:, 0:128]); tri = T(cs[0:64, 128:192]); ms = T(cs[0:64, 192:256]); mc = T(cs[0:64, 256:320])
            ones = T(cs[:, 320:448]); cw = T(cs[:, 448:544]); alg = T(cs[:, 544:552]); dtbt = T(cs[:, 552:560])
            gnwt = T(cs[:, 640:768])
            k.dma("sp", ident.ap, ident_d, writes=[ident]); k.dma("sp", tri.ap, tri_d, writes=[tri])
            k.dma("sp", ms.ap, ms_d, writes=[ms]); k.dma("sp", mc.ap, mc_d, writes=[mc])
            k.dma("sp", cw.ap, convw.rearrange("p a b -> p (a b)"), writes=[cw])
            k.dma("sp", alg.ap, alog, writes=[alg]); k.dma("sp", dtbt.ap, dtb, writes=[dtbt])
            k.dma("sp", gnwt.ap, gnw, writes=[gnwt])
            k.op("dve", lambda h: h.memset(ones.ap, 1.0), writes=[ones])
            k.op("act", lambda h: h.activation(alg.ap, alg.ap, AF.Exp), reads=[alg], writes=[alg])
            k.op("dve", lambda h: h.tensor_scalar(alg.ap, alg.ap, -1.0, None, ALU.mult), reads=[alg], writes=[alg])
            with ExitStack() as pps:
                k.stack = pps
                pp = PsumPool(k, 8)
                for hd in range(8):
                    with ExitStack() as ph:
                        k.stack = ph
                        fz = dict(hd=hd, w_in_v=w_in_v, wba_v=wba_v, validT=validT, valid_tm=valid_tm, oaT=oaT, toaT=toaT, c_first=48)
                        emit_gdn_head(k, pp, hd, h2v, None, None, ident, tri, ms, mc, ones, cw, alg, dtbt, gnwt, None, None, fz=fz)
                        k.barrier()
                    k.stack = pps
                k.barrier()
            k.stack = pst
            for g in range(2):
                with ExitStack() as ph:
                    k.stack = ph
                    emit_nsa_f(k, g, h2v, w_in_v, ident, obT, tobT, nsa_in, validk)
                    k.barrier()
                k.stack = pst
            k.barrier()
        k.stack = st
        with ExitStack() as ph:
            k.stack = ph
            pp = PsumPool(k, 8)
            xres_b = k.sb([128, 16, NT], F32); xres = [T(xres_b[:, i, :]) for i in range(16)]
            hT_b = k.sb([128, 16, NT], BF16); hT = [T(hT_b[:, i, :]) for i in range(16)]
            aT_b = k.sb([128, 22, NT], BF16); aT = [T(aT_b[:, i, :]) for i in range(22)]
            ws = WStream(k, 5, 22 * 128)
            rstd = T(k.sb([128, NT], F32)[:])
            sq_ts = [T(k.sb([128, NT], BF16)[:]) for _ in range(2)]
            tmp_ts = [T(k.sb([128, NT], F32)[:]) for _ in range(2)]
            sil_ts = [T(k.sb([128, 512], BF16)[:]) for _ in range(2)]
            sg_ts = [T(k.sb([128, 512], F32)[:]) for _ in range(2)]
            h2own = h2s[:, 3 * NT:4 * NT]
            for kc in range(16):
                k.dma("pool", hT[kc].ap, h2own[kc * 128:(kc + 1) * 128, :], reads=[th2s[3]], writes=[hT[kc]])
            for kc in range(16):
                k.dma("act", xres[kc].ap, x1s[kc * 128:(kc + 1) * 128, :], reads=[tx1s], writes=[xres[kc]])
            wmv = w_in_v[:, :, 6696:6696 + 2 * D]
            guv = gup.rearrange("(kc p) n -> p kc n", p=128)
            nuv = nup.rearrange("(kc p) n -> p kc n", p=128)
            wov = wo.rearrange("(kc p) n -> p kc n", p=128)
            oav = oaT.rearrange("(c p) t -> p c t", p=128)
            obv = obT.rearrange("(c p) t -> p c t", p=128)
            mer_t = [T(aT_b[:, i // 2, (i % 2) * 512:(i % 2 + 1) * 512]) for i in range(16)]
            oa_t = [T(aT_b[:, 8 + i // 2, (i % 2) * 512:(i % 2 + 1) * 512]) for i in range(8)]
            ob_t = [T(aT_b[:, 12 + i // 2, (i % 2) * 512:(i % 2 + 1) * 512]) for i in range(8)]
            jobs = {}
            for th in range(2):
                for fc in range(16):
                    cs_ = slice(fc * 128, (fc + 1) * 128)
                    jobs[("ma", th, fc)] = ws.add(wmv[:, :, cs_], 128)
                    jobs[("ga", th, fc)] = ws.add(guv[:, :, cs_], 128)
                    jobs[("mb", th, fc)] = ws.add(wmv[:, :, D + fc * 128:D + (fc + 1) * 128], 128)
                    jobs[("gb", th, fc)] = ws.add(nuv[:, :, cs_], 128)
                for fc in range(16):
                    jobs[("wo", th, fc)] = ws.add(wov[:, :, fc * 128:(fc + 1) * 128], 128)
            for th in range(2):
                sl = slice(th * 512, (th + 1) * 512)
                for c in range(8):
                    k.dma("pool", oa_t[c].ap, oav[:, c, sl], reads=[toaT], writes=[oa_t[c]])
                    k.dma("pool", ob_t[c].ap, obv[:, c, sl], reads=[tobT], writes=[ob_t[c]])
                for fc in range(16):
                    parts = []
                    for nm_m, nm_g, src in (("ma", "ga", oa_t), ("mb", "gb", ob_t)):
                        tm_, vm = ws.get(jobs[(nm_m, th, fc)])
                        tg_, vg = ws.get(jobs[(nm_g, th, fc)])
                        pm = pp.get(); py = pp.get()
                        for kc in range(16):
                            k.op("pe", lambda h: h.matmul(pm.ap, vm[:, kc, :], hT[kc].ap[:, sl], start=(kc == 0), stop=(kc == 15)),
                                 reads=[tm_, hT[kc]], writes=[pm], pe_accum=True)
                        for c in range(8):
                            k.op("pe", lambda h: h.matmul(py.ap, vg[:, c, :], src[c].ap, start=(c == 0), stop=(c == 7)),
                                 reads=[tg_, src[c]], writes=[py], pe_accum=True)
                        sg = sg_ts[len(parts)]
                        k.op("act", lambda h: h.activation(sg.ap, pm.ap, AF.Sigmoid), reads=[pm], writes=[sg])
                        k.op("dve", lambda h: h.tensor_tensor(sg.ap, sg.ap, py.ap, ALU.mult), reads=[sg, py], writes=[sg])
                        parts.append(sg)
                    k.op("dve", lambda h: h.tensor_tensor(mer_t[fc].ap, parts[0].ap, parts[1].ap, ALU.add),
                         reads=parts, writes=[mer_t[fc]])
                for fc2 in range(16):
                    tw, vw = ws.get(jobs[("wo", th, fc2)])
                    pz = pp.get()
                    for fc in range(16):
                        k.op("pe", lambda h: h.matmul(pz.ap, vw[:, fc, :], mer_t[fc].ap, start=(fc == 0), stop=(fc == 15)),
                             reads=[tw, mer_t[fc]], writes=[pz], pe_accum=True)
                    k.op("dve", lambda h: h.scalar_tensor_tensor(xres[fc2].ap[:, sl], pz.ap, m(5)[:, fc2:fc2 + 1],
                                                                 xres[fc2].ap[:, sl], ALU.mult, ALU.add),
                         reads=[pz, modS, xres[fc2]], writes=[xres[fc2]])
            k.barrier()
            emit_norm_mod(k, pp, xres, ones_bf, sq_ts, rstd, tmp_ts, A3.ap, m(6), lambda kc: (hT[kc], hT[kc].ap),
                          extra_reads=[A3, modS])
            emit_ffn(k, pp, ws, xres, hT, aT, sil_ts, wg2, wu2, wd2, hg3.ap, hg3)
            tout = T(outT)

            def post(kc, t_):
                k.dma("sp", outT[kc * 128:(kc + 1) * 128, :], t_.ap, reads=[t_], writes=[tout])
            emit_norm_mod(k, pp, xres, ones_bf, sq_ts, rstd, tmp_ts, nft.ap, zer.ap, lambda kc: (None, None),
                          post=post, extra_reads=[nft, zer])
            k.barrier()
        k.stack = st
        k.finish()
        print("FUSED inst", k.n_inst, "waits", k.n_wait)
    return nc


def fused_inputs(inp, c):
    b, r = c // 4, c % 4
    pad = (3 - r) * NT
    nreal = S - pad
    xTp = np.zeros((D, S), np.float32)
    xTp[:, pad:] = inp["x"][b, :nreal, :].T
    valid = (np.arange(S) >= pad).astype(np.float32)
    w = inp["w_in"][0]
    wba = np.ascontiguousarray(np.stack([w[:, col] for hd in range(8) for col in (3072 + hd, 3080 + hd)], axis=1))
    cwf = inp["gdn_conv_w"][0]
    convw = np.stack([cwf[sec * 1024 + hd * 128: sec * 1024 + (hd + 1) * 128] for sec in range(3) for hd in range(8)], axis=1)
    bc = lambda v: np.ascontiguousarray(np.broadcast_to(np.asarray(v, np.float32)[None, :], (128, len(v))))
    ii = np.arange(64)
    posn = np.maximum(np.arange(S) - pad, 0).astype(np.float32)
    inv_freq = (np.float32(500000.0) ** (-np.arange(0, 32, 2, dtype=np.float32) / np.float32(32))).astype(np.float32)
    ang = (posn[:, None] * inv_freq[None, :]).astype(np.float32)
    cos, sin = np.cos(ang).astype(np.float32), np.sin(ang).astype(np.float32)
    cosT = np.ones((128, S), np.float32); sinT = np.zeros((128, S), np.float32)
    cosT[0:16] = cos.T; cosT[16:32] = cos.T; sinT[0:16] = sin.T; sinT[16:32] = sin.T
    R = np.zeros((128, 128), np.float32)
    for d_ in range(16):
        R[d_, d_ + 16] = -1.0
        R[d_ + 16, d_] = 1.0
    n = np.arange(256); jb = np.arange(64); t = np.arange(S - NT, S); kk = np.arange(128); q = np.arange(512)
    gsel = ((n[:, None] // 4 == jb[None, :]) & (n[:, None] < 255)).astype(np.float32)
    cmask = ((16 * n[:, None] + 31 <= t[None, :]) & (n[:, None] < 255) & (16 * n[:, None] >= pad)).astype(np.float32)
    jb0 = pad // 64
    vis = (64 * jb[None, :] <= t[:, None]) & (jb[None, :] >= jb0)
    tb_ = t[:, None] // 64
    forced = (jb[None, :] == jb0) | (jb[None, :] == tb_) | (jb[None, :] == tb_ - 1)
    bmask = np.where(vis, np.where(forced, 1e3, 0.0), -1e30).astype(np.float32)
    ex = (np.arange(S)[None, :] // 64 == jb[:, None]).astype(np.float32)
    caus = np.stack([((128 * dd + kk[:, None]) <= q[None, :]) for dd in range(4)], axis=1).astype(np.float32)
    band = np.stack([((q[None, :] - (128 * dd + kk[:, None]) >= 0) & (q[None, :] - (128 * dd + kk[:, None]) < 512))
                     for dd in range(-4, 4)], axis=1).astype(np.float32)
    ada_b = np.ascontiguousarray(inp["ada_b"][0].reshape(144, 128).T)
    return {
        "xTp": xTp, "cb": _pc(inp["c"][b]), "ada_w": inp["ada_w"][0], "ada_b": ada_b,
        "n1": _pc(inp["ffn1_norm"][0]), "n2": _pc(inp["mix_norm"][0]), "n3": _pc(inp["ffn2_norm"][0]), "nf": _pc(inp["final_norm"]),
        "wg1": inp["ffn1_w_gate"][0], "wu1": inp["ffn1_w_up"][0], "wd1": inp["ffn1_w_down"][0],
        "wg2": inp["ffn2_w_gate"][0], "wu2": inp["ffn2_w_up"][0], "wd2": inp["ffn2_w_down"][0],
        "w_in": w, "wba": wba, "convw": np.ascontiguousarray(convw.astype(np.float32)),
        "alog": bc(inp["gdn_a_log"][0]), "dtb": bc(inp["gdn_dt_bias"][0]), "gnw": bc(inp["gdn_norm_w"][0]),
        "ident": np.eye(128, dtype=np.float32), "tri": (ii[:, None] <= ii[None, :]).astype(np.float32),
        "ms": np.where(ii[:, None] > ii[None, :], 0.0, 1e4).astype(np.float32),
        "mc": np.where(ii[None, :] >= ii[:, None], 0.0, -1e4).astype(np.float32),
        "validT": np.ascontiguousarray(np.broadcast_to(valid[None, :], (128, S))),
        "valid_tm": np.ascontiguousarray(valid.reshape(NCH, 64).T), "validk": np.ascontiguousarray(valid.reshape(32, 128).T),
        "cosT": cosT, "sinT": sinT, "rT": np.ascontiguousarray(R.T),
        "w1k": inp["cmp_k_w1"][0], "w1v": inp["cmp_v_w1"][0],
        "b1k": np.ascontiguousarray(inp["cmp_k_b1"][0][:, None]), "b1v": np.ascontiguousarray(inp["cmp_v_b1"][0][:, None]),
        "w2k": inp["cmp_k_w2"][0], "w2v": inp["cmp_v_w2"][0],
        "posk": np.ascontiguousarray(inp["cmp_pos_k"][0].T), "posv": np.ascontiguousarray(inp["cmp_pos_v"][0].T),
        "gsel": gsel, "cmask": np.ascontiguousarray(cmask), "bmask": np.ascontiguousarray(bmask), "ex": ex,
        "caus": np.ascontiguousarray(caus), "band": np.ascontiguousarray(band),
        "gup": inp["gdn_w_up"][0], "nup": inp["nsa_w_up"][0], "wo": inp["w_out"][0],
    }


def _pc(v):
    return np.ascontiguousarray(np.asarray(v, np.float32).reshape(16, 128).T)


def run_l1(inp):
    nc = build_l1()
    x = inp["x"]
    ada_b = np.ascontiguousarray(inp["ada_b"][0].reshape(144, 128).T)
    maps = []
    for c in range(NCORE):
        b, r = c // 4, c % 4
        maps.append({
            "xT": np.ascontiguousarray(x[b, r * NT:(r + 1) * NT, :].T),
            "cb": _pc(inp["c"][b]),
            "ada_w": inp["ada_w"][0], "ada_b": ada_b,
            "n1": _pc(inp["ffn1_norm"][0]), "n2": _pc(inp["mix_norm"][0]),
            "wg": inp["ffn1_w_gate"][0], "wu": inp["ffn1_w_up"][0], "wd": inp["ffn1_w_down"][0],
        })
    res = run_bass_kernel_spmd(nc, maps, core_ids=list(range(NCORE)))
    return res


def kernel(**inputs):
    inp = {k_: np.asarray(v) for k_, v in inputs.items()}
    nc = build_fused()
    maps = [fused_inputs(inp, c) for c in range(NCORE)]
    res = run_bass_kernel_spmd(nc, maps, core_ids=list(range(NCORE))).results
    out = np.empty((2, S, D), np.float32)
    for c in range(NCORE):
        out[c // 4, (c % 4) * NT:(c % 4 + 1) * NT, :] = res[c]["outT"].T
    return out
```

```python
import numpy as np
from contextlib import ExitStack
import concourse.bass as bass
import concourse.mybir as mybir
from concourse.bass_utils import run_bass_kernel_spmd

F32 = mybir.dt.float32
BF16 = mybir.dt.bfloat16
ALU = mybir.AluOpType
AF = mybir.ActivationFunctionType
AX = mybir.AxisListType

D = 2048
DFF = 5632
S = 4096
NT = 1024
EPS = 1e-6
NCORE = 8


class T:
    __slots__ = ("ap", "w", "r", "name")

    def __init__(self, ap, name=""):
        self.ap = ap
        self.w = None
        self.r = {}
        self.name = name


class K:
    def __init__(self, nc, stack, n_dma_sems=40):
        self.nc = nc
        self.stack = stack
        self.engs = {}
        for name, h in (("pe", nc.tensor), ("act", nc.scalar), ("dve", nc.vector),
                        ("pool", nc.gpsimd), ("sp", nc.sync)):
            sem = stack.enter_context(nc.semaphore("s_" + name))
            self.engs[name] = dict(h=h, sem=sem, cnt=0, known={}, name=name)
        self.sems = {e["name"]: e["sem"] for e in self.engs.values()}
        self.dma_sems = []
        self.dma_pool = {"sw": [], "hw": []}
        for i in range(n_dma_sems):
            s = stack.enter_context(nc.semaphore("s_dma%d" % i))
            key = "dma%d" % i
            self.sems[key] = s
            d = dict(key=key, cnt=0)
            self.dma_sems.append(d)
            self.dma_pool["sw" if i < n_dma_sems // 2 else "hw"].append(d)
        self.dma_rr = {"sw": 0, "hw": 0}
        self.n_wait = 0
        self.n_inst = 0
        self._uid = 0

    def sb(self, shape, dtype, name=None):
        self._uid += 1
        return self.stack.enter_context(self.nc.sbuf_tensor(name or "sb%d" % self._uid, shape, dtype))

    def ps(self, shape, dtype, name=None):
        self._uid += 1
        return self.stack.enter_context(self.nc.psum_tensor(name or "ps%d" % self._uid, shape, dtype))

    def _wait(self, e, toks, skip_self=False):
        need = {}
        for tok in toks:
            if tok is None:
                continue
            kk, v = tok
            if need.get(kk, 0) < v:
                need[kk] = v
        for kk, v in need.items():
            if skip_self and kk == e["name"]:
                continue
            if e["known"].get(kk, 0) >= v:
                continue
            e["h"].wait_ge(self.sems[kk], v)
            e["known"][kk] = v
            self.n_wait += 1

    def op(self, eng, fn, reads=(), writes=(), pe_accum=False):
        e = self.engs[eng]
        toks = []
        for t in reads:
            toks.append(t.w)
        for t in writes:
            toks.append(t.w)
            for kk, v in t.r.items():
                toks.append((kk, v))
        self._wait(e, toks, skip_self=pe_accum)
        ins = fn(e["h"])
        e["cnt"] += 1
        ins.then_inc(e["sem"], 1)
        tok = (eng, e["cnt"])
        for t in reads:
            if t.r.get(eng, 0) < e["cnt"]:
                t.r[eng] = e["cnt"]
        for t in writes:
            t.w = tok
            t.r = {}
        self.n_inst += 1
        return ins

    def dma(self, eng, out_ap, in_ap, reads=(), writes=(), **kw):
        e = self.engs[eng]
        kind = "sw" if eng == "pool" else "hw"
        pool_ = self.dma_pool[kind]
        d = pool_[self.dma_rr[kind]]
        self.dma_rr[kind] = (self.dma_rr[kind] + 1) % len(pool_)
        toks = []
        for t in reads:
            toks.append(t.w)
        for t in writes:
            toks.append(t.w)
            toks.extend(t.r.items())
        if d["cnt"] > 0:
            toks.append((d["key"], d["cnt"] * 16))
        self._wait(e, toks)
        ins = e["h"].dma_start(out=out_ap, in_=in_ap, **kw)
        d["cnt"] += 1
        ins.then_inc(self.sems[d["key"]], 16)
        tok = (d["key"], d["cnt"] * 16)
        for t in reads:
            t.r[d["key"]] = d["cnt"] * 16
        for t in writes:
            t.w = tok
            t.r = {}
        self.n_inst += 1
        return tok

    def barrier(self):
        toks = [(e["name"], e["cnt"]) for e in self.engs.values() if e["cnt"] > 0]
        toks += [(d["key"], d["cnt"] * 16) for d in self.dma_sems if d["cnt"] > 0]
        for e in self.engs.values():
            self._wait(e, toks)

    def finish(self):
        toks = [(e["name"], e["cnt"]) for e in self.engs.values() if e["cnt"] > 0]
        toks += [(d["key"], d["cnt"] * 16) for d in self.dma_sems if d["cnt"] > 0]
        self._wait(self.engs["sp"], toks)


class PsumPool:
    def __init__(self, k, n=8, shape=(128, 512), dtype=F32):
        self.tiles = [T(k.ps(list(shape), dtype)[:]) for _ in range(n)]
        self.i = 0

    def get(self):
        t = self.tiles[self.i]
        self.i = (self.i + 1) % len(self.tiles)
        return t


class WStream:
    def __init__(self, k, nbuf, free_elems, depth=None, eng="pool"):
        self.k = k
        self.buf = [k.sb([128, free_elems], BF16) for _ in range(nbuf)]
        self.ts = [T(b[:]) for b in self.buf]
        self.jobs = []
        self.issued = 0
        self.depth = depth if depth is not None else nbuf - 2
        self.eng = eng

    def add(self, src_ap, inner):
        self.jobs.append((src_ap, inner))
        return len(self.jobs) - 1

    def get(self, i):
        upto = min(len(self.jobs), i + 1 + self.depth)
        while self.issued < upto:
            j = self.issued
            src, inner = self.jobs[j]
            t = self.ts[j % len(self.ts)]
            n = src.shape[1] * src.shape[2]
            dst = self.buf[j % len(self.ts)][:, 0:n].rearrange("p (a b) -> p a b", b=inner)
            self.k.dma(self.eng, dst, src, writes=[t])
            self.issued += 1
        t = self.ts[i % len(self.ts)]
        n_a = self.jobs[i][0].shape[1]
        inner = self.jobs[i][1]
        view = self.buf[i % len(self.ts)][:, 0:n_a * inner].rearrange("p (a b) -> p a b", b=inner)
        return t, view


def emit_norm_mod(k, pp, xres, ones_bf, sq_ts, rstd_t, tmp_ts, A_ap, B_ap, out_fn, nt=NT, post=None, extra_reads=()):
    nh = nt // 512
    pss = [pp.get() for _ in range(nh)]
    for kc in range(16):
        sq = sq_ts[kc % len(sq_ts)]
        k.op("act", lambda h: h.activation(sq.ap, xres[kc].ap, AF.Square), reads=[xres[kc]], writes=[sq])
        for th in range(nh):
            k.op("pe", lambda h: h.matmul(pss[th].ap, ones_bf.ap, sq.ap[:, th * 512:(th + 1) * 512],
                                          start=(kc == 0), stop=(kc == 15)),
                 reads=[ones_bf, sq], writes=[pss[th]], pe_accum=True)
    for th in range(nh):
        sl = slice(th * 512, (th + 1) * 512)
        k.op("dve", lambda h: h.tensor_scalar(rstd_t.ap[:, sl], pss[th].ap, 1.0 / D, EPS, ALU.mult, ALU.add),
             reads=[pss[th]], writes=[rstd_t])
    k.op("act", lambda h: h.activation(rstd_t.ap, rstd_t.ap, AF.Sqrt), reads=[rstd_t], writes=[rstd_t])
    k.op("dve", lambda h: h.reciprocal(rstd_t.ap, rstd_t.ap), reads=[rstd_t], writes=[rstd_t])
    for kc in range(16):
        tmp = tmp_ts[kc % len(tmp_ts)]
        k.op("dve", lambda h: h.tensor_tensor(tmp.ap, xres[kc].ap, rstd_t.ap, ALU.mult),
             reads=[xres[kc], rstd_t], writes=[tmp])
        ot, oap = out_fn(kc)
        if ot is None:
            ot, oap = tmp, tmp.ap
        k.op("act", lambda h: h.activation(oap, tmp.ap, AF.Identity, bias=B_ap[:, kc:kc + 1], scale=A_ap[:, kc:kc + 1]),
             reads=[tmp] + list(extra_reads), writes=[ot])
        if post is not None:
            post(kc, ot)


def emit_ffn(k, pp, ws, xres, hT, aT, sil_ts, wg, wu, wd, hg_ap, hg_t, nt=NT):
    nh = nt // 512
    wgv = wg.rearrange("(kc p) n -> p kc n", p=128)
    wuv = wu.rearrange("(kc p) n -> p kc n", p=128)
    wdv = wd.rearrange("(jc p) f -> p jc f", p=128)
    NJ = DFF // 128
    HJ = NJ // 2
    jobs = {}
    for hh in range(2):
        for jj in range(HJ):
            j = hh * HJ + jj
            jobs[("g", j)] = ws.add(wgv[:, :, j * 128:(j + 1) * 128], 128)
            jobs[("u", j)] = ws.add(wuv[:, :, j * 128:(j + 1) * 128], 128)
        for fc in range(16):
            jobs[("d", hh, fc)] = ws.add(wdv[:, hh * HJ:(hh + 1) * HJ, fc * 128:(fc + 1) * 128], 128)
    for hh in range(2):
        for jj in range(HJ):
            j = hh * HJ + jj
            tg, vg = ws.get(jobs[("g", j)])
            tu, vu = ws.get(jobs[("u", j)])
            for th in range(nh):
                sl = slice(th * 512, (th + 1) * 512)
                pg = pp.get()
                pu = pp.get()
                for kc in range(16):
                    k.op("pe", lambda h: h.matmul(pg.ap, vg[:, kc, :], hT[kc].ap[:, sl], start=(kc == 0), stop=(kc == 15)),
                         reads=[tg, hT[kc]], writes=[pg], pe_accum=True)
                for kc in range(16):
                    k.op("pe", lambda h: h.matmul(pu.ap, vu[:, kc, :], hT[kc].ap[:, sl], start=(kc == 0), stop=(kc == 15)),
                         reads=[tu, hT[kc]], writes=[pu], pe_accum=True)
                st = sil_ts[(jj * nh + th) % len(sil_ts)]
                k.op("act", lambda h: h.activation(st.ap, pg.ap, AF.Silu), reads=[pg], writes=[st])
                k.op("dve", lambda h: h.tensor_tensor(aT[jj].ap[:, sl], st.ap, pu.ap, ALU.mult),
                     reads=[st, pu], writes=[aT[jj]])
        for fc in range(16):
            td, vd = ws.get(jobs[("d", hh, fc)])
            for th in range(nh):
                sl = slice(th * 512, (th + 1) * 512)
                py = pp.get()
                for jj in range(HJ):
                    k.op("pe", lambda h: h.matmul(py.ap, vd[:, jj, :], aT[jj].ap[:, sl], start=(jj == 0), stop=(jj == HJ - 1)),
                         reads=[td, aT[jj]], writes=[py], pe_accum=True)
                k.op("dve", lambda h: h.scalar_tensor_tensor(xres[fc].ap[:, sl], py.ap, hg_ap[:, fc:fc + 1],
                                                             xres[fc].ap[:, sl], ALU.mult, ALU.add),
                     reads=[py, hg_t, xres[fc]], writes=[xres[fc]])


def build_l1():
    nc = bass.Bass("TRN2", target_bir_lowering=False)
    dt = lambda n, s, kind: nc.dram_tensor(n, s, F32, kind=kind).ap()
    xT = dt("xT", [D, NT], "ExternalInput")
    cb = dt("cb", [128, 16], "ExternalInput")
    ada_w = dt("ada_w", [D, 9 * D], "ExternalInput")
    ada_b = dt("ada_b", [128, 144], "ExternalInput")
    n1 = dt("n1", [128, 16], "ExternalInput")
    n2 = dt("n2", [128, 16], "ExternalInput")
    wg = dt("wg", [D, DFF], "ExternalInput")
    wu = dt("wu", [D, DFF], "ExternalInput")
    wd = dt("wd", [DFF, D], "ExternalInput")
    x1T = dt("x1T", [D, NT], "ExternalOutput")
    h2T = dt("h2T", [D, NT], "ExternalOutput")
    modT = dt("modT", [128, 144], "ExternalOutput")
    with ExitStack() as st:
        k = K(nc, st)
        pp = PsumPool(k, 7)
        mps = T(k.ps([128, 144], F32)[:])
        xres_b = k.sb([128, 16, NT], F32)
        xres = [T(xres_b[:, i, :]) for i in range(16)]
        hT_b = k.sb([128, 16, NT], BF16)
        hT = [T(hT_b[:, i, :]) for i in range(16)]
        aT_b = k.sb([128, 22, NT], BF16)
        aT = [T(aT_b[:, i, :]) for i in range(22)]
        ws = WStream(k, 5, 22 * 128)
        small = k.sb([128, 512], F32)
        cact = T(small[:, 0:16]); modS = T(small[:, 16:160]); adab = T(small[:, 160:304])
        n1t = T(small[:, 304:320]); n2t = T(small[:, 320:336])
        A1 = T(small[:, 336:352]); hg1 = T(small[:, 352:368]); A2 = T(small[:, 368:384])
        ones_b = k.sb([128, 128], BF16); ones_bf = T(ones_b[:])
        rstd = T(k.sb([128, NT], F32)[:])
        sq_ts = [T(k.sb([128, NT], BF16)[:]) for _ in range(2)]
        tmp_ts = [T(k.sb([128, NT], F32)[:]) for _ in range(2)]
        sil_ts = [T(k.sb([128, 512], BF16)[:]) for _ in range(2)]
        adw = [k.sb([128, 16, 128], F32) for _ in range(2)]
        adw_t = [T(a[:]) for a in adw]

        k.op("dve", lambda h: h.memset(ones_bf.ap, 1.0), writes=[ones_bf])
        k.dma("sp", cact.ap, cb, writes=[cact])
        k.dma("sp", adab.ap, ada_b, writes=[adab])
        k.dma("sp", n1t.ap, n1, writes=[n1t])
        k.dma("sp", n2t.ap, n2, writes=[n2t])
        for kc in range(16):
            k.dma("act", xres[kc].ap, xT[kc * 128:(kc + 1) * 128, :], writes=[xres[kc]])
        k.op("act", lambda h: h.activation(cact.ap, cact.ap, AF.Silu), reads=[cact], writes=[cact])
        adv = ada_w.rearrange("(kc p) n -> p kc n", p=128)
        for g in range(144):
            wt = adw_t[g % 2]
            k.dma("sp", wt.ap, adv[:, :, g * 128:(g + 1) * 128], writes=[wt])
            for kc in range(16):
                k.op("pe", lambda h: h.matmul(mps.ap[:, g:g + 1], adw[g % 2][:, kc, :],
                                              cact.ap[:, kc:kc + 1], start=(kc == 0), stop=(kc == 15)),
                     reads=[wt, cact], writes=[mps], pe_accum=True)
        k.op("dve", lambda h: h.tensor_tensor(modS.ap, mps.ap, adab.ap, ALU.add), reads=[mps, adab], writes=[modS])
        tmod = T(modT)
        k.dma("sp", modT, modS.ap, reads=[modS], writes=[tmod])
        m = lambda j: modS.ap[:, j * 16:(j + 1) * 16]
        k.op("dve", lambda h: h.scalar_tensor_tensor(A1.ap, m(1), 1.0, n1t.ap, ALU.add, ALU.mult), reads=[modS, n1t], writes=[A1])
        k.op("dve", lambda h: h.tensor_scalar(hg1.ap, m(2), 0.5, None, ALU.mult), reads=[modS], writes=[hg1])
        k.op("dve", lambda h: h.scalar_tensor_tensor(A2.ap, m(4), 1.0, n2t.ap, ALU.add, ALU.mult), reads=[modS, n2t], writes=[A2])
        emit_norm_mod(k, pp, xres, ones_bf, sq_ts, rstd, tmp_ts, A1.ap, m(0), lambda kc: (hT[kc], hT[kc].ap))
        emit_ffn(k, pp, ws, xres, hT, aT, sil_ts, wg, wu, wd, hg1.ap, hg1)
        tx1 = T(x1T)
        for kc in range(16):
            k.dma("sp", x1T[kc * 128:(kc + 1) * 128, :], xres[kc].ap, reads=[xres[kc]], writes=[tx1])
        th2 = T(h2T)
        nh = NT // 512
        pss = [pp.get() for _ in range(nh)]
        for kc in range(16):
            sq = sq_ts[kc % 2]
            k.op("act", lambda h: h.activation(sq.ap, xres[kc].ap, AF.Square), reads=[xres[kc]], writes=[sq])
            for t_ in range(nh):
                k.op("pe", lambda h: h.matmul(pss[t_].ap, ones_bf.ap, sq.ap[:, t_ * 512:(t_ + 1) * 512],
                                              start=(kc == 0), stop=(kc == 15)),
                     reads=[ones_bf, sq], writes=[pss[t_]], pe_accum=True)
        for t_ in range(nh):
            sl = slice(t_ * 512, (t_ + 1) * 512)
            k.op("dve", lambda h: h.tensor_scalar(rstd.ap[:, sl], pss[t_].ap, 1.0 / D, EPS, ALU.mult, ALU.add),
                 reads=[pss[t_]], writes=[rstd])
        k.op("act", lambda h: h.activation(rstd.ap, rstd.ap, AF.Sqrt), reads=[rstd], writes=[rstd])
        k.op("dve", lambda h: h.reciprocal(rstd.ap, rstd.ap), reads=[rstd], writes=[rstd])
        for kc in range(16):
            tmp = tmp_ts[kc % 2]
            k.op("dve", lambda h: h.tensor_tensor(tmp.ap, xres[kc].ap, rstd.ap, ALU.mult), reads=[xres[kc], rstd], writes=[tmp])
            o = tmp
            k.op("act", lambda h: h.activation(o.ap, tmp.ap, AF.Identity, bias=m(3)[:, kc:kc + 1], scale=A2.ap[:, kc:kc + 1]),
                 reads=[tmp, modS, A2], writes=[o])
            k.dma("sp", h2T[kc * 128:(kc + 1) * 128, :], o.ap, reads=[o], writes=[th2])
        k.finish()
        print("L1 inst", k.n_inst, "waits", k.n_wait)
    return nc


def build_l3():
    nc = bass.Bass("TRN2", target_bir_lowering=False)
    dt = lambda n, s, kind="ExternalInput": nc.dram_tensor(n, s, F32, kind=kind).ap()
    x1T = dt("x1T", [D, NT]); h2T = dt("h2T", [D, NT])
    oaT = dt("oaT", [1024, NT]); obT = dt("obT", [1024, NT])
    modT = dt("modT", [128, 144]); n3 = dt("n3", [128, 16]); nf = dt("nf", [128, 16])
    wmg = dt("wmg", [D, 2 * D]); gup = dt("gup", [1024, D]); nup = dt("nup", [1024, D]); wo = dt("wo", [D, D])
    wg = dt("wg", [D, DFF]); wu = dt("wu", [D, DFF]); wd = dt("wd", [DFF, D])
    outT = dt("outT", [D, NT], "ExternalOutput")
    with ExitStack() as st:
        k = K(nc, st)
        pp = PsumPool(k, 8)
        xres_b = k.sb([128, 16, NT], F32)
        xres = [T(xres_b[:, i, :]) for i in range(16)]
        hT_b = k.sb([128, 16, NT], BF16)
        hT = [T(hT_b[:, i, :]) for i in range(16)]
        aT_b = k.sb([128, 22, NT], BF16)
        aT = [T(aT_b[:, i, :]) for i in range(22)]
        ws = WStream(k, 5, 22 * 128)
        small = k.sb([128, 512], F32)
        modS = T(small[:, 0:144]); n3t = T(small[:, 144:160]); nft = T(small[:, 160:176])
        A3 = T(small[:, 176:192]); hg3 = T(small[:, 192:208]); zer = T(small[:, 208:224])
        ones_b = k.sb([128, 128], BF16); ones_bf = T(ones_b[:])
        rstd = T(k.sb([128, NT], F32)[:])
        sq_ts = [T(k.sb([128, NT], BF16)[:]) for _ in range(2)]
        tmp_ts = [T(k.sb([128, NT], F32)[:]) for _ in range(2)]
        sil_ts = [T(k.sb([128, 512], BF16)[:]) for _ in range(2)]
        sg_ts = [T(k.sb([128, 512], F32)[:]) for _ in range(2)]
        m = lambda j: modS.ap[:, j * 16:(j + 1) * 16]

        k.op("dve", lambda h: h.memset(ones_bf.ap, 1.0), writes=[ones_bf])
        k.op("dve", lambda h: h.memset(zer.ap, 0.0), writes=[zer])
        k.dma("sp", modS.ap, modT, writes=[modS])
        k.dma("sp", n3t.ap, n3, writes=[n3t])
        k.dma("sp", nft.ap, nf, writes=[nft])
        for kc in range(16):
            k.dma("pool", hT[kc].ap, h2T[kc * 128:(kc + 1) * 128, :], writes=[hT[kc]])
        for kc in range(16):
            k.dma("act", xres[kc].ap, x1T[kc * 128:(kc + 1) * 128, :], writes=[xres[kc]])
        k.op("dve", lambda h: h.scalar_tensor_tensor(A3.ap, m(7), 1.0, n3t.ap, ALU.add, ALU.mult), reads=[modS, n3t], writes=[A3])
        k.op("dve", lambda h: h.tensor_scalar(hg3.ap, m(8), 0.5, None, ALU.mult), reads=[modS], writes=[hg3])
        wmv = wmg.rearrange("(kc p) n -> p kc n", p=128)
        guv = gup.rearrange("(kc p) n -> p kc n", p=128)
        nuv = nup.rearrange("(kc p) n -> p kc n", p=128)
        wov = wo.rearrange("(kc p) n -> p kc n", p=128)
        oav = oaT.rearrange("(c p) t -> p c t", p=128)
        obv = obT.rearrange("(c p) t -> p c t", p=128)
        mer_t = [T(aT_b[:, i // 2, (i % 2) * 512:(i % 2 + 1) * 512]) for i in range(16)]
        oa_t = [T(aT_b[:, 8 + i // 2, (i % 2) * 512:(i % 2 + 1) * 512]) for i in range(8)]
        ob_t = [T(aT_b[:, 12 + i // 2, (i % 2) * 512:(i % 2 + 1) * 512]) for i in range(8)]
        jobs = {}
        for th in range(2):
            for fc in range(16):
                cs = slice(fc * 128, (fc + 1) * 128)
                jobs[("ma", th, fc)] = ws.add(wmv[:, :, cs], 128)
                jobs[("ga", th, fc)] = ws.add(guv[:, :, cs], 128)
                jobs[("mb", th, fc)] = ws.add(wmv[:, :, D + fc * 128:D + (fc + 1) * 128], 128)
                jobs[("gb", th, fc)] = ws.add(nuv[:, :, cs], 128)
            for fc in range(16):
                jobs[("wo", th, fc)] = ws.add(wov[:, :, fc * 128:(fc + 1) * 128], 128)
        for th in range(2):
            sl = slice(th * 512, (th + 1) * 512)
            for c in range(8):
                k.dma("pool", oa_t[c].ap, oav[:, c, sl], writes=[oa_t[c]])
                k.dma("pool", ob_t[c].ap, obv[:, c, sl], writes=[ob_t[c]])
            for fc in range(16):
                parts = []
                for nm_m, nm_g, src in (("ma", "ga", oa_t), ("mb", "gb", ob_t)):
                    tm, vm = ws.get(jobs[(nm_m, th, fc)])
                    tg, vg = ws.get(jobs[(nm_g, th, fc)])
                    pm = pp.get(); py = pp.get()
                    for kc in range(16):
                        k.op("pe", lambda h: h.matmul(pm.ap, vm[:, kc, :], hT[kc].ap[:, sl], start=(kc == 0), stop=(kc == 15)),
                             reads=[tm, hT[kc]], writes=[pm], pe_accum=True)
                    for c in range(8):
                        k.op("pe", lambda h: h.matmul(py.ap, vg[:, c, :], src[c].ap, start=(c == 0), stop=(c == 7)),
                             reads=[tg, src[c]], writes=[py], pe_accum=True)
                    sg = sg_ts[len(parts)]
                    k.op("act", lambda h: h.activation(sg.ap, pm.ap, AF.Sigmoid), reads=[pm], writes=[sg])
                    k.op("dve", lambda h: h.tensor_tensor(sg.ap, sg.ap, py.ap, ALU.mult), reads=[sg, py], writes=[sg])
                    parts.append(sg)
                k.op("pool", lambda h: h.tensor_tensor(mer_t[fc].ap, parts[0].ap, parts[1].ap, ALU.add),
                     reads=parts, writes=[mer_t[fc]])
            for fc2 in range(16):
                tw, vw = ws.get(jobs[("wo", th, fc2)])
                pz = pp.get()
                for fc in range(16):
                    k.op("pe", lambda h: h.matmul(pz.ap, vw[:, fc, :], mer_t[fc].ap, start=(fc == 0), stop=(fc == 15)),
                         reads=[tw, mer_t[fc]], writes=[pz], pe_accum=True)
                k.op("dve", lambda h: h.scalar_tensor_tensor(xres[fc2].ap[:, sl], pz.ap, m(5)[:, fc2:fc2 + 1],
                                                             xres[fc2].ap[:, sl], ALU.mult, ALU.add),
                     reads=[pz, modS, xres[fc2]], writes=[xres[fc2]])
        k.barrier()
        emit_norm_mod(k, pp, xres, ones_bf, sq_ts, rstd, tmp_ts, A3.ap, m(6), lambda kc: (hT[kc], hT[kc].ap),
                      extra_reads=[A3, modS])
        emit_ffn(k, pp, ws, xres, hT, aT, sil_ts, wg, wu, wd, hg3.ap, hg3)
        tout = T(outT)

        def post(kc, t):
            k.dma("sp", outT[kc * 128:(kc + 1) * 128, :], t.ap, reads=[t], writes=[tout])
        emit_norm_mod(k, pp, xres, ones_bf, sq_ts, rstd, tmp_ts, nft.ap, zer.ap, lambda kc: (None, None),
                      post=post, extra_reads=[nft, zer])
        k.finish()
        print("L3 inst", k.n_inst, "waits", k.n_wait)
    return nc


def run_l3(inp, x1T_l, h2T_l, modT_l, oaT_l, obT_l):
    nc = build_l3()
    wmg = np.ascontiguousarray(inp["w_in"][0][:, -2 * D:])
    maps = []
    for c in range(NCORE):
        maps.append({
            "x1T": x1T_l[c], "h2T": h2T_l[c], "oaT": oaT_l[c], "obT": obT_l[c], "modT": modT_l[c],
            "n3": _pc(inp["ffn2_norm"][0]), "nf": _pc(inp["final_norm"]),
            "wmg": wmg, "gup": inp["gdn_w_up"][0], "nup": inp["nsa_w_up"][0], "wo": inp["w_out"][0],
            "wg": inp["ffn2_w_gate"][0], "wu": inp["ffn2_w_up"][0], "wd": inp["ffn2_w_down"][0],
        })
    return run_bass_kernel_spmd(nc, maps, core_ids=list(range(NCORE)))

GC = 64
NCH = S // GC


def build_l2(do_nsa=True, do_gdn=True, hp_static=0, stage=99, heads=(0, 1)):
    nc = bass.Bass("TRN2", target_bir_lowering=False)
    dt = lambda n, s, kind="ExternalInput": nc.dram_tensor(n, s, F32, kind=kind).ap()
    h2T = dt("h2T", [D, S])
    wfm = dt("wfm", [D, 16 * 128])
    wtm = dt("wtm", [D, 272])
    convw = dt("convw", [128, 6, 4])
    alog = dt("alog", [128, 2]); dtb = dt("dtb", [128, 2])
    gnw = dt("gnw", [128, 128])
    ident_d = dt("ident", [128, 128]); tri_d = dt("tri", [64, 64])
    ms_d = dt("ms", [64, 64]); mc_d = dt("mc", [64, 64])
    oa = dt("oa", [S, 256], "ExternalOutput")
    ob = dt("ob", [S, 256], "ExternalOutput")
    nsa_in = (dt("cosT", [128, S]), dt("sinT", [128, S]), dt("rT", [128, 128]),
              dt("w1k", [4096, 128]), dt("w1v", [4096, 128]), dt("b1k", [128, 1]), dt("b1v", [128, 1]),
              dt("w2k", [128, 128]), dt("w2v", [128, 128]), dt("posk", [128, 32]), dt("posv", [128, 32]),
              dt("gsel", [256, 64]), dt("cmask", [256, S]), dt("bmask", [S, 64]), dt("ex", [64, S]),
              dt("caus", [128, 4, 512]), dt("band", [128, 8, 512]))
    with ExitStack() as st:
        k = K(nc, st)
        cs = k.sb([128, 1024], F32)
        ident = T(cs[:, 0:128]); tri = T(cs[0:64, 128:192]); ms = T(cs[0:64, 192:256]); mc = T(cs[0:64, 256:320])
        ones = T(cs[:, 320:448]); cw = T(cs[:, 448:472]); alg = T(cs[:, 472:474]); dtbt = T(cs[:, 474:476])
        gnwt = T(cs[:, 512:640])
        k.dma("sp", ident.ap, ident_d, writes=[ident]); k.dma("sp", tri.ap, tri_d, writes=[tri])
        k.dma("sp", ms.ap, ms_d, writes=[ms]); k.dma("sp", mc.ap, mc_d, writes=[mc])
        k.dma("sp", cw.ap, convw.rearrange("p a b -> p (a b)"), writes=[cw])
        k.dma("sp", alg.ap, alog, writes=[alg]); k.dma("sp", dtbt.ap, dtb, writes=[dtbt])
        k.dma("sp", gnwt.ap, gnw, writes=[gnwt])
        k.op("dve", lambda h: h.memset(ones.ap, 1.0), writes=[ones])
        k.op("act", lambda h: h.activation(alg.ap, alg.ap, AF.Exp), reads=[alg], writes=[alg])
        k.op("dve", lambda h: h.tensor_scalar(alg.ap, alg.ap, -1.0, None, ALU.mult), reads=[alg], writes=[alg])
        toa = T(oa)
        h2v = h2T.rearrange("(kc p) t -> p kc t", p=128)
        wfv = wfm.rearrange("(kc p) n -> p kc n", p=128)
        wtv = wtm.rearrange("(kc p) n -> p kc n", p=128)
        with ExitStack() as pst:
            k.stack = pst
            pp = PsumPool(k, 8)
            for hl in (heads if do_gdn else ()):
                with ExitStack() as ph:
                    k.stack = ph
                    emit_gdn_head(k, pp, hl, h2v, wfv, wtv, ident, tri, ms, mc, ones, cw, alg, dtbt, gnwt, oa, toa, stage)
                    k.barrier()
                k.stack = pst
            k.barrier()
        k.stack = st
        if do_nsa:
            with ExitStack() as ph:
                k.stack = ph
                emit_nsa(k, hp_static, h2v, wfv, wtv, ident, ones, ob, nsa_in)
                k.barrier()
            k.stack = st
        k.finish()
        print("L2 inst", k.n_inst, "waits", k.n_wait)
    return nc


def emit_gdn_head(k, pp, hl, h2v, wfv, wtv, ident, tri, ms, mc, ones, cw, alg, dtbt, gnwt, oa, toa, stage=99, fz=None):
    qT_b = k.sb([128, S], F32); kT_b = k.sb([128, S], F32)
    qT = [T(qT_b[:, b * 512:(b + 1) * 512]) for b in range(8)]
    kT = [T(kT_b[:, b * 512:(b + 1) * 512]) for b in range(8)]
    gz0 = 0 if fz is None else fz["c_first"]
    ktm_b = k.sb([64, NCH, 128], F32); vtm_b = k.sb([64, NCH, 128], F32); gz_b = k.sb([64, NCH - gz0, 128], F32)
    ktm = [T(ktm_b[:, c, :]) for c in range(NCH)]
    vtm = [T(vtm_b[:, c, :]) for c in range(NCH)]
    gz = [None] * gz0 + [T(gz_b[:, c, :]) for c in range(NCH - gz0)]
    gba_b = k.sb([64, 2, NCH], F32); gba = T(gba_b[:])
    hb_b = [k.sb([128, 16, 512], BF16) for _ in range(1)]
    hb = [T(b[:]) for b in hb_b]
    wq_b = k.sb([128, 3, 16, 128], BF16); wq = T(wq_b[:])
    wt_b = k.sb([128, 16, 130], BF16); wt = T(wt_b[:])
    cst_b = [k.sb([128, 3 + 512], F32) for _ in range(3)]
    cst = [T(b[:]) for b in cst_b]
    acc_ts = [T(k.sb([128, 512], F32)[:]) for _ in range(2)]
    sil_ts = [T(k.sb([128, 512], F32)[:]) for _ in range(2)]
    sq_t = T(k.sb([128, 512], F32)[:])
    rn_t = T(k.sb([128, 512], F32)[:])
    if fz is None:
        chs = (hl, 2 + hl, 4 + hl)
        for i, ch in enumerate(chs):
            k.dma("pool", wq_b[:, i, :, :], wfv[:, :, ch * 128:(ch + 1) * 128], writes=[wq])
        k.dma("pool", wt_b[:, :, :], wtv[:, :, hl * 130:(hl + 1) * 130], writes=[wt])
        sc_col = hl
    else:
        hd = fz["hd"]
        chs = (hd, 8 + hd, 16 + hd)
        for i in range(3):
            k.dma("pool", wq_b[:, i, :, :], fz["w_in_v"][:, :, i * 1024 + hd * 128:i * 1024 + (hd + 1) * 128], writes=[wq])
        k.dma("pool", wt_b[:, :, 0:128], fz["w_in_v"][:, :, 3088 + hd * 128:3088 + (hd + 1) * 128], writes=[wt])
        k.dma("pool", wt_b[:, :, 128:130], fz["wba_v"][:, :, 2 * hd:2 * hd + 2], writes=[wt])
        sc_col = hd
        vb_t = [T(k.sb([128, 512], F32)[:]) for _ in range(2)]
        vtm_t = T(k.sb([64, NCH], F32)[:])
        k.dma("sp", vtm_t.ap, fz["valid_tm"], writes=[vtm_t])
        ofT_t = [T(k.sb([128, 64], F32)[:]) for _ in range(4)]
    for i in range(3):
        k.op("dve", lambda h: h.memset(cst_b[i][:, 0:3], 0.0), writes=[cst[i]])
    for tb in range(8 if stage > 0.15 else 0):
        hbt = hb[0]; hbb = hb_b[0]
        k.dma("pool", hbt.ap, h2v[:, :, tb * 512:(tb + 1) * 512], writes=[hbt])
        if fz is not None:
            k.dma("sp", vb_t[tb % 2].ap, fz["validT"][:, tb * 512:(tb + 1) * 512], writes=[vb_t[tb % 2]])
        for i in range(3):
            ch = chs[i]
            ps = pp.get()
            for kc in range(16):
                k.op("pe", lambda h: h.matmul(ps.ap, wq_b[:, i, kc, :], hbb[:, kc, :], start=(kc == 0), stop=(kc == 15)),
                     reads=[wq, hbt], writes=[ps], pe_accum=True)
            c_ = cst[i]; cb_ = cst_b[i]
            if tb > 0:
                k.op("act", lambda h: h.activation(cb_[:, 0:3], cb_[:, 512:515], AF.Copy), reads=[c_], writes=[c_])
            if fz is None:
                k.op("act", lambda h: h.activation(cb_[:, 3:515], ps.ap, AF.Copy), reads=[ps, c_], writes=[c_])
            else:
                k.op("dve", lambda h: h.tensor_tensor(cb_[:, 3:515], ps.ap, vb_t[tb % 2].ap, ALU.mult), reads=[ps, c_, vb_t[tb % 2]], writes=[c_])
            if stage < 0.25:
                continue
            acc = acc_ts[i % 2]
            k.op("dve", lambda h: h.tensor_scalar(acc.ap, cb_[:, 0:512], cw.ap[:, ch * 4:ch * 4 + 1], None, ALU.mult),
                 reads=[c_, cw], writes=[acc])
            for j in range(1, 4):
                k.op("dve", lambda h: h.scalar_tensor_tensor(acc.ap, cb_[:, j:j + 512], cw.ap[:, ch * 4 + j:ch * 4 + j + 1],
                                                             acc.ap, ALU.mult, ALU.add),
                     reads=[c_, cw, acc], writes=[acc])
            sl_ = sil_ts[i % 2]
            k.op("act", lambda h: h.activation(sl_.ap, acc.ap, AF.Silu), reads=[acc], writes=[sl_])
            if i < 2:
                k.op("dve", lambda h: h.tensor_tensor(sq_t.ap, sl_.ap, sl_.ap, ALU.mult), reads=[sl_], writes=[sq_t])
                pn = pp.get()
                k.op("pe", lambda h: h.matmul(pn.ap, ones.ap, sq_t.ap, start=True, stop=True), reads=[ones, sq_t], writes=[pn])
                k.op("dve", lambda h: h.tensor_scalar(rn_t.ap, pn.ap, EPS, None, ALU.add), reads=[pn], writes=[rn_t])
                k.op("act", lambda h: h.activation(rn_t.ap, rn_t.ap, AF.Sqrt), reads=[rn_t], writes=[rn_t])
                k.op("dve", lambda h: h.reciprocal(rn_t.ap, rn_t.ap), reads=[rn_t], writes=[rn_t])
                dstT = (qT, kT)[i][tb]
                if i == 0:
                    k.op("dve", lambda h: h.scalar_tensor_tensor(dstT.ap, sl_.ap, 128.0 ** -0.5, rn_t.ap, ALU.mult, ALU.mult),
                         reads=[sl_, rn_t], writes=[dstT])
                else:
                    k.op("dve", lambda h: h.tensor_tensor(dstT.ap, sl_.ap, rn_t.ap, ALU.mult), reads=[sl_, rn_t], writes=[dstT])
            if i >= 1 and stage > 0.35:
                src_t = kT[tb] if i == 1 else sl_
                dst_l = ktm if i == 1 else vtm
                for cc in range(8):
                    c = tb * 8 + cc
                    pt = pp.get()
                    k.op("pe", lambda h: h.transpose(pt.ap[0:64, 0:128], src_t.ap[:, cc * 64:(cc + 1) * 64], ident.ap),
                         reads=[src_t, ident], writes=[pt])
                    k.op("dve", lambda h: h.tensor_copy(dst_l[c].ap, pt.ap[0:64, 0:128]), reads=[pt], writes=[dst_l[c]])
        for cc in range(8 if stage > 0.45 else 0):
            c = tb * 8 + cc
            pz = pp.get()
            for kc in range(16):
                k.op("pe", lambda h: h.matmul(pz.ap[0:64, 0:130], hbb[:, kc, cc * 64:(cc + 1) * 64], wt_b[:, kc, :],
                                              start=(kc == 0), stop=(kc == 15)),
                     reads=[hbt, wt], writes=[pz], pe_accum=True)
            if gz[c] is not None:
                k.op("act", lambda h: h.activation(gz[c].ap, pz.ap[0:64, 0:128], AF.Silu), reads=[pz], writes=[gz[c]])
                k.op("dve", lambda h: h.tensor_tensor(gz[c].ap, gz[c].ap, gnwt.ap[0:64, :], ALU.mult), reads=[gz[c], gnwt], writes=[gz[c]])
                k.op("dve", lambda h: h.tensor_copy(gba_b[:, :, c], pz.ap[0:64, 128:130]), reads=[pz, gz[c]], writes=[gba])
            else:
                k.op("dve", lambda h: h.tensor_copy(gba_b[:, :, c], pz.ap[0:64, 128:130]), reads=[pz], writes=[gba])
    if stage <= 1:
        return
    sm = k.sb([128, 8, NCH], F32)
    beta = T(sm[0:64, 0, :]); g = T(sm[0:64, 1, :]); gcs = T(sm[0:64, 2, :]); egk = T(sm[0:64, 3, :])
    ekd = T(sm[0:64, 4, :]); nbeta = T(sm[0:64, 5, :]); dend = T(sm[:, 6, :]); bk = T(sm[0:64, 7, :])
    k.op("act", lambda h: h.activation(beta.ap, gba_b[:, 0, :], AF.Sigmoid), reads=[gba], writes=[beta])
    if fz is not None:
        k.op("dve", lambda h: h.tensor_tensor(beta.ap, beta.ap, vtm_t.ap, ALU.mult), reads=[beta, vtm_t], writes=[beta])
    k.op("act", lambda h: h.activation(g.ap, gba_b[:, 1, :], AF.Exp, bias=dtbt.ap[0:64, sc_col:sc_col + 1]), reads=[gba, dtbt], writes=[g])
    k.op("dve", lambda h: h.tensor_scalar(g.ap, g.ap, 1.0, None, ALU.add), reads=[g], writes=[g])
    k.op("act", lambda h: h.activation(g.ap, g.ap, AF.Ln), reads=[g], writes=[g])
    k.op("dve", lambda h: h.tensor_scalar(g.ap, g.ap, alg.ap[0:64, sc_col:sc_col + 1], None, ALU.mult), reads=[g, alg], writes=[g])
    pg = pp.get()
    k.op("pe", lambda h: h.matmul(pg.ap[0:64, 0:NCH], tri.ap, g.ap, start=True, stop=True), reads=[tri, g], writes=[pg])
    k.op("dve", lambda h: h.tensor_copy(gcs.ap, pg.ap[0:64, 0:NCH]), reads=[pg], writes=[gcs])
    pl = pp.get()
    k.op("pe", lambda h: h.matmul(pl.ap[:, 0:NCH], ones.ap[0:64, :], g.ap, start=True, stop=True), reads=[ones, g], writes=[pl])
    k.op("act", lambda h: h.activation(dend.ap, pl.ap[:, 0:NCH], AF.Exp), reads=[pl], writes=[dend])
    k.op("dve", lambda h: h.tensor_tensor(ekd.ap, pl.ap[0:64, 0:NCH], gcs.ap, ALU.subtract), reads=[pl, gcs, dend], writes=[ekd])
    k.op("act", lambda h: h.activation(ekd.ap, ekd.ap, AF.Exp), reads=[ekd], writes=[ekd])
    k.op("act", lambda h: h.activation(egk.ap, gcs.ap, AF.Exp), reads=[gcs], writes=[egk])
    k.op("dve", lambda h: h.tensor_tensor(bk.ap, beta.ap, egk.ap, ALU.mult), reads=[beta, egk], writes=[bk])
    k.op("dve", lambda h: h.tensor_scalar(nbeta.ap, beta.ap, -1.0, None, ALU.mult), reads=[beta], writes=[nbeta])
    if stage <= 2:
        return
    S_t = T(k.sb([128, 128], F32)[:])
    k.op("dve", lambda h: h.memset(S_t.ap, 0.0), writes=[S_t])
    zcol = T(k.sb([64, 1], F32)[:])
    k.op("dve", lambda h: h.memset(zcol.ap, 0.0), writes=[zcol])
    class _Sub:
        def __init__(self, tiles):
            self.tiles = tiles; self.i = 0

        def get(self):
            t_ = self.tiles[self.i]; self.i = (self.i + 1) % len(self.tiles)
            return t_
    sub = [_Sub(pp.tiles[0:3]), _Sub(pp.tiles[3:6]), _Sub(pp.tiles[6:8])]
    NB = 4
    def mk(shape):
        return [T(k.sb(shape, F32)[:]) for _ in range(NB)]
    NPP = 6 if stage == 3.5 else 2
    diag = mk([64, 64]); ds = mk([64, 64]); dT = mk([64, 64]); Nm = [mk([64, 64]) for _ in range(NPP)]; Mm = [mk([64, 64]) for _ in range(NPP)]
    Pm = [mk([64, 64]) for _ in range(NPP)]; AT = mk([64, 64]); Vb = mk([64, 128]); Kb = mk([64, 128]); u = mk([64, 128]); wT = mk([128, 64])
    eg = mk([128, 64]); qg = mk([128, 64]); kd = mk([64, 128]); vn = mk([64, 128]); osb = mk([64, 128]); ss = mk([64, 1]); of = mk([64, 128])
    pre = {}

    def precompute(c):
        b = c % NB
        ppl = sub[c % 2]
        tb, off = c // 8, (c % 8) * 64
        kTc = kT[tb].ap[:, off:off + 64]; qTc = qT[tb].ap[:, off:off + 64]
        col = slice(c, c + 1)
        k.op("dve", lambda h: h.tensor_scalar(diag[b].ap, ident.ap[0:64, 0:64], gcs.ap[:, col], None, ALU.mult),
             reads=[ident, gcs], writes=[diag[b]])
        yield
        pb = ppl.get()
        k.op("pe", lambda h: h.matmul(pb.ap[:, 0:64], ones.ap[0:64, :], diag[b].ap, start=True, stop=True),
             reads=[ones, diag[b]], writes=[pb])
        k.op("dve", lambda h: h.scalar_tensor_tensor(ds[b].ap, pb.ap[0:64, 0:64], gcs.ap[:, col], ms.ap, ALU.subtract, ALU.add),
             reads=[pb, gcs, ms], writes=[ds[b]])
        yield
        k.op("act", lambda h: h.activation(ds[b].ap, ds[b].ap, AF.Exp, scale=-1.0), reads=[ds[b]], writes=[ds[b]])
        yield
        if (fz is None) or (c >= fz["c_first"]):
            k.op("dve", lambda h: h.scalar_tensor_tensor(dT[b].ap, pb.ap[0:64, 0:64], gcs.ap[:, col], mc.ap, ALU.subtract, ALU.add),
                 reads=[pb, gcs, mc], writes=[dT[b]])
            yield
            k.op("act", lambda h: h.activation(dT[b].ap, dT[b].ap, AF.Exp), reads=[dT[b]], writes=[dT[b]])
            yield
        need_q = (fz is None) or (c >= fz["c_first"])
        if need_q:
            k.op("act", lambda h: h.activation(eg[b].ap, pb.ap[:, 0:64], AF.Exp), reads=[pb], writes=[eg[b]])
            yield
            k.op("dve", lambda h: h.tensor_tensor(qg[b].ap, qTc, eg[b].ap, ALU.mult), reads=[qT[tb], eg[b]], writes=[qg[b]])
            yield
        pk = ppl.get()
        k.op("pe", lambda h: h.matmul(pk.ap[0:64, 0:64], kTc, kTc, start=True, stop=True), reads=[kT[tb]], writes=[pk])
        N0 = Nm[0][b]
        k.op("dve", lambda h: h.scalar_tensor_tensor(N0.ap, pk.ap[0:64, 0:64], nbeta.ap[:, col], ds[b].ap, ALU.mult, ALU.mult),
             reads=[pk, nbeta, ds[b]], writes=[N0])
        yield
        pm = ppl.get()
        k.op("pe", lambda h: h.transpose(pm.ap[0:64, 0:64], N0.ap, ident.ap[0:64, 0:64]), reads=[N0, ident], writes=[pm])
        M0 = Mm[0][b]
        k.op("dve", lambda h: h.tensor_copy(M0.ap, pm.ap[0:64, 0:64]), reads=[pm], writes=[M0])
        yield
        P0 = Pm[0][b]
        k.op("dve", lambda h: h.tensor_tensor(P0.ap, pm.ap[0:64, 0:64], ident.ap[0:64, 0:64], ALU.add), reads=[pm, ident], writes=[P0])
        yield
        Ncur, Mcur, Pcur = N0, M0, P0
        for s_ in range(1, 6):
            Nn = Nm[s_ % NPP][b]; Mn = Mm[s_ % NPP][b]; Pn = Pm[s_ % NPP][b]
            pn_ = ppl.get()
            k.op("pe", lambda h: h.matmul(pn_.ap[0:64, 0:64], Mcur.ap, Ncur.ap, start=True, stop=True), reads=[Mcur, Ncur], writes=[pn_])
            if s_ < 5:
                pm_ = ppl.get()
                k.op("pe", lambda h: h.matmul(pm_.ap[0:64, 0:64], Ncur.ap, Mcur.ap, start=True, stop=True), reads=[Mcur, Ncur], writes=[pm_])
            k.op("act", lambda h: h.activation(Nn.ap, pn_.ap[0:64, 0:64], AF.Copy), reads=[pn_], writes=[Nn])
            yield
            if s_ < 5:
                k.op("dve", lambda h: h.tensor_copy(Mn.ap, pm_.ap[0:64, 0:64]), reads=[pm_], writes=[Mn])
                yield
            pq = ppl.get()
            k.op("pe", lambda h: h.matmul(pq.ap[0:64, 0:64], Nn.ap, Pcur.ap, start=True, stop=True), reads=[Nn, Pcur], writes=[pq])
            k.op("dve", lambda h: h.tensor_tensor(Pn.ap, pq.ap[0:64, 0:64], Pcur.ap, ALU.add), reads=[pq, Pcur], writes=[Pn])
            yield
            Ncur, Mcur, Pcur = Nn, Mn, Pn
        TT = Pcur
        if need_q:
            pa = ppl.get()
            k.op("pe", lambda h: h.matmul(pa.ap[0:64, 0:64], kTc, qTc, start=True, stop=True), reads=[kT[tb], qT[tb]], writes=[pa])
            k.op("dve", lambda h: h.tensor_tensor(AT[b].ap, pa.ap[0:64, 0:64], dT[b].ap, ALU.mult), reads=[pa, dT[b]], writes=[AT[b]])
            yield
        k.op("act", lambda h: h.activation(Vb[b].ap, vtm[c].ap, AF.Identity, bias=zcol.ap, scale=beta.ap[:, col]), reads=[vtm[c], beta, zcol], writes=[Vb[b]])
        yield
        k.op("act", lambda h: h.activation(Kb[b].ap, ktm[c].ap, AF.Identity, bias=zcol.ap, scale=bk.ap[:, col]), reads=[ktm[c], bk, zcol], writes=[Kb[b]])
        yield
        k.op("act", lambda h: h.activation(kd[b].ap, ktm[c].ap, AF.Identity, bias=zcol.ap, scale=ekd.ap[:, col]), reads=[ktm[c], ekd, zcol], writes=[kd[b]])
        yield
        pu = ppl.get()
        k.op("pe", lambda h: h.matmul(pu.ap[0:64, 0:128], TT.ap, Vb[b].ap, start=True, stop=True), reads=[TT, Vb[b]], writes=[pu])
        k.op("act", lambda h: h.activation(u[b].ap, pu.ap[0:64, 0:128], AF.Copy), reads=[pu], writes=[u[b]])
        yield
        pw = ppl.get()
        k.op("pe", lambda h: h.matmul(pw.ap[:, 0:64], Kb[b].ap, TT.ap, start=True, stop=True), reads=[TT, Kb[b]], writes=[pw])
        k.op("act", lambda h: h.activation(wT[b].ap, pw.ap[:, 0:64], AF.Copy), reads=[pw], writes=[wT[b]])
        yield

    def scan(c):
        b = c % NB
        col = slice(c, c + 1)
        p1 = sub[2].get()
        k.op("pe", lambda h: h.matmul(p1.ap[0:64, 0:128], wT[b].ap, S_t.ap, start=True, stop=True), reads=[wT[b], S_t], writes=[p1])
        k.op("dve", lambda h: h.tensor_tensor(vn[b].ap, u[b].ap, p1.ap[0:64, 0:128], ALU.subtract), reads=[u[b], p1], writes=[vn[b]])
        yield
        want_out = (fz is None) or (c >= fz["c_first"])
        if want_out:
            p2 = sub[2].get()
            k.op("pe", lambda h: h.matmul(p2.ap[0:64, 0:128], qg[b].ap, S_t.ap, start=True, stop=False), reads=[qg[b], S_t], writes=[p2])
            k.op("pe", lambda h: h.matmul(p2.ap[0:64, 0:128], AT[b].ap, vn[b].ap, start=False, stop=True), reads=[AT[b], vn[b]], writes=[p2],
                 pe_accum=True)
        p3 = sub[2].get()
        k.op("pe", lambda h: h.matmul(p3.ap[:, 0:128], kd[b].ap, vn[b].ap, start=True, stop=True), reads=[kd[b], vn[b]], writes=[p3])
        k.op("dve", lambda h: h.scalar_tensor_tensor(S_t.ap, S_t.ap, dend.ap[:, col], p3.ap[:, 0:128], ALU.mult, ALU.add),
             reads=[S_t, dend, p3], writes=[S_t])
        yield
        if not want_out:
            return
        k.op("act", lambda h: h.activation(osb[b].ap, p2.ap[0:64, 0:128], AF.Square, accum_out=ss[b].ap), reads=[p2], writes=[osb[b], ss[b]])
        yield
        k.op("dve", lambda h: h.tensor_scalar(ss[b].ap, ss[b].ap, 1.0 / 128, EPS, ALU.mult, ALU.add), reads=[ss[b]], writes=[ss[b]])
        yield
        k.op("act", lambda h: h.activation(ss[b].ap, ss[b].ap, AF.Sqrt), reads=[ss[b]], writes=[ss[b]])
        yield
        k.op("dve", lambda h: h.reciprocal(ss[b].ap, ss[b].ap), reads=[ss[b]], writes=[ss[b]])
        yield
        k.op("dve", lambda h: h.scalar_tensor_tensor(of[b].ap, p2.ap[0:64, 0:128], ss[b].ap, gz[c].ap, ALU.mult, ALU.mult),
             reads=[p2, ss[b], gz[c]], writes=[of[b]])
        yield
        if fz is None:
            k.dma("sp", oa[c * 64:(c + 1) * 64, hl * 128:(hl + 1) * 128], of[b].ap, reads=[of[b]], writes=[toa])
        else:
            pT_ = sub[2].get()
            k.op("pe", lambda h: h.transpose(pT_.ap[:, 0:64], of[b].ap, ident.ap[0:64, 0:64]), reads=[of[b], ident], writes=[pT_])
            oT_ = ofT_t[b]
            k.op("dve", lambda h: h.tensor_copy(oT_.ap, pT_.ap[:, 0:64]), reads=[pT_], writes=[oT_])
            yield
            lc = c - fz["c_first"]
            k.dma("sp", fz["oaT"][fz["hd"] * 128:(fz["hd"] + 1) * 128, lc * 64:(lc + 1) * 64], oT_.ap, reads=[oT_], writes=[fz["toaT"]])

    def run(g_):
        for _ in g_:
            pass

    def drive(gens):
        gens = list(gens)
        while gens:
            for g_ in list(gens):
                try:
                    next(g_)
                except StopIteration:
                    gens.remove(g_)
    run(precompute(0))
    if stage == 3.5:
        run(scan(0))
        dumps = [(0, 0, qT[0].ap[:, 0:256], [qT[0]]), (128, 0, kT[0].ap[:, 0:256], [kT[0]]),
                 (256, 0, ktm[0].ap, [ktm[0]]), (256, 128, vtm[0].ap, [vtm[0]]),
                 (320, 0, gz[0].ap, [gz[0]]), (320, 128, gba_b[:, 0, :], [gba]), (320, 192, gba_b[:, 1, :], [gba]),
                 (384, 0, beta.ap, [beta]), (384, 64, g.ap, [g]), (384, 128, gcs.ap, [gcs]), (384, 192, egk.ap, [egk]),
                 (448, 0, ekd.ap, [ekd]), (448, 64, nbeta.ap, [nbeta]), (448, 128, bk.ap, [bk]), (448, 192, dend.ap[0:64, :], [dend]),
                 (512, 0, ds[0].ap, [ds[0]]), (512, 64, dT[0].ap, [dT[0]]), (512, 128, Nm[0][0].ap, [Nm[0][0]]), (512, 192, Mm[0][0].ap, [Mm[0][0]]),
                 (576, 0, Pm[5][0].ap, [Pm[5][0]]), (576, 64, AT[0].ap, [AT[0]]), (576, 128, u[0].ap, [u[0]]),
                 (640, 0, wT[0].ap, [wT[0]]), (640, 64, qg[0].ap, [qg[0]]), (640, 128, eg[0].ap, [eg[0]]),
                 (768, 0, kd[0].ap, [kd[0]]), (768, 128, Vb[0].ap, [Vb[0]]),
                 (832, 0, vn[0].ap, [vn[0]]), (832, 128, of[0].ap, [of[0]]), (896, 0, S_t.ap, [S_t]), (896, 128, Kb[0].ap, [Kb[0]])]
        for i_ in range(6):
            dumps.append((1024 + 64 * i_, 0, Nm[i_][0].ap, [Nm[i_][0]]))
            dumps.append((1024 + 64 * i_, 128, Pm[i_][0].ap, [Pm[i_][0]]))
            if i_ < 5:
                dumps.append((1024 + 64 * i_, 64, Mm[i_][0].ap, [Mm[i_][0]]))
        for (r0, c0, ap_, ts_) in dumps:
            k.dma("sp", oa[r0 + 1024:r0 + 1024 + ap_.shape[0], c0:c0 + ap_.shape[1]], ap_, reads=ts_, writes=[toa])
        return
    if stage <= 3:
        return
    run(precompute(1))
    for i_ in range(NCH // 2):
        gens = []
        if 2 * i_ + 2 < NCH:
            gens += [precompute(2 * i_ + 2), precompute(2 * i_ + 3)]

        def scans(i_=i_):
            yield from scan(2 * i_)
            yield from scan(2 * i_ + 1)
        gens.append(scans())
        drive(gens)

def emit_nsa(k, hp, h2v, wfv, wtv, ident, ones, ob, nsa_in):
    (cosT, sinT, rT_d, w1k_d, w1v_d, b1k_d, b1v_d, w2k_d, w2v_d, posk_d, posv_d, gsel_d, cmask_d, bmask_d, ex_d, caus_d, band_d) = nsa_in
    SC = 128.0 ** -0.5
    r_own = (2 * (hp % 2), 2 * (hp % 2) + 1)
    pp = PsumPool(k, 4)
    po = [T(k.ps([128, 512], F32)[:]) for _ in range(4)]
    tob = T(ob)
    qT_b = k.sb([128, 4, S], BF16); qT = T(qT_b[:])
    qr_b = k.sb([128, 2, S], BF16); qr = T(qr_b[:])
    kc_b = k.sb([128, S], BF16); kcT = T(kc_b[:]); vc_b = k.sb([128, S], BF16); vcT = T(vc_b[:])
    ks_b = k.sb([128, S], BF16); ksT = T(ks_b[:]); kw_b = k.sb([128, S], BF16); kwT = T(kw_b[:])
    vs_b = k.sb([128, 32, 130], BF16); vs_tm = T(vs_b[:]); vw_b = k.sb([128, 32, 130], BF16); vw_tm = T(vw_b[:])
    gts_b = k.sb([128, 32, 12], F32); gts = T(gts_b[:])
    imp_b = k.sb([128, 32, 64], F32); imp = T(imp_b[:])
    oacc_b = k.sb([128, 32, 256], F32); oacc = T(oacc_b[:])
    selT_b = k.sb([64, S], BF16); selT = T(selT_b[:])
    k.op("dve", lambda h: h.memset(vs_b[:, :, 128:129], 1.0), writes=[vs_tm])
    k.op("dve", lambda h: h.memset(vw_b[:, :, 128:129], 1.0), writes=[vw_tm])
    k.op("dve", lambda h: h.memset(imp.ap, 0.0), writes=[imp])
    k.op("dve", lambda h: h.memset(oacc.ap, 0.0), writes=[oacc])
    with ExitStack() as ph:
        old = k.stack; k.stack = ph
        hb_b = k.sb([128, 16, 512], BF16); hb = T(hb_b[:])
        ws = WStream(k, 4, 16 * 128)
        wng_b = k.sb([128, 16, 12], BF16); wng = T(wng_b[:])
        k.dma("pool", wng_b[:], wtv[:, :, 260:272], writes=[wng])
        rT_b = k.sb([128, 128], BF16); rT = T(rT_b[:])
        k.dma("pool", rT.ap, rT_d, writes=[rT])
        cs_t = [T(k.sb([128, 512], F32)[:]) for _ in range(2)]
        sn_t = [T(k.sb([128, 512], F32)[:]) for _ in range(2)]
        xb_t = [T(k.sb([128, 512], BF16)[:]) for _ in range(2)]
        t1_t = [T(k.sb([128, 512], F32)[:]) for _ in range(2)]
        t2_t = [T(k.sb([128, 512], F32)[:]) for _ in range(2)]
        jobs = {}
        for tb in range(8):
            for ch in range(10):
                jobs[(tb, ch)] = ws.add(wfv[:, :, (6 + ch) * 128:(7 + ch) * 128], 128)
        nr = 0
        for tb in range(8):
            bs = slice(tb * 512, (tb + 1) * 512)
            k.dma("pool", hb.ap, h2v[:, :, bs], writes=[hb])
            k.dma("sp", cs_t[tb % 2].ap, cosT[:, bs], writes=[cs_t[tb % 2]])
            k.dma("sp", sn_t[tb % 2].ap, sinT[:, bs], writes=[sn_t[tb % 2]])
            for ch in range(10):
                tw, vw_ = ws.get(jobs[(tb, ch)])
                ps = pp.get()
                for kc in range(16):
                    k.op("pe", lambda h: h.matmul(ps.ap, vw_[:, kc, :], hb_b[:, kc, :], start=(kc == 0), stop=(kc == 15)),
                         reads=[tw, hb], writes=[ps], pe_accum=True)
                rope_dst = None
                if ch < 4:
                    k.op("act", lambda h: h.activation(qT_b[:, ch, bs], ps.ap, AF.Copy), reads=[ps], writes=[qT])
                    if ch in r_own:
                        rope_dst = (qr, qr_b[:, r_own.index(ch), bs])
                elif ch == 4:
                    k.op("act", lambda h: h.activation(kc_b[:, bs], ps.ap, AF.Copy), reads=[ps], writes=[kcT])
                elif ch == 5:
                    k.op("act", lambda h: h.activation(vc_b[:, bs], ps.ap, AF.Copy), reads=[ps], writes=[vcT])
                elif ch == 6:
                    rope_dst = (ksT, ks_b[:, bs])
                elif ch == 8:
                    rope_dst = (kwT, kw_b[:, bs])
                else:
                    dst_t, dst_b = (vs_tm, vs_b) if ch == 7 else (vw_tm, vw_b)
                    xf = t1_t[nr % 2]; nr += 1
                    k.op("act", lambda h: h.activation(xf.ap, ps.ap, AF.Copy), reads=[ps], writes=[xf])
                    for tt in range(4):
                        pt = pp.get()
                        k.op("pe", lambda h: h.transpose(pt.ap[:, 0:128], xf.ap[:, tt * 128:(tt + 1) * 128], ident.ap),
                             reads=[xf, ident], writes=[pt])
                        k.op("dve", lambda h: h.tensor_copy(dst_b[:, tb * 4 + tt, 0:128], pt.ap[:, 0:128]), reads=[pt], writes=[dst_t])
                if rope_dst is not None:
                    dt_, dap = rope_dst
                    xb = xb_t[nr % 2]; nr += 1
                    k.op("act", lambda h: h.activation(xb.ap, ps.ap, AF.Copy), reads=[ps], writes=[xb])
                    pr = pp.get()
                    k.op("pe", lambda h: h.matmul(pr.ap, rT.ap, xb.ap, start=True, stop=True), reads=[rT, xb], writes=[pr])
                    t1 = t1_t[nr % 2]; t2 = t2_t[nr % 2]
                    k.op("dve", lambda h: h.tensor_tensor(t1.ap, ps.ap, cs_t[tb % 2].ap, ALU.mult), reads=[ps, cs_t[tb % 2], xb], writes=[t1])
                    k.op("dve", lambda h: h.tensor_tensor(t2.ap, pr.ap, sn_t[tb % 2].ap, ALU.mult), reads=[pr, sn_t[tb % 2]], writes=[t2])
                    k.op("dve", lambda h: h.tensor_tensor(dap, t1.ap, t2.ap, ALU.add), reads=[t1, t2], writes=[dt_])
            for tt in range(4):
                pg = pp.get()
                for kc in range(16):
                    k.op("pe", lambda h: h.matmul(pg.ap[:, 0:12], hb_b[:, kc, tt * 128:(tt + 1) * 128], wng_b[:, kc, :],
                                                  start=(kc == 0), stop=(kc == 15)),
                         reads=[hb, wng], writes=[pg], pe_accum=True)
                k.op("act", lambda h: h.activation(gts_b[:, tb * 4 + tt, :], pg.ap[:, 0:12], AF.Sigmoid), reads=[pg], writes=[gts])
        k.barrier()
        k.stack = old
    kcmp_b = k.sb([128, 256], BF16); kcmp = T(kcmp_b[:])
    vca_b = k.sb([128, 2, 193], BF16); vca = T(vca_b[:])
    k.op("dve", lambda h: h.memset(kcmp.ap, 0.0), writes=[kcmp])
    k.op("dve", lambda h: h.memset(vca.ap, 0.0), writes=[vca])
    k.op("dve", lambda h: h.memset(vca_b[:, :, 128:129], 1.0), writes=[vca])
    k.dma("pool", vca_b[:, :, 129:193], gsel_d.rearrange("(a p) j -> p a j", p=128), writes=[vca])
    with ExitStack() as ph:
        old = k.stack; k.stack = ph
        w1_b = k.sb([128, 32, 128], BF16); w1 = T(w1_b[:])
        w2_b = k.sb([128, 128], BF16); w2 = T(w2_b[:])
        pos_b = k.sb([128, 32], BF16); pos = T(pos_b[:])
        b1_b = k.sb([128, 1], F32); b1 = T(b1_b[:])
        bias_b = k.sb([128, 1], F32); bias = T(bias_b[:])
        hid_b = k.sb([128, 256], BF16); hid = T(hid_b[:])
        for which in range(2):
            w1d, b1d, w2d, posd, src_b, src_t = ((w1k_d, b1k_d, w2k_d, posk_d, kc_b, kcT), (w1v_d, b1v_d, w2v_d, posv_d, vc_b, vcT))[which]
            k.dma("pool", w1.ap, w1d.rearrange("(j d) m -> d j m", d=128), writes=[w1])
            k.dma("pool", w2.ap, w2d, writes=[w2])
            k.dma("pool", pos.ap, posd, writes=[pos])
            k.dma("sp", b1.ap, b1d, writes=[b1])
            pb = pp.get()
            for j in range(32):
                k.op("pe", lambda h: h.matmul(pb.ap[:, 0:1], w1_b[:, j, :], pos_b[:, j:j + 1], start=(j == 0), stop=(j == 31)),
                     reads=[w1, pos], writes=[pb], pe_accum=True)
            k.op("dve", lambda h: h.tensor_tensor(bias.ap, pb.ap[:, 0:1], b1.ap, ALU.add), reads=[pb, b1], writes=[bias])
            ph_ = pp.get()
            for j in range(32):
                k.op("pe", lambda h: h.matmul(ph_.ap[:, 0:255], w1_b[:, j, :], src_b[:, j:j + 16 * 254 + 1:16], start=(j == 0), stop=(j == 31)),
                     reads=[w1, src_t], writes=[ph_], pe_accum=True)
            k.op("dve", lambda h: h.memset(hid.ap, 0.0), writes=[hid])
            k.op("act", lambda h: h.activation(hid_b[:, 0:255], ph_.ap[:, 0:255], AF.Silu, bias=bias.ap), reads=[ph_, bias], writes=[hid])
            if which == 0:
                pk = pp.get()
                k.op("pe", lambda h: h.matmul(pk.ap[:, 0:255], w2.ap, hid_b[:, 0:255], start=True, stop=True), reads=[w2, hid], writes=[pk])
                k.op("act", lambda h: h.activation(kcmp_b[:, 0:255], pk.ap[:, 0:255], AF.Copy), reads=[pk], writes=[kcmp])
            else:
                for nt_ in range(2):
                    pv = pp.get()
                    k.op("pe", lambda h: h.matmul(pv.ap[:, 0:128], hid_b[:, nt_ * 128:(nt_ + 1) * 128], w2.ap, start=True, stop=True),
                         reads=[w2, hid], writes=[pv])
                    k.op("act", lambda h: h.activation(vca_b[:, nt_, 0:128], pv.ap[:, 0:128], AF.Copy), reads=[pv], writes=[vca])
        k.barrier()
        k.stack = old
    e_t = [T(k.sb([128, 512], BF16)[:]) for _ in range(3)]
    p_t = [T(k.sb([128, 512], BF16)[:]) for _ in range(3)]
    mk_t = [T(k.sb([128, 512], BF16)[:]) for _ in range(2)]
    rd_t = [T(k.sb([128, 1], F32)[:]) for _ in range(2)]
    rg_t = [T(k.sb([128, 1], F32)[:]) for _ in range(2)]
    tmpo = [T(k.sb([128, 128], F32)[:]) for _ in range(2)]
    cnt = [0]

    def finish_sub(pacc, qt, r, gate_j, with_imp):
        i = cnt[0] % 2; cnt[0] += 1
        rd = rd_t[i]; rg = rg_t[i]
        k.op("dve", lambda h: h.tensor_scalar(rd.ap, pacc.ap[:, 128:129], 1e-30, None, ALU.max), reads=[pacc], writes=[rd])
        k.op("dve", lambda h: h.reciprocal(rd.ap, rd.ap), reads=[rd], writes=[rd])
        if with_imp:
            k.op("dve", lambda h: h.scalar_tensor_tensor(imp_b[:, qt, :], pacc.ap[:, 129:193], rd.ap, imp_b[:, qt, :], ALU.mult, ALU.add),
                 reads=[pacc, rd, imp], writes=[imp])
        if r in r_own:
            lo = r_own.index(r)
            k.op("dve", lambda h: h.tensor_tensor(rg.ap, rd.ap, gts_b[:, qt, r * 3 + gate_j:r * 3 + gate_j + 1], ALU.mult),
                 reads=[rd, gts], writes=[rg])
            k.op("dve", lambda h: h.scalar_tensor_tensor(oacc_b[:, qt, lo * 128:(lo + 1) * 128], pacc.ap[:, 0:128], rg.ap,
                                                         oacc_b[:, qt, lo * 128:(lo + 1) * 128], ALU.mult, ALU.add),
                 reads=[pacc, rg, oacc], writes=[oacc])

    for r in range(4):
        for qg in range(8):
            qs = slice(qg * 512, (qg + 1) * 512)
            pts = []
            for nt_ in range(2):
                ps = pp.get()
                k.op("pe", lambda h: h.matmul(ps.ap, kcmp_b[:, nt_ * 128:(nt_ + 1) * 128], qT_b[:, r, qs], start=True, stop=True),
                     reads=[kcmp, qT], writes=[ps])
                e = e_t[nt_]; mkk = mk_t[nt_]; pt = p_t[nt_]
                k.op("act", lambda h: h.activation(e.ap, ps.ap, AF.Exp, scale=SC), reads=[ps], writes=[e])
                k.dma("pool", mkk.ap, cmask_d[nt_ * 128:(nt_ + 1) * 128, qs], writes=[mkk])
                k.op("dve", lambda h: h.tensor_tensor(pt.ap, e.ap, mkk.ap, ALU.mult), reads=[e, mkk], writes=[pt])
                pts.append(pt)
            for sub in range(4):
                pa = po[sub]
                for nt_ in range(2):
                    k.op("pe", lambda h: h.matmul(pa.ap[:, 0:193], pts[nt_].ap[:, sub * 128:(sub + 1) * 128], vca_b[:, nt_, :],
                                                  start=(nt_ == 0), stop=(nt_ == 1)),
                         reads=[pts[nt_], vca], writes=[pa], pe_accum=True)
                finish_sub(pa, qg * 4 + sub, r, 0, True)
    sc_t = [T(k.sb([128, 64], F32)[:]) for _ in range(2)]
    wk_t = [T(k.sb([128, 64], F32)[:]) for _ in range(2)]
    bm_t = [T(k.sb([128, 64], F32)[:]) for _ in range(2)]
    m8_t = [T(k.sb([128, 8], F32)[:]) for _ in range(2)]
    thr_t = [T(k.sb([128, 1], F32)[:]) for _ in range(2)]
    selb_t = [T(k.sb([128, 64], F32)[:]) for _ in range(2)]
    for qt in range(32):
        i = qt % 2
        sc = sc_t[i]; wk = wk_t[i]; bm = bm_t[i]; m8 = m8_t[i]; thr = thr_t[i]; selb = selb_t[i]
        k.dma("sp", bm.ap, bmask_d[qt * 128:(qt + 1) * 128, :], writes=[bm])
        k.op("dve", lambda h: h.tensor_tensor(sc.ap, imp_b[:, qt, :], bm.ap, ALU.add), reads=[imp, bm], writes=[sc])
        k.op("dve", lambda h: h.max(out=m8.ap, in_=sc.ap), reads=[sc], writes=[m8])
        k.op("dve", lambda h: h.match_replace(out=wk.ap, in_to_replace=m8.ap, in_values=sc.ap, imm_value=-3e38), reads=[sc, m8], writes=[wk])
        k.op("dve", lambda h: h.max(out=m8.ap, in_=wk.ap), reads=[wk], writes=[m8])
        k.op("dve", lambda h: h.tensor_reduce(out=thr.ap, in_=m8.ap, axis=AX.X, op=ALU.min), reads=[m8], writes=[thr])
        k.op("dve", lambda h: h.tensor_scalar(wk.ap, sc.ap, thr.ap, None, ALU.is_ge), reads=[sc, thr], writes=[wk])
        k.op("dve", lambda h: h.tensor_scalar(sc.ap, bm.ap, -1.0, None, ALU.is_ge), reads=[bm], writes=[sc])
        k.op("dve", lambda h: h.tensor_tensor(selb.ap, wk.ap, sc.ap, ALU.mult), reads=[wk, sc], writes=[selb])
        pt = pp.get()
        k.op("pe", lambda h: h.transpose(pt.ap[0:64, 0:128], selb.ap, ident.ap), reads=[selb, ident], writes=[pt])
        k.op("dve", lambda h: h.tensor_copy(selT_b[:, qt * 128:(qt + 1) * 128], pt.ap[0:64, 0:128]), reads=[pt], writes=[selT])
    ex_b = k.sb([64, S], BF16); ex = T(ex_b[:])
    k.dma("pool", ex.ap, ex_d, writes=[ex])
    caus_b = k.sb([128, 4, 512], BF16); caus = T(caus_b[:])
    k.dma("pool", caus.ap, caus_d, writes=[caus])
    band_b = k.sb([128, 8, 512], BF16); band = T(band_b[:])
    k.dma("pool", band.ap, band_d, writes=[band])
    for (br, kT_b, kT_t, v_b, v_t, gate_j) in (("slc", ks_b, ksT, vs_b, vs_tm, 1), ("win", kw_b, kwT, vw_b, vw_tm, 2)):
        for lo, r in enumerate(r_own):
            for qg in range(8):
                qs = slice(qg * 512, (qg + 1) * 512)
                kt0 = 0 if br == "slc" else max(0, 4 * qg - 4)
                kts = list(range(kt0, 4 * qg + 4))
                for kt in kts:
                    dd = kt - 4 * qg
                    ps = pp.get()
                    k.op("pe", lambda h: h.matmul(ps.ap, kT_b[:, kt * 128:(kt + 1) * 128], qr_b[:, lo, qs], start=True, stop=True),
                         reads=[kT_t, qr], writes=[ps])
                    i = cnt[0] % 3; cnt[0] += 1
                    e = e_t[i]; pt = p_t[i]
                    k.op("act", lambda h: h.activation(e.ap, ps.ap, AF.Exp, scale=SC), reads=[ps], writes=[e])
                    if br == "slc":
                        pm = pp.get()
                        k.op("pe", lambda h: h.matmul(pm.ap, ex_b[:, kt * 128:(kt + 1) * 128], selT_b[:, qs], start=True, stop=True),
                             reads=[ex, selT], writes=[pm])
                        k.op("dve", lambda h: h.tensor_tensor(pt.ap, e.ap, pm.ap, ALU.mult), reads=[e, pm], writes=[pt])
                        if dd >= 0:
                            k.op("dve", lambda h: h.tensor_tensor(pt.ap, pt.ap, caus_b[:, dd, :], ALU.mult), reads=[pt, caus], writes=[pt])
                    else:
                        k.op("dve", lambda h: h.tensor_tensor(pt.ap, e.ap, band_b[:, dd + 4, :], ALU.mult), reads=[e, band], writes=[pt])
                    for sub in range(4):
                        k.op("pe", lambda h: h.matmul(po[sub].ap[:, 0:129], pt.ap[:, sub * 128:(sub + 1) * 128], v_b[:, kt, 0:129],
                                                      start=(kt == kts[0]), stop=(kt == kts[-1])),
                             reads=[pt, v_t], writes=[po[sub]], pe_accum=True)
                for sub in range(4):
                    finish_sub(po[sub], qg * 4 + sub, r, gate_j, False)
    for qt in range(32):
        k.dma("sp", ob[qt * 128:(qt + 1) * 128, :], oacc_b[:, qt, :], reads=[oacc], writes=[tob])


def emit_nsa_f(k, g, h2v, w_in_v, ident, obT, tobT, nsa_in, validk_d):
    (cosT, sinT, rT_d, w1k_d, w1v_d, b1k_d, b1v_d, w2k_d, w2v_d, posk_d, posv_d, gsel_d, cmask_d, bmask_d, ex_d, caus_d, band_d) = nsa_in
    SC = 128.0 ** -0.5
    r_own = (0, 1, 2, 3)
    QG0 = 6
    pp = PsumPool(k, 4)
    po = [T(k.ps([128, 512], F32)[:]) for _ in range(4)]
    qT_b = k.sb([128, 4, NT], BF16); qT = T(qT_b[:])
    qr_b = k.sb([128, 4, NT], BF16); qr = T(qr_b[:])
    kc_b = k.sb([128, S], BF16); kcT = T(kc_b[:]); vc_b = k.sb([128, S], BF16); vcT = T(vc_b[:])
    ks_b = k.sb([128, S], BF16); ksT = T(ks_b[:]); kw_b = k.sb([128, S], BF16); kwT = T(kw_b[:])
    vs_b = k.sb([128, 32, 130], BF16); vs_tm = T(vs_b[:]); vw_b = k.sb([128, 32, 130], BF16); vw_tm = T(vw_b[:])
    gts_b = k.sb([128, 8, 12], F32); gts = T(gts_b[:])
    imp_b = k.sb([128, 8, 64], F32); imp = T(imp_b[:])
    oacc_b = k.sb([128, 8, 512], F32); oacc = T(oacc_b[:])
    selT_b = k.sb([64, NT], BF16); selT = T(selT_b[:])
    k.op("dve", lambda h: h.memset(vs_b[:, :, 128:129], 1.0), writes=[vs_tm])
    k.op("dve", lambda h: h.memset(vw_b[:, :, 128:129], 1.0), writes=[vw_tm])
    k.op("dve", lambda h: h.memset(imp.ap, 0.0), writes=[imp])
    k.op("dve", lambda h: h.memset(oacc.ap, 0.0), writes=[oacc])
    with ExitStack() as ph:
        old = k.stack; k.stack = ph
        hb_b = k.sb([128, 16, 512], BF16); hb = T(hb_b[:])
        ws = WStream(k, 4, 16 * 128)
        wng_b = k.sb([128, 16, 12], BF16); wng = T(wng_b[:])
        k.dma("pool", wng_b[:], w_in_v[:, :, 6672 + 12 * g:6672 + 12 * g + 12], writes=[wng])
        vk_t = None
        rT_b = k.sb([128, 128], BF16); rT = T(rT_b[:])
        k.dma("pool", rT.ap, rT_d, writes=[rT])
        cs_t = [T(k.sb([128, 512], F32)[:]) for _ in range(2)]
        sn_t = [T(k.sb([128, 512], F32)[:]) for _ in range(2)]
        xb_t = [T(k.sb([128, 512], BF16)[:]) for _ in range(2)]
        t1_t = [T(k.sb([128, 512], F32)[:]) for _ in range(2)]
        t2_t = [T(k.sb([128, 512], F32)[:]) for _ in range(2)]
        jobs = {}
        def wcol(ch):
            if ch < 4:
                c0 = 4112 + (4 * g + ch) * 128
            else:
                c0 = 5136 + (ch - 4) * 256 + g * 128
            return w_in_v[:, :, c0:c0 + 128]
        for tb in range(8):
            for ch in range(10):
                if ch < 4 and tb < QG0:
                    continue
                jobs[(tb, ch)] = ws.add(wcol(ch), 128)
        nr = 0
        for tb in range(8):
            bs = slice(tb * 512, (tb + 1) * 512)
            k.dma("pool", hb.ap, h2v[:, :, bs], writes=[hb])
            k.dma("sp", cs_t[tb % 2].ap, cosT[:, bs], writes=[cs_t[tb % 2]])
            k.dma("sp", sn_t[tb % 2].ap, sinT[:, bs], writes=[sn_t[tb % 2]])
            for ch in range(10):
                if ch < 4 and tb < QG0:
                    continue
                lbs = slice((tb - QG0) * 512, (tb - QG0 + 1) * 512)
                tw, vw_ = ws.get(jobs[(tb, ch)])
                ps = pp.get()
                for kc in range(16):
                    k.op("pe", lambda h: h.matmul(ps.ap, vw_[:, kc, :], hb_b[:, kc, :], start=(kc == 0), stop=(kc == 15)),
                         reads=[tw, hb], writes=[ps], pe_accum=True)
                rope_dst = None
                if ch < 4:
                    k.op("act", lambda h: h.activation(qT_b[:, ch, lbs], ps.ap, AF.Copy), reads=[ps], writes=[qT])
                    rope_dst = (qr, qr_b[:, ch, lbs])
                elif ch == 4:
                    k.op("act", lambda h: h.activation(kc_b[:, bs], ps.ap, AF.Copy), reads=[ps], writes=[kcT])
                elif ch == 5:
                    k.op("act", lambda h: h.activation(vc_b[:, bs], ps.ap, AF.Copy), reads=[ps], writes=[vcT])
                elif ch == 6:
                    rope_dst = (ksT, ks_b[:, bs])
                elif ch == 8:
                    rope_dst = (kwT, kw_b[:, bs])
                else:
                    dst_t, dst_b = (vs_tm, vs_b) if ch == 7 else (vw_tm, vw_b)
                    xf = t1_t[nr % 2]; nr += 1
                    k.op("act", lambda h: h.activation(xf.ap, ps.ap, AF.Copy), reads=[ps], writes=[xf])
                    for tt in range(4):
                        pt = pp.get()
                        k.op("pe", lambda h: h.transpose(pt.ap[:, 0:128], xf.ap[:, tt * 128:(tt + 1) * 128], ident.ap),
                             reads=[xf, ident], writes=[pt])
                        k.op("dve", lambda h: h.tensor_copy(dst_b[:, tb * 4 + tt, 0:128], pt.ap[:, 0:128]), reads=[pt], writes=[dst_t])
                if rope_dst is not None:
                    dt_, dap = rope_dst
                    xb = xb_t[nr % 2]; nr += 1
                    k.op("act", lambda h: h.activation(xb.ap, ps.ap, AF.Copy), reads=[ps], writes=[xb])
                    pr = pp.get()
                    k.op("pe", lambda h: h.matmul(pr.ap, rT.ap, xb.ap, start=True, stop=True), reads=[rT, xb], writes=[pr])
                    t1 = t1_t[nr % 2]; t2 = t2_t[nr % 2]
                    k.op("dve", lambda h: h.tensor_tensor(t1.ap, ps.ap, cs_t[tb % 2].ap, ALU.mult), reads=[ps, cs_t[tb % 2], xb], writes=[t1])
                    k.op("dve", lambda h: h.tensor_tensor(t2.ap, pr.ap, sn_t[tb % 2].ap, ALU.mult), reads=[pr, sn_t[tb % 2]], writes=[t2])
                    k.op("dve", lambda h: h.tensor_tensor(dap, t1.ap, t2.ap, ALU.add), reads=[t1, t2], writes=[dt_])
            for tt in range(4 if tb >= QG0 else 0):
                pg = pp.get()
                for kc in range(16):
                    k.op("pe", lambda h: h.matmul(pg.ap[:, 0:12], hb_b[:, kc, tt * 128:(tt + 1) * 128], wng_b[:, kc, :],
                                                  start=(kc == 0), stop=(kc == 15)),
                         reads=[hb, wng], writes=[pg], pe_accum=True)
                k.op("act", lambda h: h.activation(gts_b[:, (tb - QG0) * 4 + tt, :], pg.ap[:, 0:12], AF.Sigmoid), reads=[pg], writes=[gts])
        k.barrier()
        k.stack = old
    kcmp_b = k.sb([128, 256], BF16); kcmp = T(kcmp_b[:])
    vca_b = k.sb([128, 2, 193], BF16); vca = T(vca_b[:])
    k.op("dve", lambda h: h.memset(kcmp.ap, 0.0), writes=[kcmp])
    k.op("dve", lambda h: h.memset(vca.ap, 0.0), writes=[vca])
    k.op("dve", lambda h: h.memset(vca_b[:, :, 128:129], 1.0), writes=[vca])
    k.dma("pool", vca_b[:, :, 129:193], gsel_d.rearrange("(a p) j -> p a j", p=128), writes=[vca])
    with ExitStack() as ph:
        old = k.stack; k.stack = ph
        w1_b = k.sb([128, 32, 128], BF16); w1 = T(w1_b[:])
        w2_b = k.sb([128, 128], BF16); w2 = T(w2_b[:])
        pos_b = k.sb([128, 32], BF16); pos = T(pos_b[:])
        b1_b = k.sb([128, 1], F32); b1 = T(b1_b[:])
        bias_b = k.sb([128, 1], F32); bias = T(bias_b[:])
        hid_b = k.sb([128, 256], BF16); hid = T(hid_b[:])
        for which in range(2):
            w1d, b1d, w2d, posd, src_b, src_t = ((w1k_d, b1k_d, w2k_d, posk_d, kc_b, kcT), (w1v_d, b1v_d, w2v_d, posv_d, vc_b, vcT))[which]
            k.dma("pool", w1.ap, w1d.rearrange("(j d) m -> d j m", d=128), writes=[w1])
            k.dma("pool", w2.ap, w2d, writes=[w2])
            k.dma("pool", pos.ap, posd, writes=[pos])
            k.dma("sp", b1.ap, b1d, writes=[b1])
            pb = pp.get()
            for j in range(32):
                k.op("pe", lambda h: h.matmul(pb.ap[:, 0:1], w1_b[:, j, :], pos_b[:, j:j + 1], start=(j == 0), stop=(j == 31)),
                     reads=[w1, pos], writes=[pb], pe_accum=True)
            k.op("dve", lambda h: h.tensor_tensor(bias.ap, pb.ap[:, 0:1], b1.ap, ALU.add), reads=[pb, b1], writes=[bias])
            ph_ = pp.get()
            for j in range(32):
                k.op("pe", lambda h: h.matmul(ph_.ap[:, 0:255], w1_b[:, j, :], src_b[:, j:j + 16 * 254 + 1:16], start=(j == 0), stop=(j == 31)),
                     reads=[w1, src_t], writes=[ph_], pe_accum=True)
            k.op("dve", lambda h: h.memset(hid.ap, 0.0), writes=[hid])
            k.op("act", lambda h: h.activation(hid_b[:, 0:255], ph_.ap[:, 0:255], AF.Silu, bias=bias.ap), reads=[ph_, bias], writes=[hid])
            if which == 0:
                pk = pp.get()
                k.op("pe", lambda h: h.matmul(pk.ap[:, 0:255], w2.ap, hid_b[:, 0:255], start=True, stop=True), reads=[w2, hid], writes=[pk])
                k.op("act", lambda h: h.activation(kcmp_b[:, 0:255], pk.ap[:, 0:255], AF.Copy), reads=[pk], writes=[kcmp])
            else:
                for nt_ in range(2):
                    pv = pp.get()
                    k.op("pe", lambda h: h.matmul(pv.ap[:, 0:128], hid_b[:, nt_ * 128:(nt_ + 1) * 128], w2.ap, start=True, stop=True),
                         reads=[w2, hid], writes=[pv])
                    k.op("act", lambda h: h.activation(vca_b[:, nt_, 0:128], pv.ap[:, 0:128], AF.Copy), reads=[pv], writes=[vca])
        k.barrier()
        k.stack = old
    e_t = [T(k.sb([128, 512], BF16)[:]) for _ in range(3)]
    p_t = [T(k.sb([128, 512], BF16)[:]) for _ in range(3)]
    mk_t = [T(k.sb([128, 512], BF16)[:]) for _ in range(2)]
    rd_t = [T(k.sb([128, 1], F32)[:]) for _ in range(2)]
    rg_t = [T(k.sb([128, 1], F32)[:]) for _ in range(2)]
    tmpo = [T(k.sb([128, 128], F32)[:]) for _ in range(2)]
    cnt = [0]

    def finish_sub(pacc, qt, r, gate_j, with_imp):
        i = cnt[0] % 2; cnt[0] += 1
        rd = rd_t[i]; rg = rg_t[i]
        k.op("dve", lambda h: h.tensor_scalar(rd.ap, pacc.ap[:, 128:129], 1e-30, None, ALU.max), reads=[pacc], writes=[rd])
        k.op("dve", lambda h: h.reciprocal(rd.ap, rd.ap), reads=[rd], writes=[rd])
        if with_imp:
            k.op("dve", lambda h: h.scalar_tensor_tensor(imp_b[:, qt, :], pacc.ap[:, 129:193], rd.ap, imp_b[:, qt, :], ALU.mult, ALU.add),
                 reads=[pacc, rd, imp], writes=[imp])
        if r in r_own:
            lo = r
            k.op("dve", lambda h: h.tensor_tensor(rg.ap, rd.ap, gts_b[:, qt, r * 3 + gate_j:r * 3 + gate_j + 1], ALU.mult),
                 reads=[rd, gts], writes=[rg])
            k.op("dve", lambda h: h.scalar_tensor_tensor(oacc_b[:, qt, lo * 128:(lo + 1) * 128], pacc.ap[:, 0:128], rg.ap,
                                                         oacc_b[:, qt, lo * 128:(lo + 1) * 128], ALU.mult, ALU.add),
                 reads=[pacc, rg, oacc], writes=[oacc])

    for r in range(4):
        for qg in range(2):
            qs = slice(qg * 512, (qg + 1) * 512)
            pts = []
            for nt_ in range(2):
                ps = pp.get()
                k.op("pe", lambda h: h.matmul(ps.ap, kcmp_b[:, nt_ * 128:(nt_ + 1) * 128], qT_b[:, r, qs], start=True, stop=True),
                     reads=[kcmp, qT], writes=[ps])
                e = e_t[nt_]; mkk = mk_t[nt_]; pt = p_t[nt_]
                k.op("act", lambda h: h.activation(e.ap, ps.ap, AF.Exp, scale=SC), reads=[ps], writes=[e])
                k.dma("pool", mkk.ap, cmask_d[nt_ * 128:(nt_ + 1) * 128, qs], writes=[mkk])
                k.op("dve", lambda h: h.tensor_tensor(pt.ap, e.ap, mkk.ap, ALU.mult), reads=[e, mkk], writes=[pt])
                pts.append(pt)
            for sub in range(4):
                pa = po[sub]
                for nt_ in range(2):
                    k.op("pe", lambda h: h.matmul(pa.ap[:, 0:193], pts[nt_].ap[:, sub * 128:(sub + 1) * 128], vca_b[:, nt_, :],
                                                  start=(nt_ == 0), stop=(nt_ == 1)),
                         reads=[pts[nt_], vca], writes=[pa], pe_accum=True)
                finish_sub(pa, qg * 4 + sub, r, 0, True)
    sc_t = [T(k.sb([128, 64], F32)[:]) for _ in range(2)]
    wk_t = [T(k.sb([128, 64], F32)[:]) for _ in range(2)]
    bm_t = [T(k.sb([128, 64], F32)[:]) for _ in range(2)]
    m8_t = [T(k.sb([128, 8], F32)[:]) for _ in range(2)]
    thr_t = [T(k.sb([128, 1], F32)[:]) for _ in range(2)]
    selb_t = [T(k.sb([128, 64], F32)[:]) for _ in range(2)]
    for qt in range(8):
        i = qt % 2
        sc = sc_t[i]; wk = wk_t[i]; bm = bm_t[i]; m8 = m8_t[i]; thr = thr_t[i]; selb = selb_t[i]
        k.dma("sp", bm.ap, bmask_d[qt * 128:(qt + 1) * 128, :], writes=[bm])
        k.op("dve", lambda h: h.tensor_tensor(sc.ap, imp_b[:, qt, :], bm.ap, ALU.add), reads=[imp, bm], writes=[sc])
        k.op("dve", lambda h: h.max(out=m8.ap, in_=sc.ap), reads=[sc], writes=[m8])
        k.op("dve", lambda h: h.match_replace(out=wk.ap, in_to_replace=m8.ap, in_values=sc.ap, imm_value=-3e38), reads=[sc, m8], writes=[wk])
        k.op("dve", lambda h: h.max(out=m8.ap, in_=wk.ap), reads=[wk], writes=[m8])
        k.op("dve", lambda h: h.tensor_reduce(out=thr.ap, in_=m8.ap, axis=AX.X, op=ALU.min), reads=[m8], writes=[thr])
        k.op("dve", lambda h: h.tensor_scalar(wk.ap, sc.ap, thr.ap, None, ALU.is_ge), reads=[sc, thr], writes=[wk])
        k.op("dve", lambda h: h.tensor_scalar(sc.ap, bm.ap, -1.0, None, ALU.is_ge), reads=[bm], writes=[sc])
        k.op("dve", lambda h: h.tensor_tensor(selb.ap, wk.ap, sc.ap, ALU.mult), reads=[wk, sc], writes=[selb])
        pt = pp.get()
        k.op("pe", lambda h: h.transpose(pt.ap[0:64, 0:128], selb.ap, ident.ap), reads=[selb, ident], writes=[pt])
        k.op("dve", lambda h: h.tensor_copy(selT_b[:, qt * 128:(qt + 1) * 128], pt.ap[0:64, 0:128]), reads=[pt], writes=[selT])
    ex_b = k.sb([64, S], BF16); ex = T(ex_b[:])
    k.dma("pool", ex.ap, ex_d, writes=[ex])
    caus_b = k.sb([128, 4, 512], BF16); caus = T(caus_b[:])
    k.dma("pool", caus.ap, caus_d, writes=[caus])
    band_b = k.sb([128, 8, 512], BF16); band = T(band_b[:])
    k.dma("pool", band.ap, band_d, writes=[band])
    vk = T(k.sb([128, 32], F32)[:])
    k.dma("sp", vk.ap, validk_d, writes=[vk])
    for (br, kT_b, kT_t, v_b, v_t, gate_j) in (("slc", ks_b, ksT, vs_b, vs_tm, 1), ("win", kw_b, kwT, vw_b, vw_tm, 2)):
        for lo, r in enumerate(r_own):
            for lqg in range(2):
                qg = QG0 + lqg
                qs = slice(lqg * 512, (lqg + 1) * 512)
                kt0 = 0 if br == "slc" else max(0, 4 * qg - 4)
                kts = list(range(kt0, 4 * qg + 4))
                for kt in kts:
                    dd = kt - 4 * qg
                    ps = pp.get()
                    k.op("pe", lambda h: h.matmul(ps.ap, kT_b[:, kt * 128:(kt + 1) * 128], qr_b[:, lo, qs], start=True, stop=True),
                         reads=[kT_t, qr], writes=[ps])
                    i = cnt[0] % 3; cnt[0] += 1
                    e = e_t[i]; pt = p_t[i]
                    k.op("act", lambda h: h.activation(e.ap, ps.ap, AF.Exp, scale=SC), reads=[ps], writes=[e])
                    if br == "slc":
                        pm = pp.get()
                        k.op("pe", lambda h: h.matmul(pm.ap, ex_b[:, kt * 128:(kt + 1) * 128], selT_b[:, qs], start=True, stop=True),
                             reads=[ex, selT], writes=[pm])
                        k.op("dve", lambda h: h.tensor_tensor(pt.ap, e.ap, pm.ap, ALU.mult), reads=[e, pm], writes=[pt])
                        if dd >= 0:
                            k.op("dve", lambda h: h.tensor_tensor(pt.ap, pt.ap, caus_b[:, dd, :], ALU.mult), reads=[pt, caus], writes=[pt])
                    else:
                        k.op("dve", lambda h: h.scalar_tensor_tensor(pt.ap, e.ap, vk.ap[:, kt:kt + 1], band_b[:, dd + 4, :], ALU.mult, ALU.mult),
                             reads=[e, band, vk], writes=[pt])
                    for sub in range(4):
                        k.op("pe", lambda h: h.matmul(po[sub].ap[:, 0:129], pt.ap[:, sub * 128:(sub + 1) * 128], v_b[:, kt, 0:129],
                                                      start=(kt == kts[0]), stop=(kt == kts[-1])),
                             reads=[pt, v_t], writes=[po[sub]], pe_accum=True)
                for sub in range(4):
                    finish_sub(po[sub], lqg * 4 + sub, r, gate_j, False)
    oT_t = [T(k.sb([128, 128], F32)[:]) for _ in range(2)]
    for qt in range(8):
        for r in range(4):
            pt = pp.get()
            k.op("pe", lambda h: h.transpose(pt.ap[:, 0:128], oacc_b[:, qt, r * 128:(r + 1) * 128], ident.ap), reads=[oacc, ident], writes=[pt])
            oT_ = oT_t[(qt * 4 + r) % 2]
            k.op("dve", lambda h: h.tensor_copy(oT_.ap, pt.ap[:, 0:128]), reads=[pt], writes=[oT_])
            k.dma("sp", obT[(4 * g + r) * 128:(4 * g + r + 1) * 128, qt * 128:(qt + 1) * 128], oT_.ap, reads=[oT_], writes=[tobT])


def nsa_consts():
    pos = np.arange(S, dtype=np.float32)
    inv_freq = (np.float32(500000.0) ** (-np.arange(0, 32, 2, dtype=np.float32) / np.float32(32))).astype(np.float32)
    ang = (pos[:, None] * inv_freq[None, :]).astype(np.float32)
    cos, sin = np.cos(ang).astype(np.float32), np.sin(ang).astype(np.float32)
    cosT = np.ones((128, S), np.float32); sinT = np.zeros((128, S), np.float32)
    cosT[0:16] = cos.T; cosT[16:32] = cos.T; sinT[0:16] = sin.T; sinT[16:32] = sin.T
    R = np.zeros((128, 128), np.float32)
    for d_ in range(16):
        R[d_, d_ + 16] = -1.0
        R[d_ + 16, d_] = 1.0
    n = np.arange(256); jb = np.arange(64); t = np.arange(S); kk = np.arange(128); q = np.arange(512)
    gsel = ((n[:, None] // 4 == jb[None, :]) & (n[:, None] < 255)).astype(np.float32)
    cmask = ((16 * n[:, None] + 31 <= t[None, :]) & (n[:, None] < 255)).astype(np.float32)
    vis = 64 * jb[None, :] <= t[:, None]
    tb_ = t[:, None] // 64
    forced = (jb[None, :] == 0) | (jb[None, :] == tb_) | (jb[None, :] == tb_ - 1)
    bmask = np.where(vis, np.where(forced, 1e3, 0.0), -1e30).astype(np.float32)
    ex = (t[None, :] // 64 == jb[:, None]).astype(np.float32)
    caus = np.stack([((128 * dd + kk[:, None]) <= q[None, :]) for dd in range(4)], axis=1).astype(np.float32)
    band = np.stack([((q[None, :] - (128 * dd + kk[:, None]) >= 0) & (q[None, :] - (128 * dd + kk[:, None]) < 512))
                     for dd in range(-4, 4)], axis=1).astype(np.float32)
    return dict(cosT=cosT, sinT=sinT, rT=np.ascontiguousarray(R.T), gsel=gsel, cmask=cmask, bmask=bmask, ex=ex,
                caus=np.ascontiguousarray(caus), band=np.ascontiguousarray(band))


def l2_inputs(inp, h2T_full, c):
    b, hp = c // 4, c % 4
    g = hp // 2
    w = inp["w_in"][0]
    heads = (2 * hp, 2 * hp + 1)
    cols = []
    for sec in range(3):
        for hd in heads:
            cols.append(np.arange(sec * 1024 + hd * 128, sec * 1024 + (hd + 1) * 128))
    hq_order = [2 * hp, 2 * hp + 1] + [hq for hq in range(g * 4, g * 4 + 4) if hq not in (2 * hp, 2 * hp + 1)]
    for hq in hq_order:
        cols.append(np.arange(4112 + hq * 128, 4112 + (hq + 1) * 128))
    for j in range(6):
        cols.append(np.arange(5136 + j * 256 + g * 128, 5136 + j * 256 + (g + 1) * 128))
    wfm = np.ascontiguousarray(w[:, np.concatenate(cols)])
    tcols = []
    for hd in heads:
        tcols += [np.arange(3088 + hd * 128, 3088 + (hd + 1) * 128), np.array([3072 + hd, 3080 + hd])]
    tcols += [np.arange(6672 + hq * 3, 6672 + hq * 3 + 3) for hq in hq_order]
    wtm = np.ascontiguousarray(w[:, np.concatenate(tcols)])
    cwf = inp["gdn_conv_w"][0]
    convw = np.stack([cwf[sec * 1024 + hd * 128: sec * 1024 + (hd + 1) * 128] for sec in range(3) for hd in heads], axis=1)
    bc = lambda v: np.ascontiguousarray(np.broadcast_to(np.asarray(v, np.float32)[None, :], (128, len(v))))
    ii = np.arange(64)
    return {
        "h2T": h2T_full[b], "wfm": wfm, "wtm": wtm, "convw": np.ascontiguousarray(convw.astype(np.float32)),
        "alog": bc(inp["gdn_a_log"][0][list(heads)]), "dtb": bc(inp["gdn_dt_bias"][0][list(heads)]),
        "gnw": bc(inp["gdn_norm_w"][0]),
        "ident": np.eye(128, dtype=np.float32),
        "tri": (ii[:, None] <= ii[None, :]).astype(np.float32),
        "ms": np.where(ii[:, None] > ii[None, :], 0.0, 1e4).astype(np.float32),
        "mc": np.where(ii[None, :] >= ii[:, None], 0.0, -1e4).astype(np.float32),
        "w1k": inp["cmp_k_w1"][0], "w1v": inp["cmp_v_w1"][0],
        "b1k": np.ascontiguousarray(inp["cmp_k_b1"][0][:, None]), "b1v": np.ascontiguousarray(inp["cmp_v_b1"][0][:, None]),
        "w2k": inp["cmp_k_w2"][0], "w2v": inp["cmp_v_w2"][0],
        "posk": np.ascontiguousarray(inp["cmp_pos_k"][0].T), "posv": np.ascontiguousarray(inp["cmp_pos_v"][0].T),
    }


def run_l2(inp, h2T_full, do_nsa=True, do_gdn=True, stage=99, heads=(0, 1)):
    nc = build_l2(do_nsa, do_gdn, stage=stage, heads=heads)
    consts = nsa_consts()
    maps = []
    for c in range(NCORE):
        m_ = l2_inputs(inp, h2T_full, c)
        m_.update(consts)
        maps.append(m_)
    return run_bass_kernel_spmd(nc, maps, core_ids=list(range(NCORE)))


def build_fused(phases="ABCD", tlog=None):
    nc = bass.Bass("TRN2", target_bir_lowering=False)
    dt = lambda n, s_, kind="ExternalInput": nc.dram_tensor(n, s_, F32, kind=kind).ap()
    xTp = dt("xTp", [D, S]); cb = dt("cb", [128, 16]); ada_w = dt("ada_w", [D, 9 * D]); ada_b = dt("ada_b", [128, 144])
    n1 = dt("n1", [128, 16]); n2 = dt("n2", [128, 16]); n3 = dt("n3", [128, 16]); nf = dt("nf", [128, 16])
    wg1 = dt("wg1", [D, DFF]); wu1 = dt("wu1", [D, DFF]); wd1 = dt("wd1", [DFF, D])
    wg2 = dt("wg2", [D, DFF]); wu2 = dt("wu2", [D, DFF]); wd2 = dt("wd2", [DFF, D])
    w_in = dt("w_in", [D, 10792]); wba = dt("wba", [D, 16])
    convw = dt("convw", [128, 24, 4]); alog = dt("alog", [128, 8]); dtb = dt("dtb", [128, 8]); gnw = dt("gnw", [128, 128])
    ident_d = dt("ident", [128, 128]); tri_d = dt("tri", [64, 64]); ms_d = dt("ms", [64, 64]); mc_d = dt("mc", [64, 64])
    validT = dt("validT", [128, S]); valid_tm = dt("valid_tm", [64, NCH]); validk = dt("validk", [128, 32])
    nsa_in = (dt("cosT", [128, S]), dt("sinT", [128, S]), dt("rT", [128, 128]),
              dt("w1k", [4096, 128]), dt("w1v", [4096, 128]), dt("b1k", [128, 1]), dt("b1v", [128, 1]),
              dt("w2k", [128, 128]), dt("w2v", [128, 128]), dt("posk", [128, 32]), dt("posv", [128, 32]),
              dt("gsel", [256, 64]), dt("cmask", [256, NT]), dt("bmask", [NT, 64]), dt("ex", [64, S]),
              dt("caus", [128, 4, 512]), dt("band", [128, 8, 512]))
    gup = dt("gup", [1024, D]); nup = dt("nup", [1024, D]); wo = dt("wo", [D, D])
    outT = dt("outT", [D, NT], "ExternalOutput")
    h2s = nc.dram_tensor("h2s", [D, S], F32).ap(); x1s = nc.dram_tensor("x1s", [D, NT], F32).ap()
    oaT = nc.dram_tensor("oaT", [1024, NT], F32).ap(); obT = nc.dram_tensor("obT", [1024, NT], F32).ap()
    th2s = [T(h2s[:, i * NT:(i + 1) * NT]) for i in range(4)]; tx1s = T(x1s); toaT = T(oaT); tobT = T(obT)
    with ExitStack() as st:
        k = K(nc, st)
        small = k.sb([128, 512], F32)
        cact = T(small[:, 0:16]); modS = T(small[:, 16:160]); adab = T(small[:, 160:304])
        n1t = T(small[:, 304:320]); n2t = T(small[:, 320:336])
        A1 = T(small[:, 336:352]); hg1 = T(small[:, 352:368]); A2 = T(small[:, 368:384])
        n3t = T(small[:, 384:400]); nft = T(small[:, 400:416]); A3 = T(small[:, 416:432]); hg3 = T(small[:, 432:448]); zer = T(small[:, 448:464])
        ones_b = k.sb([128, 128], BF16); ones_bf = T(ones_b[:])
        m = lambda j: modS.ap[:, j * 16:(j + 1) * 16]
        k.op("dve", lambda h: h.memset(ones_bf.ap, 1.0), writes=[ones_bf])
        k.op("dve", lambda h: h.memset(zer.ap, 0.0), writes=[zer])
        for t_, d_ in ((cact, cb), (adab, ada_b), (n1t, n1), (n2t, n2), (n3t, n3), (nft, nf)):
            k.dma("sp", t_.ap, d_, writes=[t_])
        with ExitStack() as ph:
            if tlog is not None:
                tlog.append(("A0", k.n_inst))
            k.stack = ph
            pp = PsumPool(k, 7)
            mps = T(k.ps([128, 144], F32)[:])
            xres_b = k.sb([128, 16, NT], F32); xres = [T(xres_b[:, i, :]) for i in range(16)]
            hT_b = k.sb([128, 16, NT], BF16); hT = [T(hT_b[:, i, :]) for i in range(16)]
            aT_b = k.sb([128, 22, NT], BF16); aT = [T(aT_b[:, i, :]) for i in range(22)]
            ws = WStream(k, 5, 22 * 128)
            rstd = T(k.sb([128, NT], F32)[:])
            sq_ts = [T(k.sb([128, NT], BF16)[:]) for _ in range(2)]
            tmp_ts = [T(k.sb([128, NT], F32)[:]) for _ in range(2)]
            sil_ts = [T(k.sb([128, 512], BF16)[:]) for _ in range(2)]
            adw = [k.sb([128, 16, 128], F32) for _ in range(2)]
            adw_t = [T(a_[:]) for a_ in adw]
            k.op("act", lambda h: h.activation(cact.ap, cact.ap, AF.Silu), reads=[cact], writes=[cact])
            adv = ada_w.rearrange("(kc p) n -> p kc n", p=128)
            for g_ in range(144):
                wt_ = adw_t[g_ % 2]
                k.dma("sp", wt_.ap, adv[:, :, g_ * 128:(g_ + 1) * 128], writes=[wt_])
                for kc in range(16):
                    k.op("pe", lambda h: h.matmul(mps.ap[:, g_:g_ + 1], adw[g_ % 2][:, kc, :],
                                                  cact.ap[:, kc:kc + 1], start=(kc == 0), stop=(kc == 15)),
                         reads=[wt_, cact], writes=[mps], pe_accum=True)
            k.op("dve", lambda h: h.tensor_tensor(modS.ap, mps.ap, adab.ap, ALU.add), reads=[mps, adab], writes=[modS])
            k.op("dve", lambda h: h.scalar_tensor_tensor(A1.ap, m(1), 1.0, n1t.ap, ALU.add, ALU.mult), reads=[modS, n1t], writes=[A1])
            k.op("dve", lambda h: h.tensor_scalar(hg1.ap, m(2), 0.5, None, ALU.mult), reads=[modS], writes=[hg1])
            k.op("dve", lambda h: h.scalar_tensor_tensor(A2.ap, m(4), 1.0, n2t.ap, ALU.add, ALU.mult), reads=[modS, n2t], writes=[A2])
            k.op("dve", lambda h: h.scalar_tensor_tensor(A3.ap, m(7), 1.0, n3t.ap, ALU.add, ALU.mult), reads=[modS, n3t], writes=[A3])
            k.op("dve", lambda h: h.tensor_scalar(hg3.ap, m(8), 0.5, None, ALU.mult), reads=[modS], writes=[hg3])
            for blk in range(4):
                bsl = slice(blk * NT, (blk + 1) * NT)
                for kc in range(16):
                    k.dma("act", xres[kc].ap, xTp[kc * 128:(kc + 1) * 128, bsl], writes=[xres[kc]])
                emit_norm_mod(k, pp, xres, ones_bf, sq_ts, rstd, tmp_ts, A1.ap, m(0), lambda kc: (hT[kc], hT[kc].ap),
                              extra_reads=[A1, modS])
                emit_ffn(k, pp, ws, xres, hT, aT, sil_ts, wg1, wu1, wd1, hg1.ap, hg1)
                if blk == 3:
                    for kc in range(16):
                        k.dma("sp", x1s[kc * 128:(kc + 1) * 128, :], xres[kc].ap, reads=[xres[kc]], writes=[tx1s])

                def post_h2(kc, t_, blk=blk, bsl=bsl):
                    k.dma("sp", h2s[kc * 128:(kc + 1) * 128, bsl], t_.ap, reads=[t_], writes=[th2s[blk]])
                emit_norm_mod(k, pp, xres, ones_bf, sq_ts, rstd, tmp_ts, A2.ap, m(3), lambda kc: (None, None),
                              post=post_h2, extra_reads=[A2, modS])
            k.barrier()
        k.stack = st
        if tlog is not None:
            tlog.append(("A1", k.n_inst))
        h2v = h2s.rearrange("(kc p) t -> p kc t", p=128)
        w_in_v = w_in.rearrange("(kc p) n -> p kc n", p=128)
        wba_v = wba.rearrange("(kc p) n -> p kc n", p=128)
        with ExitStack() as pst:
            k.stack = pst
            cs = k.sb([128, 1024], F32)
            ident = T(cs[:, 0:128]); tri = T(cs[0:64, 128:192]); ms = T(cs[0:64, 192:256]); mc = T(cs[0:64, 256:320])
            ones = T(cs[:, 320:448]); cw = T(cs[:, 448:544]); alg = T(cs[:, 544:552]); dtbt = T(cs[:, 552:560])
            gnwt = T(cs[:, 640:768])
            k.dma("sp", ident.ap, ident_d, writes=[ident]); k.dma("sp", tri.ap, tri_d, writes=[tri])
            k.dma("sp", ms.ap, ms_d, writes=[ms]); k.dma("sp", mc.ap, mc_d, writes=[mc])
            k.dma("sp", cw.ap, convw.rearrange("p a b -> p (a b)"), writes=[cw])
            k.dma("sp", alg.ap, alog, writes=[alg]); k.dma("sp", dtbt.ap, dtb, writes=[dtbt])
            k.dma("sp", gnwt.ap, gnw, writes=[gnwt])
            k.op("dve", lambda h: h.memset(ones.ap, 1.0), writes=[ones])
            k.op("act", lambda h: h.activation(alg.ap, alg.ap, AF.Exp), reads=[alg], writes=[alg])
            k.op("dve", lambda h: h.tensor_scalar(alg.ap, alg.ap, -1.0, None, ALU.mult), reads=[alg], writes=[alg])
            with ExitStack() as pps:
                k.stack = pps
                pp = PsumPool(k, 8)
                for hd in range(8 if "B" in phases else 0):
                    with ExitStack() as ph:
                        k.stack = ph
                        fz = dict(hd=hd, w_in_v=w_in_v, wba_v=wba_v, validT=validT, valid_tm=valid_tm, oaT=oaT, toaT=toaT, c_first=48)
                        emit_gdn_head(k, pp, hd, h2v, None, None, ident, tri, ms, mc, ones, cw, alg, dtbt, gnwt, None, None, fz=fz)
                        k.barrier()
                    k.stack = pps
                k.barrier()
            k.stack = pst
            for g in range(2 if "C" in phases else 0):
                with ExitStack() as ph:
                    k.stack = ph
                    emit_nsa_f(k, g, h2v, w_in_v, ident, obT, tobT, nsa_in, validk)
                    k.barrier()
                k.stack = pst
            k.barrier()
        k.stack = st
        with ExitStack() as ph:
            if "D" not in phases:
                k.finish()
                return nc
            k.stack = ph
            pp = PsumPool(k, 8)
            xres_b = k.sb([128, 16, NT], F32); xres = [T(xres_b[:, i, :]) for i in range(16)]
            hT_b = k.sb([128, 16, NT], BF16); hT = [T(hT_b[:, i, :]) for i in range(16)]
            aT_b = k.sb([128, 22, NT], BF16); aT = [T(aT_b[:, i, :]) for i in range(22)]
            ws = WStream(k, 5, 22 * 128)
            rstd = T(k.sb([128, NT], F32)[:])
            sq_ts = [T(k.sb([128, NT], BF16)[:]) for _ in range(2)]
            tmp_ts = [T(k.sb([128, NT], F32)[:]) for _ in range(2)]
            sil_ts = [T(k.sb([128, 512], BF16)[:]) for _ in range(2)]
            sg_ts = [T(k.sb([128, 512], F32)[:]) for _ in range(2)]
            h2own = h2s[:, 3 * NT:4 * NT]
            for kc in range(16):
                k.dma("pool", hT[kc].ap, h2own[kc * 128:(kc + 1) * 128, :], reads=[th2s[3]], writes=[hT[kc]])
            for kc in range(16):
                k.dma("act", xres[kc].ap, x1s[kc * 128:(kc + 1) * 128, :], reads=[tx1s], writes=[xres[kc]])
            wmv = w_in_v[:, :, 6696:6696 + 2 * D]
            guv = gup.rearrange("(kc p) n -> p kc n", p=128)
            nuv = nup.rearrange("(kc p) n -> p kc n", p=128)
            wov = wo.rearrange("(kc p) n -> p kc n", p=128)
            oav = oaT.rearrange("(c p) t -> p c t", p=128)
            obv = obT.rearrange("(c p) t -> p c t", p=128)
            mer_t = [T(aT_b[:, i // 2, (i % 2) * 512:(i % 2 + 1) * 512]) for i in range(16)]
            oa_t = [T(aT_b[:, 8 + i // 2, (i % 2) * 512:(i % 2 + 1) * 512]) for i in range(8)]
            ob_t = [T(aT_b[:, 12 + i // 2, (i % 2) * 512:(i % 2 + 1) * 512]) for i in range(8)]
            jobs = {}
            for th in range(2):
                for fc in range(16):
                    cs_ = slice(fc * 128, (fc + 1) * 128)
                    jobs[("ma", th, fc)] = ws.add(wmv[:, :, cs_], 128)
                    jobs[("ga", th, fc)] = ws.add(guv[:, :, cs_], 128)
                    jobs[("mb", th, fc)] = ws.add(wmv[:, :, D + fc * 128:D + (fc + 1) * 128], 128)
                    jobs[("gb", th, fc)] = ws.add(nuv[:, :, cs_], 128)
                for fc in range(16):
                    jobs[("wo", th, fc)] = ws.add(wov[:, :, fc * 128:(fc + 1) * 128], 128)
            for th in range(2):
                sl = slice(th * 512, (th + 1) * 512)
                for c in range(8):
                    k.dma("pool", oa_t[c].ap, oav[:, c, sl], reads=[toaT], writes=[oa_t[c]])
                    k.dma("pool", ob_t[c].ap, obv[:, c, sl], reads=[tobT], writes=[ob_t[c]])
                for fc in range(16):
                    parts = []
                    for nm_m, nm_g, src in (("ma", "ga", oa_t), ("mb", "gb", ob_t)):
                        tm_, vm = ws.get(jobs[(nm_m, th, fc)])
                        tg_, vg = ws.get(jobs[(nm_g, th, fc)])
                        pm = pp.get(); py = pp.get()
                        for kc in range(16):
                            k.op("pe", lambda h: h.matmul(pm.ap, vm[:, kc, :], hT[kc].ap[:, sl], start=(kc == 0), stop=(kc == 15)),
                                 reads=[tm_, hT[kc]], writes=[pm], pe_accum=True)
                        for c in range(8):
                            k.op("pe", lambda h: h.matmul(py.ap, vg[:, c, :], src[c].ap, start=(c == 0), stop=(c == 7)),
                                 reads=[tg_, src[c]], writes=[py], pe_accum=True)
                        sg = sg_ts[len(parts)]
                        k.op("act", lambda h: h.activation(sg.ap, pm.ap, AF.Sigmoid), reads=[pm], writes=[sg])
                        k.op("dve", lambda h: h.tensor_tensor(sg.ap, sg.ap, py.ap, ALU.mult), reads=[sg, py], writes=[sg])
                        parts.append(sg)
                    k.op("dve", lambda h: h.tensor_tensor(mer_t[fc].ap, parts[0].ap, parts[1].ap, ALU.add),
                         reads=parts, writes=[mer_t[fc]])
                for fc2 in range(16):
                    tw, vw = ws.get(jobs[("wo", th, fc2)])
                    pz = pp.get()
                    for fc in range(16):
                        k.op("pe", lambda h: h.matmul(pz.ap, vw[:, fc, :], mer_t[fc].ap, start=(fc == 0), stop=(fc == 15)),
                             reads=[tw, mer_t[fc]], writes=[pz], pe_accum=True)
                    k.op("dve", lambda h: h.scalar_tensor_tensor(xres[fc2].ap[:, sl], pz.ap, m(5)[:, fc2:fc2 + 1],
                                                                 xres[fc2].ap[:, sl], ALU.mult, ALU.add),
                         reads=[pz, modS, xres[fc2]], writes=[xres[fc2]])
            k.barrier()
            emit_norm_mod(k, pp, xres, ones_bf, sq_ts, rstd, tmp_ts, A3.ap, m(6), lambda kc: (hT[kc], hT[kc].ap),
                          extra_reads=[A3, modS])
            emit_ffn(k, pp, ws, xres, hT, aT, sil_ts, wg2, wu2, wd2, hg3.ap, hg3)
            tout = T(outT)

            def post(kc, t_):
                k.dma("sp", outT[kc * 128:(kc + 1) * 128, :], t_.ap, reads=[t_], writes=[tout])
            emit_norm_mod(k, pp, xres, ones_bf, sq_ts, rstd, tmp_ts, nft.ap, zer.ap, lambda kc: (None, None),
                          post=post, extra_reads=[nft, zer])
            k.barrier()
        k.stack = st
        k.finish()
        print("FUSED inst", k.n_inst, "waits", k.n_wait)
    return nc


def fused_inputs(inp, c):
    b, r = c // 4, c % 4
    pad = (3 - r) * NT
    nreal = S - pad
    xTp = np.zeros((D, S), np.float32)
    xTp[:, pad:] = inp["x"][b, :nreal, :].T
    valid = (np.arange(S) >= pad).astype(np.float32)
    w = inp["w_in"][0]
    wba = np.ascontiguousarray(np.stack([w[:, col] for hd in range(8) for col in (3072 + hd, 3080 + hd)], axis=1))
    cwf = inp["gdn_conv_w"][0]
    convw = np.stack([cwf[sec * 1024 + hd * 128: sec * 1024 + (hd + 1) * 128] for sec in range(3) for hd in range(8)], axis=1)
    bc = lambda v: np.ascontiguousarray(np.broadcast_to(np.asarray(v, np.float32)[None, :], (128, len(v))))
    ii = np.arange(64)
    posn = np.maximum(np.arange(S) - pad, 0).astype(np.float32)
    inv_freq = (np.float32(500000.0) ** (-np.arange(0, 32, 2, dtype=np.float32) / np.float32(32))).astype(np.float32)
    ang = (posn[:, None] * inv_freq[None, :]).astype(np.float32)
    cos, sin = np.cos(ang).astype(np.float32), np.sin(ang).astype(np.float32)
    cosT = np.ones((128, S), np.float32); sinT = np.zeros((128, S), np.float32)
    cosT[0:16] = cos.T; cosT[16:32] = cos.T; sinT[0:16] = sin.T; sinT[16:32] = sin.T
    R = np.zeros((128, 128), np.float32)
    for d_ in range(16):
        R[d_, d_ + 16] = -1.0
        R[d_ + 16, d_] = 1.0
    n = np.arange(256); jb = np.arange(64); t = np.arange(S - NT, S); kk = np.arange(128); q = np.arange(512)
    gsel = ((n[:, None] // 4 == jb[None, :]) & (n[:, None] < 255)).astype(np.float32)
    cmask = ((16 * n[:, None] + 31 <= t[None, :]) & (n[:, None] < 255) & (16 * n[:, None] >= pad)).astype(np.float32)
    jb0 = pad // 64
    vis = (64 * jb[None, :] <= t[:, None]) & (jb[None, :] >= jb0)
    tb_ = t[:, None] // 64
    forced = (jb[None, :] == jb0) | (jb[None, :] == tb_) | (jb[None, :] == tb_ - 1)
    bmask = np.where(vis, np.where(forced, 1e3, 0.0), -1e30).astype(np.float32)
    ex = (np.arange(S)[None, :] // 64 == jb[:, None]).astype(np.float32)
    caus = np.stack([((128 * dd + kk[:, None]) <= q[None, :]) for dd in range(4)], axis=1).astype(np.float32)
    band = np.stack([((q[None, :] - (128 * dd + kk[:, None]) >= 0) & (q[None, :] - (128 * dd + kk[:, None]) < 512))
                     for dd in range(-4, 4)], axis=1).astype(np.float32)
    ada_b = np.ascontiguousarray(inp["ada_b"][0].reshape(144, 128).T)
    return {
        "xTp": xTp, "cb": _pc(inp["c"][b]), "ada_w": inp["ada_w"][0], "ada_b": ada_b,
        "n1": _pc(inp["ffn1_norm"][0]), "n2": _pc(inp["mix_norm"][0]), "n3": _pc(inp["ffn2_norm"][0]), "nf": _pc(inp["final_norm"]),
        "wg1": inp["ffn1_w_gate"][0], "wu1": inp["ffn1_w_up"][0], "wd1": inp["ffn1_w_down"][0],
        "wg2": inp["ffn2_w_gate"][0], "wu2": inp["ffn2_w_up"][0], "wd2": inp["ffn2_w_down"][0],
        "w_in": w, "wba": wba, "convw": np.ascontiguousarray(convw.astype(np.float32)),
        "alog": bc(inp["gdn_a_log"][0]), "dtb": bc(inp["gdn_dt_bias"][0]), "gnw": bc(inp["gdn_norm_w"][0]),
        "ident": np.eye(128, dtype=np.float32), "tri": (ii[:, None] <= ii[None, :]).astype(np.float32),
        "ms": np.where(ii[:, None] > ii[None, :], 0.0, 1e4).astype(np.float32),
        "mc": np.where(ii[None, :] >= ii[:, None], 0.0, -1e4).astype(np.float32),
        "validT": np.ascontiguousarray(np.broadcast_to(valid[None, :], (128, S))),
        "valid_tm": np.ascontiguousarray(valid.reshape(NCH, 64).T), "validk": np.ascontiguousarray(valid.reshape(32, 128).T),
        "cosT": cosT, "sinT": sinT, "rT": np.ascontiguousarray(R.T),
        "w1k": inp["cmp_k_w1"][0], "w1v": inp["cmp_v_w1"][0],
        "b1k": np.ascontiguousarray(inp["cmp_k_b1"][0][:, None]), "b1v": np.ascontiguousarray(inp["cmp_v_b1"][0][:, None]),
        "w2k": inp["cmp_k_w2"][0], "w2v": inp["cmp_v_w2"][0],
        "posk": np.ascontiguousarray(inp["cmp_pos_k"][0].T), "posv": np.ascontiguousarray(inp["cmp_pos_v"][0].T),
        "gsel": gsel, "cmask": np.ascontiguousarray(cmask), "bmask": np.ascontiguousarray(bmask), "ex": ex,
        "caus": np.ascontiguousarray(caus), "band": np.ascontiguousarray(band),
        "gup": inp["gdn_w_up"][0], "nup": inp["nsa_w_up"][0], "wo": inp["w_out"][0],
    }


def _pc(v):
    return np.ascontiguousarray(np.asarray(v, np.float32).reshape(16, 128).T)


def run_l1(inp):
    nc = build_l1()
    x = inp["x"]
    ada_b = np.ascontiguousarray(inp["ada_b"][0].reshape(144, 128).T)
    maps = []
    for c in range(NCORE):
        b, r = c // 4, c % 4
        maps.append({
            "xT": np.ascontiguousarray(x[b, r * NT:(r + 1) * NT, :].T),
            "cb": _pc(inp["c"][b]),
            "ada_w": inp["ada_w"][0], "ada_b": ada_b,
            "n1": _pc(inp["ffn1_norm"][0]), "n2": _pc(inp["mix_norm"][0]),
            "wg": inp["ffn1_w_gate"][0], "wu": inp["ffn1_w_up"][0], "wd": inp["ffn1_w_down"][0],
        })
    res = run_bass_kernel_spmd(nc, maps, core_ids=list(range(NCORE)))
    return res


def kernel(**inputs):
    inp = {k_: np.asarray(v) for k_, v in inputs.items()}
    nc = build_fused()
    maps = [fused_inputs(inp, c) for c in range(NCORE)]
    res = run_bass_kernel_spmd(nc, maps, core_ids=list(range(NCORE))).results
    out = np.empty((2, S, D), np.float32)
    for c in range(NCORE):
        out[c // 4, (c % 4) * NT:(c % 4 + 1) * NT, :] = res[c]["outT"].T
    return out
```
